# Optimizing a Trainium2 kernel written in Bass

```python
import math
import numpy as np
import jax
import jax.numpy as jnp
from jax import lax

D_MODEL = 2048
BATCH = 8
SEQ = 2048
DEPTH = 1

CHUNK = 64
Q_BLOCK = 128
HA = 8
DA_QK = 64
DA_V = 2 * DA_QK
QK_A = HA * 2 * DA_QK
WIDTH_A = HA * DA_V
HB = 8
DB = 128
WIDTH_B = HB * DB
LEFT_CHUNKS = 8
BAND = (LEFT_CHUNKS + 1) * CHUNK
REL_CLIP = 256
T5_BUCKETS = 32
T5_MAX_DIST = 128
IN_SPLITS = [QK_A, 2 * QK_A, 2 * QK_A + WIDTH_A,
             2 * QK_A + WIDTH_A + WIDTH_B,
             2 * QK_A + WIDTH_A + 2 * WIDTH_B,
             2 * QK_A + WIDTH_A + 3 * WIDTH_B]
IN_COLS = 2 * QK_A + WIDTH_A + 3 * WIDTH_B + 2 * D_MODEL
N_EXPERTS = 64
TOP_K = 8
N_GROUPS = 8
TOPK_GROUPS = 4
D_EXPERT = 512
D_SHARED = 512
ROUTED_SCALE = 2.5
MOE_BLOCK = 128
N_MOD = 6
EPS = 1e-6
HEAD_EPS = 1e-5
NEG = -1e30

kernel_name = "hybrid_chunk_causal_diffattn_bandattn_moe"


def rmsnorm(x, g):
    xf = x.astype(jnp.float32)
    y = xf * lax.rsqrt(jnp.mean(xf * xf, axis=-1, keepdims=True) + EPS)
    return (y * g.astype(jnp.float32)).astype(x.dtype)


def head_rmsnorm(x):
    xf = x.astype(jnp.float32)
    return (xf * lax.rsqrt(jnp.mean(xf * xf, axis=-1, keepdims=True) + HEAD_EPS)).astype(x.dtype)


def t5_bucket(rel):
    nb = T5_BUCKETS // 2
    ret = (rel > 0).astype(np.int32) * nb
    n = np.abs(rel)
    max_exact = nb // 2
    large = max_exact + (np.log(np.maximum(n, 1) / max_exact) / math.log(T5_MAX_DIST / max_exact)
                         * (nb - max_exact)).astype(np.int32)
    large = np.minimum(large, nb - 1)
    return (ret + np.where(n < max_exact, n, large)).astype(np.int32)


def diff_attention(q, k, v, lam, lam_init, t5_table):
    b_, s_ = q.shape[0], q.shape[1]
    scale = DA_QK ** -0.5
    outs = []
    for start in range(0, s_, Q_BLOCK):
        end = start + Q_BLOCK
        qpos = np.arange(start, end)
        kpos = np.arange(end)
        allowed = (kpos[None, :] // CHUNK) <= (qpos[:, None] // CHUNK)
        bias = jnp.transpose(t5_table[t5_bucket(kpos[None, :] - qpos[:, None])], (2, 0, 1))
        s = jnp.einsum('bqhmd,bkhmd->bhmqk', q[:, start:end], k[:, :end]).astype(jnp.float32) * scale
        s = jnp.where(allowed, s + bias.astype(jnp.float32)[None, :, None], NEG)
        p = jax.nn.softmax(s, axis=-1)
        a = p[:, :, 0] - lam * p[:, :, 1]
        outs.append(jnp.einsum('bhqk,bkhe->bqhe', a.astype(v.dtype), v[:, :end]))
    o = jnp.concatenate(outs, axis=1)
    o = head_rmsnorm(o) * (1.0 - lam_init)
    return o.reshape(b_, s_, WIDTH_A)


def chunk_band_attention(q, k, v, rel_table):
    b_, s_ = q.shape[0], q.shape[1]
    nc = s_ // CHUNK
    qc = q.reshape(b_, nc, CHUNK, HB, DB)
    pad = ((0, 0), (LEFT_CHUNKS * CHUNK, 0), (0, 0), (0, 0))
    kp = jnp.pad(k, pad).reshape(b_, nc + LEFT_CHUNKS, CHUNK, HB, DB)
    vp = jnp.pad(v, pad).reshape(b_, nc + LEFT_CHUNKS, CHUNK, HB, DB)
    idx = np.arange(nc)[:, None] + np.arange(LEFT_CHUNKS + 1)[None, :]
    kb = kp[:, idx].reshape(b_, nc, BAND, HB, DB)
    vb = vp[:, idx].reshape(b_, nc, BAND, HB, DB)
    s = jnp.einsum('bnqhd,bnkhd->bhnqk', qc, kb).astype(jnp.float32) * (DB ** -0.5)
    qq = np.arange(CHUNK)
    kk = np.arange(BAND) - LEFT_CHUNKS * CHUNK
    rel = np.clip(kk[None, :] - qq[:, None], -REL_CLIP, REL_CLIP) + REL_CLIP
    bias = jnp.transpose(rel_table[rel], (2, 0, 1)).astype(jnp.float32)
    valid = (np.arange(nc)[:, None] * CHUNK + kk[None, :]) >= 0
    s = jnp.where(valid[None, None, :, None, :], s + bias[None, :, None], NEG)
    p = jax.nn.softmax(s, axis=-1)
    o = jnp.einsum('bhnqk,bnkhd->bnqhd', p.astype(v.dtype), vb)
    return o.reshape(b_, s_, WIDTH_B)


def moe_ffn(u, w_router, router_bias, wg, wu, wd, sg, su, sd):
    t_ = u.shape[0]
    scores = jax.nn.sigmoid(jnp.dot(u, w_router).astype(jnp.float32))
    biased = scores + router_bias.astype(jnp.float32)
    grp = biased.reshape(t_, N_GROUPS, N_EXPERTS // N_GROUPS)
    grp_score = lax.top_k(grp, 2)[0].sum(-1)
    _, gidx = lax.top_k(grp_score, TOPK_GROUPS)
    gmask = jax.nn.one_hot(gidx, N_GROUPS, dtype=jnp.float32).sum(1) > 0
    emask = jnp.repeat(gmask, N_EXPERTS // N_GROUPS, axis=1)
    _, eidx = lax.top_k(jnp.where(emask, biased, -jnp.inf), TOP_K)
    w = jnp.take_along_axis(scores, eidx, axis=1)
    w = w / jnp.sum(w, axis=-1, keepdims=True) * ROUTED_SCALE
    a_ = t_ * TOP_K
    nb = -(-(a_ + N_EXPERTS * (MOE_BLOCK - 1)) // MOE_BLOCK)
    p_ = nb * MOE_BLOCK
    flat_e = eidx.reshape(a_)
    flat_tok = jnp.repeat(jnp.arange(t_, dtype=jnp.int32), TOP_K)
    flat_w = w.reshape(a_).astype(u.dtype)
    order = jnp.argsort(flat_e)
    se = flat_e[order]
    counts = jnp.bincount(flat_e, length=N_EXPERTS)
    starts = jnp.cumsum(counts) - counts
    pcounts = (counts + MOE_BLOCK - 1) // MOE_BLOCK * MOE_BLOCK
    pends = jnp.cumsum(pcounts)
    pstarts = pends - pcounts
    dest = pstarts[se] + jnp.arange(a_, dtype=jnp.int32) - starts[se]
    slot_tok = jnp.zeros((p_,), jnp.int32).at[dest].set(flat_tok[order])
    slot_w = jnp.zeros((p_,), u.dtype).at[dest].set(flat_w[order])
    block_e = jnp.minimum(jnp.searchsorted(pends, jnp.arange(nb) * MOE_BLOCK, side='right'),
                          N_EXPERTS - 1)

    def expert_block(args):
        tok, e = args
        xb = u[tok]
        hb = jax.nn.silu(xb @ wg[e]) * (xb @ wu[e])
        return hb @ wd[e]

    out = lax.map(expert_block, (slot_tok.reshape(nb, MOE_BLOCK), block_e))
    routed = jax.ops.segment_sum(out.reshape(p_, -1) * slot_w[:, None], slot_tok, num_segments=t_)
    shared = (jax.nn.silu(u @ sg) * (u @ su)) @ sd
    return routed + shared


def setup_inputs(seed: int = 0) -> dict:
    key = jax.random.key(seed)
    ks = jax.random.split(key, 24)
    f32 = jnp.float32
    D = D_MODEL

    def nrm(k, shape, scale):
        return jax.random.normal(k, shape, f32) * scale

    return {
        "x": nrm(ks[0], (BATCH, SEQ, D), 1.0),
        "c": nrm(ks[1], (BATCH, D), 1.0),
        "w_ada": nrm(ks[2], (DEPTH, D, N_MOD * D), 0.5 * D ** -0.5),
        "b_ada": nrm(ks[3], (DEPTH, N_MOD * D), 0.02),
        "g_attn": 1.0 + nrm(ks[4], (DEPTH, D), 0.02),
        "w_in": nrm(ks[5], (DEPTH, D, IN_COLS), D ** -0.5),
        "lambda_qk": nrm(ks[6], (DEPTH, 4, DA_QK), 0.1),
        "t5_bias": nrm(ks[7], (T5_BUCKETS, HA), 0.5),
        "rel_bias_b": nrm(ks[8], (DEPTH, 2 * REL_CLIP + 1, HB), 0.5),
        "w_up_a": nrm(ks[9], (DEPTH, WIDTH_A, D), WIDTH_A ** -0.5),
        "w_up_b": nrm(ks[10], (DEPTH, WIDTH_B, D), WIDTH_B ** -0.5),
        "w_o": nrm(ks[11], (DEPTH, D, D), D ** -0.5),
        "g_moe": 1.0 + nrm(ks[12], (DEPTH, D), 0.02),
        "w_router": nrm(ks[13], (DEPTH, D, N_EXPERTS), D ** -0.5),
        "router_bias": nrm(ks[14], (DEPTH, N_EXPERTS), 0.01),
        "w_exp_gate": nrm(ks[15], (DEPTH, N_EXPERTS, D, D_EXPERT), D ** -0.5),
        "w_exp_up": nrm(ks[16], (DEPTH, N_EXPERTS, D, D_EXPERT), D ** -0.5),
        "w_exp_down": nrm(ks[17], (DEPTH, N_EXPERTS, D_EXPERT, D), D_EXPERT ** -0.5),
        "w_sh_gate": nrm(ks[18], (DEPTH, D, D_SHARED), D ** -0.5),
        "w_sh_up": nrm(ks[19], (DEPTH, D, D_SHARED), D ** -0.5),
        "w_sh_down": nrm(ks[20], (DEPTH, D_SHARED, D), D_SHARED ** -0.5),
        "g_final": 1.0 + nrm(ks[21], (D,), 0.02),
    }


def reference(x, c, w_ada, b_ada, g_attn, w_in, lambda_qk, t5_bias, rel_bias_b, w_up_a, w_up_b, w_o,
              g_moe, w_router, router_bias, w_exp_gate, w_exp_up, w_exp_down,
              w_sh_gate, w_sh_up, w_sh_down, g_final):
    b_, s_, d_ = x.shape
    h = x
    for l in range(DEPTH):
        mod = jnp.dot(jax.nn.silu(c), w_ada[l]) + b_ada[l]
        sh_a, sc_a, gt_a, sh_m, sc_m, gt_m = jnp.split(mod[:, None, :], N_MOD, axis=-1)

        u = rmsnorm(h, g_attn[l]) * (1.0 + sc_a) + sh_a
        proj = u @ w_in[l]
        qa, ka, va, qb, kb, vb, gates = jnp.split(proj, IN_SPLITS, axis=-1)
        lam_init = 0.8 - 0.6 * math.exp(-0.3 * l)
        lq = lambda_qk[l].astype(jnp.float32)
        lam = jnp.exp(jnp.sum(lq[0] * lq[1])) - jnp.exp(jnp.sum(lq[2] * lq[3])) + lam_init
        ya = diff_attention(qa.reshape(b_, s_, HA, 2, DA_QK), ka.reshape(b_, s_, HA, 2, DA_QK),
                            va.reshape(b_, s_, HA, DA_V), lam, lam_init, t5_bias)
        yb = chunk_band_attention(qb.reshape(b_, s_, HB, DB), kb.reshape(b_, s_, HB, DB),
                                  vb.reshape(b_, s_, HB, DB), rel_bias_b[l])
        g_a, g_b = jnp.split(jax.nn.sigmoid(gates), 2, axis=-1)
        merged = g_a * (ya @ w_up_a[l]) + g_b * (yb @ w_up_b[l])
        h = h + gt_a * (merged @ w_o[l])

        v = rmsnorm(h, g_moe[l]) * (1.0 + sc_m) + sh_m
        y = moe_ffn(v.reshape(b_ * s_, d_), w_router[l], router_bias[l], w_exp_gate[l], w_exp_up[l],
                    w_exp_down[l], w_sh_gate[l], w_sh_up[l], w_sh_down[l])
        h = h + gt_m * y.reshape(b_, s_, d_)
    return rmsnorm(h, g_final)
```

```python
import contextlib
import os
import math
import numpy as np
import concourse.bass as bass
import concourse.mybir as mybir
from concourse.bass_utils import run_bass_kernel_spmd

F32 = mybir.dt.float32
BF16 = mybir.dt.bfloat16
AF = mybir.ActivationFunctionType
ALU = mybir.AluOpType
AX = mybir.AxisListType

T = 2048
D = 2048
KC = 16
NE = 64
NEG = -1e30


class _Stop(Exception):
    pass


class Tk:
    __slots__ = ("sem", "val", "eng")

    def __init__(self, sem, val, eng):
        self.sem = sem
        self.val = val
        self.eng = eng


class Buf:
    def __init__(self, name=""):
        self.name = name
        self.w = {}
        self.r = {}


class DSem:
    def __init__(self, h):
        self.h = h
        self.cnt = 0


class KB:
    def __init__(self, nc, es):
        self.nc = nc
        self.es = es
        self.E = {"pe": nc.tensor, "act": nc.scalar, "dve": nc.vector, "pool": nc.gpsimd, "sp": nc.sync}
        self.psem = {e: es.enter_context(nc.semaphore("prog_" + e)) for e in ("pe", "act", "dve", "pool")}
        self.pcnt = {e: 0 for e in self.psem}
        self.waited = {e: {} for e in self.E}
        self.dsems = []

    def dsem(self, name):
        d = DSem(self.es.enter_context(self.nc.semaphore(name)))
        self.dsems.append(d)
        return d

    def wait(self, e, tk):
        if tk is None:
            return
        if tk.eng == "pe" and e == "pe":
            return
        k = id(tk.sem)
        if self.waited[e].get(k, 0) >= tk.val:
            return
        self.E[e].wait_ge(tk.sem, tk.val)
        self.waited[e][k] = tk.val

    def _deps(self, e, reads, writes, waw, skipsem=None):
        for b in reads:
            for t in b.w.values():
                self.wait(e, t)
        for b in writes:
            if waw:
                for t in b.w.values():
                    if skipsem is not None and t.sem is skipsem:
                        continue
                    self.wait(e, t)
            for t in b.r.values():
                self.wait(e, t)

    def _commit(self, tk, reads, writes, waw):
        k = id(tk.sem)
        for b in reads:
            o = b.r.get(k)
            if o is None or o.val < tk.val:
                b.r[k] = tk
        for b in writes:
            o = b.w.get(k)
            if o is None or o.val < tk.val:
                b.w[k] = tk

    def op(self, e, fn, reads=(), writes=(), waw=True):
        self._deps(e, reads, writes, waw)
        ins = fn(self.E[e])
        self.pcnt[e] += 1
        ins.then_inc(self.psem[e], 1)
        tk = Tk(self.psem[e], self.pcnt[e], e)
        self._commit(tk, reads, writes, waw)
        return tk

    def dma(self, q, ds, out, in_, reads=(), writes=(), waw=True):
        self._deps(q, reads, writes, waw, skipsem=ds.h)
        ins = self.E[q].dma_start(out=out, in_=in_)
        ds.cnt += 16
        ins.then_inc(ds.h, 16)
        tk = Tk(ds.h, ds.cnt, "dma")
        self._commit(tk, reads, writes, waw)
        return tk

    def barrier(self):
        for e in self.E:
            for p in self.psem:
                if self.pcnt[p] > 0:
                    self.wait(e, Tk(self.psem[p], self.pcnt[p], "x"))
            for d in self.dsems:
                if d.cnt > 0:
                    self.wait(e, Tk(d.h, d.cnt, "dma"))


def build(upto=9, dbg=False):
    nc = bass.Bass("TRN2", target_bir_lowering=False)

    def din(name, shape, dt=F32):
        return nc.dram_tensor(name, shape, dt, kind="ExternalInput").ap()

    def dscr(name, shape, dt):
        return nc.dram_tensor(name, shape, dt, kind=("ExternalOutput" if dbg else "Internal")).ap()

    x = din("x", [T, D])
    cT = din("cT", [128, 16])
    w_ada = din("w_ada", [D, 6 * D])
    b_ada = din("b_ada", [1, 6 * D])
    gfm = din("gfm", [128, 32])
    g_final = din("g_final", [1, D])
    w_in = din("w_in", [D, 10240])
    lam_in = din("lam", [1, 256])
    biasA = din("biasA", [8, 128, 2048])
    biasB = din("biasB", [8, 128, 640])
    w_up_a = din("w_up_a", [1024, D])
    w_up_b = din("w_up_b", [1024, D])
    w_o = din("w_o", [D, D])
    w_router = din("w_router", [D, NE])
    rbias = din("rbias", [1, NE])
    if upto >= 7:
        wgu = din("wgu", [NE + 1, 4, 128, 4096])
        wd = din("wd", [NE + 1, 512, D])
    out = nc.dram_tensor("out", [T, D], F32, kind="ExternalOutput").ap()

    qkT = [dscr(f"qkT{i}", [1024, T], BF16) for i in range(4)]
    vv = [dscr(f"vv{i}", [T, 1024], BF16) for i in range(2)]
    gT = dscr("gT", [4096, T], BF16)
    h1d = dscr("h1d", [T, D], F32)
    VTd = dscr("VTd", [KC, 128, T], BF16)
    wcTd = dscr("wcTd", [NE, T], F32)
    modsave = dscr("modsave", [2, D], F32)

    with contextlib.suppress(_Stop), contextlib.ExitStack() as es:
        kb = KB(nc, es)

        def phase(n):
            if n > upto:
                kb.barrier()
                raise _Stop()

        uniq = [0]

        def sb(stack, name, shape, dt):
            uniq[0] += 1
            return stack.enter_context(nc.sbuf_tensor(f"{name}_{uniq[0]}", shape, dt))

        def ps(stack, name, shape, dt):
            uniq[0] += 1
            return stack.enter_context(nc.psum_tensor(f"{name}_{uniq[0]}", shape, dt))

        identf = sb(es, "identf", [128, 128], F32)
        identb = sb(es, "identb", [128, 128], BF16)
        ones_f = sb(es, "ones_f", [128, 128], F32)
        B_const = Buf("const")
        kb.op("pool", lambda g: g.memset(identf[:], 1.0), writes=[B_const])
        kb.op("pool", lambda g: g.affine_select(out=identf[:], in_=identf[:], pattern=[[-1, 128]],
                                                compare_op=ALU.is_equal, fill=0.0, base=0,
                                                channel_multiplier=1), reads=[B_const], writes=[B_const])
        kb.op("dve", lambda v: v.tensor_copy(out=identb[:], in_=identf[:]), reads=[B_const], writes=[B_const], waw=False)
        kb.op("dve", lambda v: v.memset(ones_f[:], 1.0), writes=[B_const], waw=False)

        A_a = sb(es, "A_a", [128, 16], F32)
        S_a = sb(es, "S_a", [128, 16], F32)
        A_m = sb(es, "A_m", [128, 16], F32)
        S_m = sb(es, "S_m", [128, 16], F32)
        B_mod = Buf("modfm")

        dq = [kb.dsem(f"dq{i}") for i in range(12)]
        dp = [kb.dsem(f"dp{i}") for i in range(4)]

        with contextlib.ExitStack() as s0:
            cts = sb(s0, "cts", [128, 16], F32)
            scs = sb(s0, "scs", [128, 16], F32)
            gfs = sb(s0, "gfs", [128, 32], F32)
            scb = sb(s0, "scb", [128, 16, 128], F32)
            wab = [sb(s0, f"wab{i}", [128, 16, 512], F32) for i in range(2)]
            bbc = [sb(s0, f"bbc{i}", [128, 512], F32) for i in range(2)]
            modbc = [sb(s0, f"modbc{i}", [128, D], F32) for i in range(6)]
            tmpd = sb(s0, "tmpd", [128, 128], F32)
            fm = sb(s0, "fm", [128, 4, 16], F32)
            pmod = [ps(s0, f"pmod{i}", [128, 512], F32) for i in range(2)]
            B_c = Buf()
            B_scb = Buf()
            B_wab = [Buf(), Buf()]
            B_bbc = [Buf(), Buf()]
            B_pm = [Buf(), Buf()]
            B_modbc = [Buf() for _ in range(6)]
            B_tmp = Buf()
            B_fm = Buf()
            kb.dma("sp", dq[0], cts[:], cT[:, :], writes=[B_c])
            kb.dma("sp", dq[0], gfs[:], gfm[:, :], writes=[B_c])
            kb.op("act", lambda a: a.activation(out=scs[:], in_=cts[:], func=AF.Silu), reads=[B_c], writes=[B_scb])
            for kc in range(KC):
                kb.op("dve", lambda v, kc=kc: v.tensor_scalar(out=scb[:, kc, :], in0=ones_f[:], scalar1=scs[:, kc:kc + 1],
                                                             scalar2=None, op0=ALU.mult),
                      reads=[B_scb, B_const], writes=[B_c], waw=False)
            wav = w_ada.rearrange("(kc p) n -> p kc n", p=128)
            for j in range(24):
                i = j % 2
                kb.dma("sp", dq[1 + i], wab[i][:, 0:8, :], wav[:, 0:8, j * 512:(j + 1) * 512], writes=[B_wab[i]])
                kb.dma("act", dq[1 + i], wab[i][:, 8:16, :], wav[:, 8:16, j * 512:(j + 1) * 512], writes=[B_wab[i]])
                kb.dma("sp", dq[3 + i], bbc[i][:], b_ada[0, j * 512:(j + 1) * 512].partition_broadcast(128),
                       writes=[B_bbc[i]])

                def mm(pe, i=i):
                    r = None
                    for kc in range(KC):
                        r = pe.matmul(pmod[i][:], lhsT=scb[:, kc, :], rhs=wab[i][:, kc, :], start=(kc == 0), stop=(kc == KC - 1))
                    return r
                kb.op("pe", mm, reads=[B_c, B_wab[i]], writes=[B_pm[i]])
                mi, cb = j // 4, (j % 4) * 512
                kb.op("dve", lambda v, i=i, mi=mi, cb=cb: v.tensor_tensor(out=modbc[mi][:, cb:cb + 512], in0=pmod[i][:],
                                                                           in1=bbc[i][:], op=ALU.add),
                      reads=[B_pm[i], B_bbc[i]], writes=[B_modbc[mi]], waw=False)
            for fi, mi in enumerate((0, 1, 3, 4)):
                for kc in range(KC):
                    kb.op("dve", lambda v, mi=mi, kc=kc: v.tensor_tensor(out=tmpd[:], in0=modbc[mi][:, kc * 128:(kc + 1) * 128],
                                                                         in1=identf[:], op=ALU.mult),
                          reads=[B_modbc[mi], B_const], writes=[B_tmp])
                    kb.op("dve", lambda v, fi=fi, kc=kc: v.reduce_sum(out=fm[:, fi, kc:kc + 1], in_=tmpd[:], axis=AX.X),
                          reads=[B_tmp], writes=[B_fm], waw=False)
            kb.op("dve", lambda v: v.tensor_copy(out=S_a[:], in_=fm[:, 0, :]), reads=[B_fm], writes=[B_mod], waw=False)
            kb.op("dve", lambda v: v.tensor_copy(out=S_m[:], in_=fm[:, 2, :]), reads=[B_fm], writes=[B_mod], waw=False)
            kb.op("dve", lambda v: v.scalar_tensor_tensor(out=A_a[:], in0=fm[:, 1, :], scalar=1.0, in1=gfs[:, 0:16],
                                                          op0=ALU.add, op1=ALU.mult), reads=[B_fm, B_c], writes=[B_mod], waw=False)
            kb.op("dve", lambda v: v.scalar_tensor_tensor(out=A_m[:], in0=fm[:, 3, :], scalar=1.0, in1=gfs[:, 16:32],
                                                          op0=ALU.add, op1=ALU.mult), reads=[B_fm, B_c], writes=[B_mod], waw=False)
            B_ms = Buf()
            kb.dma("sp", dq[5], modsave[0:1, :], modbc[2][0:1, :], reads=[B_modbc[2]], writes=[B_ms], waw=False)
            kb.dma("sp", dq[5], modsave[1:2, :], modbc[5][0:1, :], reads=[B_modbc[5]], writes=[B_ms], waw=False)
            kb.barrier()

        def norm_tile(stack_bufs, src_tile, B_src, tt, A_fm, S_fm, dst_bf, B_dst, dst32=None, B_dst16=None):
            junk, ss, rstd, xh, p4, B_junk, B_ss, B_xh, B_p4 = stack_bufs
            kb.op("act", lambda a: a.activation(out=junk[:], in_=src_tile[:], func=AF.Square, accum_out=ss[:]),
                  reads=[B_src], writes=[B_junk, B_ss])
            kb.op("act", lambda a: a.activation(out=rstd[:], in_=ss[:], func=AF.Sqrt, bias=1e-6, scale=1.0 / D),
                  reads=[B_ss], writes=[B_ss])
            kb.op("dve", lambda v: v.reciprocal(out=rstd[:], in_=rstd[:]), reads=[B_ss], writes=[B_ss])
            kb.op("dve", lambda v: v.tensor_scalar(out=xh[:], in0=src_tile[:], scalar1=rstd[:, 0:1], scalar2=None, op0=ALU.mult),
                  reads=[B_src, B_ss], writes=[B_xh])

            def tr(pe):
                r = None
                for kc in range(KC):
                    r = pe.transpose(out=p4[:, kc * 128:(kc + 1) * 128], in_=xh[:, kc * 128:(kc + 1) * 128], identity=identf[:])
                return r
            kb.op("pe", tr, reads=[B_xh, B_const], writes=[B_p4])
            for kc in range(KC):
                o = dst32(kc) if dst32 is not None else dst_bf(kc)
                if kc < 8:
                    kb.op("act", lambda a, kc=kc, o=o: a.activation(out=o, in_=p4[:, kc * 128:(kc + 1) * 128], func=AF.Identity,
                                                                    bias=S_fm[:, kc:kc + 1], scale=A_fm[:, kc:kc + 1]),
                          reads=[B_p4, B_mod], writes=[B_dst], waw=False)
                else:
                    kb.op("dve", lambda v, kc=kc, o=o: v.tensor_scalar(out=o, in0=p4[:, kc * 128:(kc + 1) * 128],
                                                                       scalar1=A_fm[:, kc:kc + 1], scalar2=S_fm[:, kc:kc + 1],
                                                                       op0=ALU.mult, op1=ALU.add),
                          reads=[B_p4, B_mod], writes=[B_dst], waw=False)
            if dst32 is not None:
                kb.op("pool", lambda g: g.tensor_copy(out=dst_bf(None), in_=dst32(None)), reads=[B_dst], writes=[B_dst16])

        with contextlib.ExitStack() as s1:
            uT = sb(s1, "uT", [128, KC, T], BF16)
            B_uT = Buf("uT")
            phase(1)
            with contextlib.ExitStack() as s1a:
                xt = [sb(s1a, f"xt{i}", [128, D], F32) for i in range(2)]
                junk = sb(s1a, "junk", [128, D], BF16)
                ss = sb(s1a, "ss", [128, 1], F32)
                rstd = sb(s1a, "rstd", [128, 1], F32)
                xh = sb(s1a, "xh", [128, D], F32)
                p4 = ps(s1a, "p4", [128, D], F32)
                B_xt = [Buf(), Buf()]
                nb = (junk, ss, rstd, xh, p4, Buf(), Buf(), Buf(), Buf())
                for tt in range(int(os.environ.get('NT1', '16'))):
                    i = tt % 2
                    kb.dma("sp", dq[i], xt[i][:], x[tt * 128:(tt + 1) * 128, :], writes=[B_xt[i]])
                    norm_tile(nb, xt[i], B_xt[i], tt, A_a, S_a,
                              lambda kc, tt=tt: uT[:, kc, tt * 128:(tt + 1) * 128], B_uT)
                kb.barrier()

            phase(2)
            with contextlib.ExitStack() as s2:
                wb = [sb(s2, f"wb{i}", [128, KC, 512], BF16) for i in range(2)]
                B_wb = [Buf(), Buf()]
                stg = [sb(s2, f"stg{i}", [128, 512], BF16) for i in range(4)]
                B_stg = [Buf() for _ in range(4)]
                pb = [ps(s2, f"pb{i}", [128, 512], F32) for i in range(4)]
                B_pb = [Buf() for _ in range(4)]
                B_scr = Buf("scratchA2")
                wiv = w_in.rearrange("(kc p) n -> p kc n", p=128)
                cnt = [0]

                def evac_store(pi, kind, scale, dst_ap):
                    n = cnt[0]
                    cnt[0] += 1
                    si = n % 4
                    if kind == "sig":
                        kb.op("act", lambda a: a.activation(out=stg[si][:], in_=pb[pi][:], func=AF.Sigmoid),
                              reads=[B_pb[pi]], writes=[B_stg[si]])
                    elif n % 2 == 0:
                        kb.op("act", lambda a: a.activation(out=stg[si][:], in_=pb[pi][:], func=AF.Identity, scale=float(scale)),
                              reads=[B_pb[pi]], writes=[B_stg[si]])
                    else:
                        kb.op("dve", lambda v: v.tensor_scalar(out=stg[si][:], in0=pb[pi][:], scalar1=float(scale), scalar2=None,
                                                               op0=ALU.mult),
                              reads=[B_pb[pi]], writes=[B_stg[si]])
                    kb.dma("sp", dq[4 + si], dst_ap, stg[si][:], reads=[B_stg[si]], writes=[B_scr], waw=False)

                groups = []
                for g in range(20):
                    if g < 2:
                        groups.append(("fm", qkT[0], g * 512, 0.125, "lin"))
                    elif g < 4:
                        groups.append(("fm", qkT[1], (g - 2) * 512, 1.0, "lin"))
                    elif g < 6:
                        groups.append(("tm", vv[0], (g - 4) * 512, 1.0, "lin"))
                    elif g < 8:
                        groups.append(("fm", qkT[2], (g - 6) * 512, 128.0 ** -0.5, "lin"))
                    elif g < 10:
                        groups.append(("fm", qkT[3], (g - 8) * 512, 1.0, "lin"))
                    elif g < 12:
                        groups.append(("tm", vv[1], (g - 10) * 512, 1.0, "lin"))
                    else:
                        groups.append(("fm", gT, (g - 12) * 512, 1.0, "sig"))
                pcount = 0
                for g in range(20):
                    i = g % 2
                    kind, dst, base, scale, ev = groups[g]
                    kb.dma("pool", dp[i], wb[i][:, 0:8, :], wiv[:, 0:8, g * 512:(g + 1) * 512], writes=[B_wb[i]])
                    kb.dma("pool", dp[i], wb[i][:, 8:16, :], wiv[:, 8:16, g * 512:(g + 1) * 512], writes=[B_wb[i]])
                    if kind == "fm":
                        for m in range(4):
                            for tb in range(4):
                                pi = pcount % 4
                                pcount += 1

                                def mm(pe, i=i, m=m, tb=tb, pi=pi):
                                    r = None
                                    for kc in range(KC):
                                        r = pe.matmul(pb[pi][:], lhsT=wb[i][:, kc, m * 128:(m + 1) * 128],
                                                      rhs=uT[:, kc, tb * 512:(tb + 1) * 512], start=(kc == 0), stop=(kc == KC - 1))
                                    return r
                                kb.op("pe", mm, reads=[B_wb[i], B_uT], writes=[B_pb[pi]])
                                evac_store(pi, ev, scale, dst[base + m * 128: base + (m + 1) * 128, tb * 512:(tb + 1) * 512])
                    else:
                        for tt in range(16):
                            pi = pcount % 4
                            pcount += 1

                            def mm(pe, i=i, tt=tt, pi=pi):
                                r = None
                                for kc in range(KC):
                                    r = pe.matmul(pb[pi][:], lhsT=uT[:, kc, tt * 128:(tt + 1) * 128], rhs=wb[i][:, kc, :],
                                                  start=(kc == 0), stop=(kc == KC - 1))
                                return r
                            kb.op("pe", mm, reads=[B_wb[i], B_uT], writes=[B_pb[pi]])
                            evac_store(pi, ev, scale, dst[tt * 128:(tt + 1) * 128, base:base + 512])
                kb.barrier()

        phase(3)
        with contextlib.ExitStack() as s3:
            yT = [sb(s3, f"yT{i}", [128, 8, T], BF16) for i in range(2)]
            B_yT = [Buf(), Buf()]
            with contextlib.ExitStack() as s3a:
                qTh = [sb(s3a, f"qTh{i}", [128, T], BF16) for i in range(2)]
                kTh = [sb(s3a, f"kTh{i}", [128, T], BF16) for i in range(2)]
                vh = [sb(s3a, f"vh{i}", [128, 16, 128], BF16) for i in range(2)]
                Rh = [sb(s3a, f"Rh{i}", [128, T], BF16) for i in range(2)]
                B_hd = [Buf(), Buf()]
                P = [sb(s3a, f"P{i}", [128, T], BF16) for i in range(2)]
                B_P = [Buf(), Buf()]
                PTs = [sb(s3a, f"PTs{i}", [128, 16, 128], BF16) for i in range(2)]
                B_PTs = [Buf(), Buf()]
                lq = sb(s3a, "lq", [128, 256], F32)
                ltmp = sb(s3a, "ltmp", [128, 64], F32)
                lst = sb(s3a, "lst", [128, 4], F32)
                nl = sb(s3a, "nl", [128, 1], F32)
                B_l = Buf()
                nmx = [sb(s3a, f"nmx{i}", [128, 1], F32) for i in range(2)]
                sm = [sb(s3a, f"sm{i}", [128, 1], F32) for i in range(2)]
                rr = [sb(s3a, f"rr{i}", [128, 1], F32) for i in range(2)]
                B_st = [Buf(), Buf()]
                o0 = sb(s3a, "o0", [128, 128], F32)
                oo = sb(s3a, "oo", [128, 128], F32)
                hjk = sb(s3a, "hjk", [128, 128], F32)
                hs = sb(s3a, "hs", [128, 1], F32)
                B_o = Buf()
                ybf = [sb(s3a, f"ybf{i}", [128, 128], BF16) for i in range(2)]
                B_ybf = [Buf(), Buf()]
                S = ps(s3a, "S", [128, T], F32)
                PT = ps(s3a, "PT", [128, T], BF16)
                O_ = ps(s3a, "O", [128, 512], F32)
                O = O_[:, 0:128]
                ytp_ = ps(s3a, "ytp", [128, 1024], BF16)
                ytp = ytp_[:, 0:128]
                B_S, B_PT, B_O, B_ytp = Buf(), Buf(), Buf(), Buf()

                kb.dma("sp", dq[8], lq[:], lam_in[0, :].partition_broadcast(128), writes=[B_l])
                for j in range(2):
                    kb.op("dve", lambda v, j=j: v.tensor_tensor(out=ltmp[:], in0=lq[:, j * 128:j * 128 + 64],
                                                                in1=lq[:, j * 128 + 64:j * 128 + 128], op=ALU.mult),
                          reads=[B_l], writes=[B_l])
                    kb.op("dve", lambda v, j=j: v.reduce_sum(out=lst[:, j:j + 1], in_=ltmp[:], axis=AX.X), reads=[B_l], writes=[B_l])
                kb.op("act", lambda a: a.activation(out=lst[:, 2:4], in_=lst[:, 0:2], func=AF.Exp), reads=[B_l], writes=[B_l])
                lam_init = 0.8 - 0.6 * math.exp(-0.3 * 0)
                kb.op("dve", lambda v: v.scalar_tensor_tensor(out=nl[:], in0=lst[:, 3:4], scalar=-lam_init, in1=lst[:, 2:3],
                                                              op0=ALU.add, op1=ALU.subtract), reads=[B_l], writes=[B_l])

                step = [0]
                for which in range(2):
                    nmaps = 2 if which == 0 else 1
                    dk = 64 if which == 0 else 128
                    W = 2048 if which == 0 else 640
                    bsrc = biasA if which == 0 else biasB
                    qd, kd, vd = qkT[2 * which], qkT[2 * which + 1], vv[which]
                    vdv = vd.rearrange("(tt p) c -> p tt c", p=128)
                    for h in range(8):
                        hi = (which * 8 + h) % 2
                        kb.dma("sp", dq[hi], qTh[hi][:], qd[h * 128:(h + 1) * 128, :], writes=[B_hd[hi]])
                        kb.dma("sp", dq[hi], kTh[hi][:], kd[h * 128:(h + 1) * 128, :], writes=[B_hd[hi]])
                        kb.dma("act", dq[hi], vh[hi][:], vdv[:, :, h * 128:(h + 1) * 128], writes=[B_hd[hi]])
                        kb.dma("pool", dp[hi], Rh[hi][:, 0:W], bsrc[h, :, :], writes=[B_hd[hi]], waw=False)
                        for qb in range(16):
                            kend = (qb + 1) * 128
                            k0 = 0 if which == 0 else max(0, qb * 128 - 512)
                            nk = kend - k0
                            roff = W - nk
                            nblk = nk // 128
                            for m in range(nmaps):
                                sidx = step[0] % 2
                                step[0] += 1

                                def qk(pe, hi=hi, m=m, qb=qb, k0=k0, nk=nk, roff=roff):
                                    r = None
                                    c0 = 0
                                    while c0 < nk:
                                        n = min(512, nk - c0)
                                        pe.matmul(S[:, c0:c0 + n], lhsT=qTh[hi][m * dk:(m + 1) * dk, qb * 128:(qb + 1) * 128],
                                                  rhs=kTh[hi][m * dk:(m + 1) * dk, k0 + c0:k0 + c0 + n], start=True, stop=False)
                                        r = pe.matmul(S[:, c0:c0 + n], lhsT=identb[:], rhs=Rh[hi][:, roff + c0:roff + c0 + n],
                                                      start=False, stop=True)
                                        c0 += n
                                    return r
                                kb.op("pe", qk, reads=[B_hd[hi], B_const], writes=[B_S])
                                kb.op("dve", lambda v, sidx=sidx, nk=nk: v.tensor_reduce(out=nmx[sidx][:], in_=S[:, 0:nk], axis=AX.X,
                                                                                          op=ALU.max, negate=True),
                                      reads=[B_S], writes=[B_st[sidx]])
                                kb.op("act", lambda a, sidx=sidx, nk=nk: a.activation(out=P[sidx][:, 0:nk], in_=S[:, 0:nk], func=AF.Exp,
                                                                                      bias=nmx[sidx][:, 0:1], scale=1.0,
                                                                                      accum_out=sm[sidx][:]),
                                      reads=[B_S, B_st[sidx]], writes=[B_P[sidx], B_st[sidx]])

                                def trp(pe, sidx=sidx, nblk=nblk):
                                    r = None
                                    for b in range(nblk):
                                        r = pe.transpose(out=PT[:, b * 128:(b + 1) * 128], in_=P[sidx][:, b * 128:(b + 1) * 128],
                                                         identity=identb[:])
                                    return r
                                kb.op("pe", trp, reads=[B_P[sidx], B_const], writes=[B_PT])
                                half = min(nblk, 8)
                                kb.op("act", lambda a, sidx=sidx, half=half: a.copy(out=PTs[sidx][:, 0:half, :],
                                                                                    in_=PT[:, 0:half * 128].rearrange("p (b q) -> p b q", q=128)),
                                      reads=[B_PT], writes=[B_PTs[sidx]])
                                if nblk > half:
                                    kb.op("dve", lambda v, sidx=sidx, half=half, nblk=nblk: v.tensor_copy(
                                        out=PTs[sidx][:, half:nblk, :],
                                        in_=PT[:, half * 128:nblk * 128].rearrange("p (b q) -> p b q", q=128)),
                                        reads=[B_PT], writes=[B_PTs[sidx]], waw=False)

                                def pv(pe, sidx=sidx, nblk=nblk, hi=hi, k0=k0):
                                    r = None
                                    for b in range(nblk):
                                        r = pe.matmul(O[:], lhsT=PTs[sidx][:, b, :], rhs=vh[hi][:, k0 // 128 + b, :],
                                                      start=(b == 0), stop=(b == nblk - 1))
                                    return r
                                kb.op("pe", pv, reads=[B_PTs[sidx], B_hd[hi]], writes=[B_O])
                                kb.op("dve", lambda v, sidx=sidx: v.reciprocal(out=rr[sidx][:], in_=sm[sidx][:]),
                                      reads=[B_st[sidx]], writes=[B_st[sidx]])
                                yi = (which * 128 + h * 16 + qb) % 2
                                if which == 1:
                                    kb.op("dve", lambda v, sidx=sidx, yi=yi: v.tensor_scalar(out=ybf[yi][:], in0=O[:], scalar1=rr[sidx][:, 0:1],
                                                                                            scalar2=None, op0=ALU.mult),
                                          reads=[B_O, B_st[sidx]], writes=[B_ybf[yi]])
                                elif m == 0:
                                    kb.op("dve", lambda v, sidx=sidx: v.tensor_scalar(out=o0[:], in0=O[:], scalar1=rr[sidx][:, 0:1],
                                                                                     scalar2=None, op0=ALU.mult),
                                          reads=[B_O, B_st[sidx]], writes=[B_o])
                                else:
                                    kb.op("dve", lambda v, sidx=sidx: v.tensor_tensor(out=rr[sidx][:], in0=rr[sidx][:], in1=nl[:], op=ALU.mult),
                                          reads=[B_st[sidx], B_l], writes=[B_st[sidx]])
                                    kb.op("dve", lambda v, sidx=sidx: v.scalar_tensor_tensor(out=oo[:], in0=O[:], scalar=rr[sidx][:, 0:1],
                                                                                            in1=o0[:], op0=ALU.mult, op1=ALU.add),
                                          reads=[B_O, B_st[sidx], B_o], writes=[B_o])
                            if which == 0:
                                kb.op("act", lambda a: a.activation(out=hjk[:], in_=oo[:], func=AF.Square, accum_out=hs[:]),
                                      reads=[B_o], writes=[B_o])
                                kb.op("act", lambda a: a.activation(out=hs[:], in_=hs[:], func=AF.Sqrt, bias=1e-5, scale=1.0 / 128),
                                      reads=[B_o], writes=[B_o])
                                kb.op("dve", lambda v: v.reciprocal(out=hs[:], in_=hs[:]), reads=[B_o], writes=[B_o])
                                kb.op("dve", lambda v, yi=yi: v.tensor_scalar(out=ybf[yi][:], in0=oo[:], scalar1=hs[:, 0:1],
                                                                             scalar2=float(1.0 - lam_init), op0=ALU.mult, op1=ALU.mult),
                                      reads=[B_o], writes=[B_ybf[yi]])
                            kb.op("pe", lambda pe, yi=yi: pe.transpose(out=ytp[:], in_=ybf[yi][:], identity=identb[:]),
                                  reads=[B_ybf[yi], B_const], writes=[B_ytp])
                            kb.op("act", lambda a, which=which, h=h, qb=qb: a.copy(out=yT[which][:, h, qb * 128:(qb + 1) * 128], in_=ytp[:]),
                                  reads=[B_ytp], writes=[B_yT[which]], waw=False)
                kb.barrier()

            phase(4)
            with contextlib.ExitStack() as s4:
                mT = sb(s4, "mT", [128, KC, T], BF16)
                B_mT = Buf("mT")
                with contextlib.ExitStack() as s4a:
                    wua = [sb(s4a, f"wua{i}", [128, 8, 256], BF16) for i in range(2)]
                    wub = [sb(s4a, f"wub{i}", [128, 8, 256], BF16) for i in range(2)]
                    B_wu = [Buf(), Buf()]
                    gat = [sb(s4a, f"gat{i}", [128, 512], BF16) for i in range(2)]
                    gbt = [sb(s4a, f"gbt{i}", [128, 512], BF16) for i in range(2)]
                    B_gt = [Buf(), Buf()]
                    m1 = [sb(s4a, f"m1{i}", [128, 512], F32) for i in range(2)]
                    m2 = [sb(s4a, f"m2{i}", [128, 512], F32) for i in range(2)]
                    B_m = [Buf(), Buf()]
                    pa = [ps(s4a, f"pa{i}", [128, 512], F32) for i in range(2)]
                    pbb = [ps(s4a, f"pbb{i}", [128, 512], F32) for i in range(2)]
                    B_pab = [Buf(), Buf()]
                    wuav = w_up_a.rearrange("(kc p) n -> p kc n", p=128)
                    wubv = w_up_b.rearrange("(kc p) n -> p kc n", p=128)
                    n = 0
                    for dg in range(8):
                        wi = dg % 2
                        kb.dma("pool", dp[wi], wua[wi][:], wuav[:, :, dg * 256:(dg + 1) * 256], writes=[B_wu[wi]])
                        kb.dma("pool", dp[wi], wub[wi][:], wubv[:, :, dg * 256:(dg + 1) * 256], writes=[B_wu[wi]])
                        for sub in range(2):
                            dc = dg * 2 + sub
                            for tb in range(4):
                                i = n % 2
                                n += 1
                                kb.dma("sp", dq[2 + i], gat[i][:], gT[dc * 128:(dc + 1) * 128, tb * 512:(tb + 1) * 512], writes=[B_gt[i]])
                                kb.dma("sp", dq[2 + i], gbt[i][:], gT[2048 + dc * 128:2048 + (dc + 1) * 128, tb * 512:(tb + 1) * 512],
                                       writes=[B_gt[i]])

                                def mm(pe, wi=wi, sub=sub, tb=tb, i=i):
                                    r = None
                                    for kc in range(8):
                                        pe.matmul(pa[i][:], lhsT=wua[wi][:, kc, sub * 128:(sub + 1) * 128],
                                                  rhs=yT[0][:, kc, tb * 512:(tb + 1) * 512], start=(kc == 0), stop=(kc == 7))
                                    for kc in range(8):
                                        r = pe.matmul(pbb[i][:], lhsT=wub[wi][:, kc, sub * 128:(sub + 1) * 128],
                                                      rhs=yT[1][:, kc, tb * 512:(tb + 1) * 512], start=(kc == 0), stop=(kc == 7))
                                    return r
                                kb.op("pe", mm, reads=[B_wu[wi], B_yT[0], B_yT[1]], writes=[B_pab[i]])
                                kb.op("dve", lambda v, i=i: v.tensor_tensor(out=m1[i][:], in0=pa[i][:], in1=gat[i][:], op=ALU.mult),
                                      reads=[B_pab[i], B_gt[i]], writes=[B_m[i]])
                                kb.op("dve", lambda v, i=i: v.tensor_tensor(out=m2[i][:], in0=pbb[i][:], in1=gbt[i][:], op=ALU.mult),
                                      reads=[B_pab[i], B_gt[i]], writes=[B_m[i]], waw=False)
                                kb.op("dve", lambda v, i=i, dc=dc, tb=tb: v.tensor_tensor(out=mT[:, dc, tb * 512:(tb + 1) * 512], in0=m1[i][:],
                                                                                          in1=m2[i][:], op=ALU.add),
                                      reads=[B_m[i]], writes=[B_mT], waw=False)
                    kb.barrier()

                phase(5)
                with contextlib.ExitStack() as s4b:
                    wo = [sb(s4b, f"wo{i}", [128, KC, 512], BF16) for i in range(2)]
                    B_wo = [Buf(), Buf()]
                    gtA = sb(s4b, "gtA", [128, D], F32)
                    B_gtA = Buf()
                    xp = [sb(s4b, f"xp{i}", [128, 512], F32) for i in range(3)]
                    B_xp = [Buf() for _ in range(3)]
                    hp = [sb(s4b, f"hp{i}", [128, 512], F32) for i in range(3)]
                    B_hp = [Buf() for _ in range(3)]
                    po = [ps(s4b, f"po{i}", [128, 512], F32) for i in range(3)]
                    B_po = [Buf() for _ in range(3)]
                    B_h1 = Buf("h1d")
                    kb.dma("sp", dq[8], gtA[:], modsave[0, :].partition_broadcast(128), writes=[B_gtA])
                    wov = w_o.rearrange("(kc p) n -> p kc n", p=128)
                    n = 0
                    for ob in range(4):
                        wi = ob % 2
                        kb.dma("pool", dp[wi], wo[wi][:, 0:8, :], wov[:, 0:8, ob * 512:(ob + 1) * 512], writes=[B_wo[wi]])
                        kb.dma("pool", dp[wi], wo[wi][:, 8:16, :], wov[:, 8:16, ob * 512:(ob + 1) * 512], writes=[B_wo[wi]])
                        for tt in range(16):
                            i = n % 3
                            n += 1
                            kb.dma("sp", dq[2 + i], xp[i][:], x[tt * 128:(tt + 1) * 128, ob * 512:(ob + 1) * 512], writes=[B_xp[i]])

                            def mm(pe, wi=wi, tt=tt, i=i):
                                r = None
                                for kc in range(KC):
                                    r = pe.matmul(po[i][:], lhsT=mT[:, kc, tt * 128:(tt + 1) * 128], rhs=wo[wi][:, kc, :],
                                                  start=(kc == 0), stop=(kc == KC - 1))
                                return r
                            kb.op("pe", mm, reads=[B_wo[wi], B_mT], writes=[B_po[i]])
                            kb.op("dve", lambda v, i=i, ob=ob: v.tensor_tensor(out=hp[i][:], in0=po[i][:], in1=gtA[:, ob * 512:(ob + 1) * 512],
                                                                               op=ALU.mult),
                                  reads=[B_po[i], B_gtA], writes=[B_hp[i]])
                            kb.op("dve", lambda v, i=i: v.tensor_tensor(out=hp[i][:], in0=hp[i][:], in1=xp[i][:], op=ALU.add),
                                  reads=[B_hp[i], B_xp[i]], writes=[B_hp[i]])
                            kb.dma("sp", dq[5 + i], h1d[tt * 128:(tt + 1) * 128, ob * 512:(ob + 1) * 512], hp[i][:],
                                   reads=[B_hp[i]], writes=[B_h1], waw=False)
                    kb.barrier()

        phase(6)
        with contextlib.ExitStack() as s5:
            h1t = [sb(s5, f"h1t{i}", [128, D], F32) for i in range(2)]
            B_h1t = [Buf(), Buf()]
            junk = sb(s5, "junk5", [128, D], BF16)
            ss = sb(s5, "ss5", [128, 1], F32)
            rstd = sb(s5, "rstd5", [128, 1], F32)
            xh = sb(s5, "xh5", [128, D], F32)
            p4 = ps(s5, "p45", [128, D], F32)
            nb = (junk, ss, rstd, xh, p4, Buf(), Buf(), Buf(), Buf())
            v16 = [sb(s5, f"v16{i}", [128, KC, 128], BF16) for i in range(2)]
            v32 = [sb(s5, f"v32{i}", [128, KC, 128], F32) for i in range(2)]
            B_v = [Buf(), Buf()]
            B_v16 = [Buf(), Buf()]
            wr = sb(s5, "wr", [128, KC, NE], F32)
            rb = sb(s5, "rb", [128, NE], F32)
            B_wr = Buf()
            plg_ = ps(s5, "plg", [128, 512], F32)
            plg = plg_[:, 0:NE]
            pwt_ = ps(s5, "pwt", [128, 512], F32)
            pwt = pwt_[0:NE, 0:128]
            B_plg, B_pwt = Buf(), Buf()
            sc = sb(s5, "sc", [128, NE], F32)
            bi = sb(s5, "bi", [128, NE], F32)
            t8 = sb(s5, "t8", [128, 8, 8], F32)
            gs = sb(s5, "gs", [128, 8], F32)
            g8 = sb(s5, "g8", [128, 8], F32)
            gm = sb(s5, "gm", [128, 8], F32)
            mb = sb(s5, "mb", [128, 8], F32)
            msk = sb(s5, "msk", [128, NE], F32)
            m8 = sb(s5, "m8", [128, 8], F32)
            sel = sb(s5, "sel", [128, NE], F32)
            den = sb(s5, "den", [128, 1], F32)
            wc = sb(s5, "wc", [128, NE], F32)
            wcs = [sb(s5, f"wcs{i}", [NE, 128], F32) for i in range(2)]
            B_wcs = [Buf(), Buf()]
            B_rt = Buf()
            B_VTd, B_wcTd = Buf(), Buf()
            kb.dma("sp", dq[8], wr[:], w_router.rearrange("(kc p) n -> p kc n", p=128), writes=[B_wr])
            kb.dma("sp", dq[8], rb[:], rbias[0, :].partition_broadcast(128), writes=[B_wr])
            for tt in range(16):
                i = tt % 2
                kb.dma("sp", dq[i], h1t[i][:], h1d[tt * 128:(tt + 1) * 128, :], writes=[B_h1t[i]])
                norm_tile(nb, h1t[i], B_h1t[i], tt, A_m, S_m,
                          lambda kc, i=i: (v16[i][:] if kc is None else v16[i][:, kc, :]), B_v[i],
                          dst32=lambda kc, i=i: (v32[i][:] if kc is None else v32[i][:, kc, :]), B_dst16=B_v16[i])
                kb.dma("sp", dq[2 + i], VTd[:, :, tt * 128:(tt + 1) * 128].rearrange("k p t -> p k t"), v16[i][:],
                       reads=[B_v16[i]], writes=[B_VTd], waw=False)

                def mm(pe, i=i):
                    r = None
                    for kc in range(KC):
                        r = pe.matmul(plg[:], lhsT=v32[i][:, kc, :], rhs=wr[:, kc, :], start=(kc == 0), stop=(kc == KC - 1))
                    return r
                kb.op("pe", mm, reads=[B_v[i], B_wr], writes=[B_plg])
                R = [B_rt]
                kb.op("act", lambda a: a.activation(out=sc[:], in_=plg[:], func=AF.Sigmoid), reads=[B_plg], writes=R)
                kb.op("dve", lambda v: v.tensor_tensor(out=bi[:], in0=sc[:], in1=rb[:], op=ALU.add), reads=R + [B_wr], writes=R)
                for g in range(8):
                    kb.op("dve", lambda v, g=g: v.max(out=t8[:, g, :], in_=bi[:, g * 8:(g + 1) * 8]), reads=R, writes=R)
                kb.op("dve", lambda v: v.tensor_tensor(out=gs[:], in0=t8[:, :, 0], in1=t8[:, :, 1], op=ALU.add), reads=R, writes=R)
                kb.op("dve", lambda v: v.max(out=g8[:], in_=gs[:]), reads=R, writes=R)
                kb.op("dve", lambda v: v.tensor_scalar(out=gm[:], in0=gs[:], scalar1=g8[:, 3:4], scalar2=None, op0=ALU.is_ge),
                      reads=R, writes=R)
                kb.op("dve", lambda v: v.tensor_scalar(out=mb[:], in0=gm[:], scalar1=-1.0, scalar2=1e9, op0=ALU.add, op1=ALU.mult),
                      reads=R, writes=R)
                for g in range(8):
                    kb.op("dve", lambda v, g=g: v.tensor_scalar(out=msk[:, g * 8:(g + 1) * 8], in0=bi[:, g * 8:(g + 1) * 8],
                                                               scalar1=gm[:, g:g + 1], scalar2=mb[:, g:g + 1], op0=ALU.mult, op1=ALU.add),
                          reads=R, writes=R)
                kb.op("dve", lambda v: v.max(out=m8[:], in_=msk[:]), reads=R, writes=R)
                kb.op("dve", lambda v: v.tensor_scalar(out=sel[:], in0=msk[:], scalar1=m8[:, 7:8], scalar2=None, op0=ALU.is_ge),
                      reads=R, writes=R)
                kb.op("dve", lambda v: v.tensor_tensor(out=sel[:], in0=sel[:], in1=sc[:], op=ALU.mult), reads=R, writes=R)
                kb.op("dve", lambda v: v.reduce_sum(out=den[:], in_=sel[:], axis=AX.X), reads=R, writes=R)
                kb.op("dve", lambda v: v.reciprocal(out=den[:], in_=den[:]), reads=R, writes=R)
                kb.op("dve", lambda v: v.tensor_scalar(out=wc[:], in0=sel[:], scalar1=den[:, 0:1], scalar2=2.5, op0=ALU.mult, op1=ALU.mult),
                      reads=R, writes=R)
                kb.op("pe", lambda pe: pe.transpose(out=pwt[:], in_=wc[:], identity=identf[:]), reads=R + [B_const], writes=[B_pwt])
                kb.op("act", lambda a, i=i: a.copy(out=wcs[i][:], in_=pwt[:]), reads=[B_pwt], writes=[B_wcs[i]])
                kb.dma("sp", dq[4 + i], wcTd[:, tt * 128:(tt + 1) * 128], wcs[i][:], reads=[B_wcs[i]], writes=[B_wcTd], waw=False)
            kb.barrier()

        phase(7)
        for pz in range(2):
            t0 = pz * 1024
            with contextlib.ExitStack() as s6:
                acc = sb(s6, "acc", [128, 8, D], F32)
                B_acc = [Buf() for _ in range(8)]
                with contextlib.ExitStack() as s6a:
                    vt = sb(s6a, "vt", [128, KC, 1024], BF16)
                    B_vt = Buf()
                    wct = sb(s6a, "wct", [NE, 1024], F32)
                    B_wct = Buf()
                    gu = [sb(s6a, f"gu{i}", [128, 2, KC, 128], BF16) for i in range(3)]
                    B_gu = [Buf() for _ in range(3)]
                    dd = sb(s6a, "dd", [128, 4, D], BF16)
                    B_dd = Buf()
                    hT = sb(s6a, "hT", [128, 4, 1024], BF16)
                    B_hT = Buf()
                    sg = [sb(s6a, f"sg{i}", [128, 512], F32) for i in range(2)]
                    sgw = [sb(s6a, f"sgw{i}", [128, 512], F32) for i in range(2)]
                    B_sg = [Buf(), Buf()]
                    B_sgw = [Buf(), Buf()]
                    wbc = sb(s6a, "wbc", [128, 1024], F32)
                    B_wbc = Buf()
                    sl = [sb(s6a, f"sl{i}", [NE, 128], F32) for i in range(2)]
                    B_sl = [Buf(), Buf()]
                    pg = [ps(s6a, f"pg{i}", [128, 512], F32) for i in range(2)]
                    pu = [ps(s6a, f"pu{i}", [128, 512], F32) for i in range(2)]
                    B_pgu = [Buf(), Buf()]
                    py = [ps(s6a, f"py{i}", [128, 512], F32) for i in range(3)]
                    B_py = [Buf() for _ in range(3)]
                    pw = ps(s6a, "pw", [128, 512], F32)
                    B_pw = Buf()

                    for kc4 in range(4):
                        kb.dma("sp", dq[8], vt[:, kc4 * 4:(kc4 + 1) * 4, :],
                               VTd[kc4 * 4:(kc4 + 1) * 4, :, t0:t0 + 1024].rearrange("k p t -> p k t"), writes=[B_vt])
                    kb.dma("sp", dq[9], wct[:], wcTd[:, t0:t0 + 1024], writes=[B_wct])

                    NU = (NE + 1) * 4

                    def load_gu(u):
                        e, c = divmod(u, 4)
                        bi_ = u % 3
                        kb.dma("pool", dp[bi_], gu[bi_][:].rearrange("p g k m -> p g (k m)"),
                               wgu[e, c].rearrange("p (g j) -> p g j", g=2), writes=[B_gu[bi_]])

                    def load_d(e):
                        for c in range(4):
                            kb.dma("pool", dp[3], dd[:, c, :], wd[e, c * 128:(c + 1) * 128, :], writes=[B_dd])

                    load_gu(0)
                    load_gu(1)
                    load_d(0)
                    ny = 0
                    for e in range(NE + 1):
                        if e < NE:
                            si = e % 2
                            kb.op("dve", lambda v, si=si, e=e: v.tensor_scalar(out=sl[si][:], in0=ones_f[0:NE, :], scalar1=identf[0:NE, e:e + 1],
                                                                               scalar2=None, op0=ALU.mult),
                                  reads=[B_const], writes=[B_sl[si]])
                            for j in range(2):
                                kb.op("pe", lambda pe, si=si, j=j: pe.matmul(pw[:], lhsT=sl[si][:], rhs=wct[:, j * 512:(j + 1) * 512],
                                                                             start=True, stop=True),
                                      reads=[B_sl[si], B_wct], writes=[B_pw])
                                kb.op("act", lambda a, j=j: a.copy(out=wbc[:, j * 512:(j + 1) * 512], in_=pw[:]),
                                      reads=[B_pw], writes=[B_wbc], waw=(j == 0))
                        else:
                            kb.op("dve", lambda v: v.memset(wbc[:], 1.0), writes=[B_wbc])
                        for c in range(4):
                            u = e * 4 + c
                            if u + 2 < NU:
                                load_gu(u + 2)
                            bi_ = u % 3
                            for tb in range(2):
                                i = (u * 2 + tb) % 2

                                def mm(pe, bi_=bi_, tb=tb, i=i):
                                    r = None
                                    for kc in range(KC):
                                        pe.matmul(pg[i][:], lhsT=gu[bi_][:, 0, kc, :], rhs=vt[:, kc, tb * 512:(tb + 1) * 512],
                                                  start=(kc == 0), stop=(kc == KC - 1))
                                    for kc in range(KC):
                                        r = pe.matmul(pu[i][:], lhsT=gu[bi_][:, 1, kc, :], rhs=vt[:, kc, tb * 512:(tb + 1) * 512],
                                                      start=(kc == 0), stop=(kc == KC - 1))
                                    return r
                                kb.op("pe", mm, reads=[B_gu[bi_], B_vt], writes=[B_pgu[i]])
                                kb.op("act", lambda a, i=i: a.activation(out=sg[i][:], in_=pg[i][:], func=AF.Silu),
                                      reads=[B_pgu[i]], writes=[B_sg[i]])
                                kb.op("dve", lambda v, i=i, tb=tb: v.tensor_tensor(out=sgw[i][:], in0=sg[i][:], in1=wbc[:, tb * 512:(tb + 1) * 512],
                                                                                   op=ALU.mult),
                                      reads=[B_sg[i], B_wbc], writes=[B_sgw[i]])
                                kb.op("dve", lambda v, i=i, c=c, tb=tb: v.tensor_tensor(out=hT[:, c, tb * 512:(tb + 1) * 512], in0=pu[i][:],
                                                                                        in1=sgw[i][:], op=ALU.mult),
                                      reads=[B_pgu[i], B_sgw[i]], writes=[B_hT], waw=False)
                        for tt in range(8):
                            for ob in range(4):
                                yi = ny % 3
                                ny += 1

                                def mmd(pe, tt=tt, ob=ob, yi=yi):
                                    r = None
                                    for c in range(4):
                                        r = pe.matmul(py[yi][:], lhsT=hT[:, c, tt * 128:(tt + 1) * 128], rhs=dd[:, c, ob * 512:(ob + 1) * 512],
                                                      start=(c == 0), stop=(c == 3))
                                    return r
                                kb.op("pe", mmd, reads=[B_hT, B_dd], writes=[B_py[yi]])
                                if e == 0:
                                    kb.op("dve", lambda v, tt=tt, ob=ob, yi=yi: v.tensor_copy(out=acc[:, tt, ob * 512:(ob + 1) * 512], in_=py[yi][:]),
                                          reads=[B_py[yi]], writes=[B_acc[tt]], waw=False)
                                else:
                                    kb.op("dve", lambda v, tt=tt, ob=ob, yi=yi: v.tensor_tensor(out=acc[:, tt, ob * 512:(ob + 1) * 512],
                                                                                                in0=py[yi][:], in1=acc[:, tt, ob * 512:(ob + 1) * 512],
                                                                                                op=ALU.add),
                                          reads=[B_py[yi], B_acc[tt]], writes=[B_acc[tt]], waw=False)
                        if e + 1 <= NE:
                            load_d(e + 1)
                    kb.barrier()

                with contextlib.ExitStack() as s6b:
                    gtM = sb(s6b, "gtM", [128, D], F32)
                    gF = sb(s6b, "gF", [128, D], F32)
                    B_gc = Buf()
                    h1f = [sb(s6b, f"h1f{i}", [128, D], F32) for i in range(2)]
                    B_h1f = [Buf(), Buf()]
                    ot = [sb(s6b, f"ot{i}", [128, D], F32) for i in range(2)]
                    B_ot = [Buf(), Buf()]
                    jk = sb(s6b, "jk6", [128, D], BF16)
                    fs = [sb(s6b, f"fs{i}", [128, 1], F32) for i in range(2)]
                    B_fs = [Buf(), Buf()]
                    B_jk = Buf()
                    B_out = Buf()
                    kb.dma("sp", dq[8], gtM[:], modsave[1, :].partition_broadcast(128), writes=[B_gc])
                    kb.dma("sp", dq[8], gF[:], g_final[0, :].partition_broadcast(128), writes=[B_gc])
                    for tt in range(8):
                        i = tt % 2
                        r0 = t0 + tt * 128
                        kb.dma("sp", dq[i], h1f[i][:], h1d[r0:r0 + 128, :], writes=[B_h1f[i]])
                        kb.op("dve", lambda v, tt=tt: v.tensor_tensor(out=acc[:, tt, :], in0=acc[:, tt, :], in1=gtM[:], op=ALU.mult),
                              reads=[B_acc[tt], B_gc], writes=[B_acc[tt]])
                        kb.op("dve", lambda v, tt=tt, i=i: v.tensor_tensor(out=h1f[i][:], in0=h1f[i][:], in1=acc[:, tt, :], op=ALU.add),
                              reads=[B_acc[tt], B_h1f[i]], writes=[B_h1f[i]])
                        kb.op("act", lambda a, i=i: a.activation(out=jk[:], in_=h1f[i][:], func=AF.Square, accum_out=fs[i][:]),
                              reads=[B_h1f[i]], writes=[B_jk, B_fs[i]])
                        kb.op("act", lambda a, i=i: a.activation(out=fs[i][:], in_=fs[i][:], func=AF.Sqrt, bias=1e-6, scale=1.0 / D),
                              reads=[B_fs[i]], writes=[B_fs[i]])
                        kb.op("dve", lambda v, i=i: v.reciprocal(out=fs[i][:], in_=fs[i][:]), reads=[B_fs[i]], writes=[B_fs[i]])
                        kb.op("dve", lambda v, i=i: v.scalar_tensor_tensor(out=ot[i][:], in0=h1f[i][:], scalar=fs[i][:, 0:1], in1=gF[:],
                                                                           op0=ALU.mult, op1=ALU.mult),
                              reads=[B_h1f[i], B_fs[i], B_gc], writes=[B_ot[i]])
                        kb.dma("sp", dq[2 + i], out[r0:r0 + 128, :], ot[i][:], reads=[B_ot[i]], writes=[B_out], waw=False)
                    kb.barrier()
    return nc


_NC = None


def t5_bucket(rel):
    nb = 16
    ret = (rel > 0).astype(np.int32) * nb
    n = np.abs(rel)
    max_exact = nb // 2
    large = max_exact + (np.log(np.maximum(n, 1) / max_exact) / math.log(128 / max_exact) * (nb - max_exact)).astype(np.int32)
    large = np.minimum(large, nb - 1)
    return (ret + np.where(n < max_exact, n, large)).astype(np.int32)


def kernel(x, c, w_ada, b_ada, g_attn, w_in, lambda_qk, t5_bias, rel_bias_b, w_up_a, w_up_b, w_o,
           g_moe, w_router, router_bias, w_exp_gate, w_exp_up, w_exp_down,
           w_sh_gate, w_sh_up, w_sh_down, g_final):
    global _NC
    f = lambda a: np.ascontiguousarray(np.asarray(a, dtype=np.float32))
    x = f(x); c = f(c)
    gfm = np.concatenate([f(g_attn)[0].reshape(16, 128).T, f(g_moe)[0].reshape(16, 128).T], axis=1)
    qpos = np.arange(1920, 2048)
    kpos = np.arange(2048)
    idx = t5_bucket(kpos[None, :] - qpos[:, None])
    allowed = (kpos[None, :] // 64) <= (qpos[:, None] // 64)
    t5 = f(t5_bias)
    biasA = np.stack([np.where(allowed, t5[idx, h], np.float32(NEG)) for h in range(8)]).astype(np.float32)
    q2 = np.arange(128)
    k2 = np.arange(640) - 512
    rel = np.clip(k2[None, :] - q2[:, None], -256, 256) + 256
    cq = q2 // 64
    ok = (k2[None, :] >= (cq[:, None] * 64 - 512)) & (k2[None, :] < (cq[:, None] * 64 + 64))
    rb_ = f(rel_bias_b)[0]
    biasB = np.stack([np.where(ok, rb_[rel, h], np.float32(NEG)) for h in range(8)]).astype(np.float32)

    def lay(w):
        return w.reshape(16, 128, 4, 128).transpose(2, 1, 0, 3).reshape(4, 128, 2048)
    wg = f(w_exp_gate)[0]; wu = f(w_exp_up)[0]
    wgu = np.empty((NE + 1, 4, 128, 4096), np.float32)
    for e in range(NE):
        wgu[e, :, :, 0:2048] = lay(wg[e])
        wgu[e, :, :, 2048:4096] = lay(wu[e])
    wgu[NE, :, :, 0:2048] = lay(f(w_sh_gate)[0])
    wgu[NE, :, :, 2048:4096] = lay(f(w_sh_up)[0])
    wdn = np.concatenate([f(w_exp_down)[0], f(w_sh_down)], axis=0)

    shared = {
        "w_ada": f(w_ada)[0], "b_ada": f(b_ada), "gfm": np.ascontiguousarray(gfm), "g_final": f(g_final).reshape(1, D),
        "w_in": f(w_in)[0], "lam": f(lambda_qk).reshape(1, 256), "biasA": biasA, "biasB": biasB,
        "w_up_a": f(w_up_a)[0], "w_up_b": f(w_up_b)[0], "w_o": f(w_o)[0], "w_router": f(w_router)[0],
        "rbias": f(router_bias), "wgu": wgu, "wd": np.ascontiguousarray(wdn),
    }
    in_maps = []
    for b in range(8):
        m = dict(shared)
        m["x"] = x[b]
        m["cT"] = np.ascontiguousarray(c[b].reshape(16, 128).T)
        in_maps.append(m)
    if _NC is None:
        _NC = build()
    res = run_bass_kernel_spmd(_NC, in_maps, core_ids=list(range(8)))
    return np.stack([np.asarray(r["out"], dtype=np.float32) for r in res.results], axis=0)
```

```python
import contextlib
import os
import math
import numpy as np
import concourse.bass as bass
import concourse.mybir as mybir
from concourse.bass_utils import run_bass_kernel_spmd

F32 = mybir.dt.float32
BF16 = mybir.dt.bfloat16
AF = mybir.ActivationFunctionType
ALU = mybir.AluOpType
AX = mybir.AxisListType

T = 2048
D = 2048
KC = 16
NE = 64
NEG = -1e30


class _Stop(Exception):
    pass


class Tk:
    __slots__ = ("sem", "val", "eng")

    def __init__(self, sem, val, eng):
        self.sem = sem
        self.val = val
        self.eng = eng


class Buf:
    def __init__(self, name=""):
        self.name = name
        self.w = {}
        self.r = {}


class DSem:
    def __init__(self, h):
        self.h = h
        self.cnt = 0


class KB:
    def __init__(self, nc, es):
        self.nc = nc
        self.es = es
        self.E = {"pe": nc.tensor, "act": nc.scalar, "dve": nc.vector, "pool": nc.gpsimd, "sp": nc.sync}
        self.psem = {e: es.enter_context(nc.semaphore("prog_" + e)) for e in ("pe", "act", "dve", "pool")}
        self.pcnt = {e: 0 for e in self.psem}
        self.waited = {e: {} for e in self.E}
        self.dsems = []

    def dsem(self, name):
        d = DSem(self.es.enter_context(self.nc.semaphore(name)))
        self.dsems.append(d)
        return d

    def wait(self, e, tk):
        if tk is None:
            return
        if tk.eng == "pe" and e == "pe":
            return
        k = id(tk.sem)
        if self.waited[e].get(k, 0) >= tk.val:
            return
        self.E[e].wait_ge(tk.sem, tk.val)
        self.waited[e][k] = tk.val

    def _deps(self, e, reads, writes, waw, skipsem=None):
        for b in reads:
            for t in b.w.values():
                self.wait(e, t)
        for b in writes:
            if waw:
                for t in b.w.values():
                    if skipsem is not None and t.sem is skipsem:
                        continue
                    self.wait(e, t)
            for t in b.r.values():
                self.wait(e, t)

    def _commit(self, tk, reads, writes, waw):
        k = id(tk.sem)
        for b in reads:
            o = b.r.get(k)
            if o is None or o.val < tk.val:
                b.r[k] = tk
        for b in writes:
            o = b.w.get(k)
            if o is None or o.val < tk.val:
                b.w[k] = tk

    def op(self, e, fn, reads=(), writes=(), waw=True):
        self._deps(e, reads, writes, waw)
        ins = fn(self.E[e])
        self.pcnt[e] += 1
        ins.then_inc(self.psem[e], 1)
        tk = Tk(self.psem[e], self.pcnt[e], e)
        self._commit(tk, reads, writes, waw)
        return tk

    def dma(self, q, ds, out, in_, reads=(), writes=(), waw=True):
        self._deps(q, reads, writes, waw, skipsem=ds.h)
        ins = self.E[q].dma_start(out=out, in_=in_)
        ds.cnt += 16
        ins.then_inc(ds.h, 16)
        tk = Tk(ds.h, ds.cnt, "dma")
        self._commit(tk, reads, writes, waw)
        return tk

    def barrier(self):
        for e in self.E:
            for p in self.psem:
                if self.pcnt[p] > 0:
                    self.wait(e, Tk(self.psem[p], self.pcnt[p], "x"))
            for d in self.dsems:
                if d.cnt > 0:
                    self.wait(e, Tk(d.h, d.cnt, "dma"))


def build(upto=9, dbg=False):
    nc = bass.Bass("TRN2", target_bir_lowering=False)

    def din(name, shape, dt=F32):
        return nc.dram_tensor(name, shape, dt, kind="ExternalInput").ap()

    def dscr(name, shape, dt):
        return nc.dram_tensor(name, shape, dt, kind=("ExternalOutput" if dbg else "Internal")).ap()

    x = din("x", [T, D])
    cT = din("cT", [128, 16])
    w_ada = din("w_ada", [D, 6 * D])
    b_ada = din("b_ada", [1, 6 * D])
    gfm = din("gfm", [128, 32])
    g_final = din("g_final", [1, D])
    w_in = din("w_in", [D, 10240])
    lam_in = din("lam", [1, 256])
    biasAT = din("biasAT", [8, 128, 5 * 512])
    biasBT = din("biasBT", [8, 128, 8 * 512])
    cvecA = din("cvecA", [128, 8])
    w_up_a = din("w_up_a", [1024, D])
    w_up_b = din("w_up_b", [1024, D])
    w_o = din("w_o", [D, D])
    w_router = din("w_router", [D, NE])
    rbias = din("rbias", [1, NE])
    if upto >= 7:
        wgu = din("wgu", [NE + 1, 4, 128, 4096])
        wd = din("wd", [NE + 1, 512, D])
    out = nc.dram_tensor("out", [T, D], F32, kind="ExternalOutput").ap()

    qkT = [dscr(f"qkT{i}", [1024, T], BF16) for i in range(4)]
    vv = [dscr(f"vv{i}", [T, 1024], BF16) for i in range(2)]
    gT = dscr("gT", [4096, T], BF16)
    h1d = dscr("h1d", [T, D], F32)
    VTd = dscr("VTd", [KC, 128, T], BF16)
    wcTd = dscr("wcTd", [NE, T], F32)
    modsave = dscr("modsave", [2, D], F32)

    with contextlib.suppress(_Stop), contextlib.ExitStack() as es:
        kb = KB(nc, es)

        def phase(n):
            if n > upto:
                kb.barrier()
                raise _Stop()

        uniq = [0]

        def sb(stack, name, shape, dt):
            uniq[0] += 1
            return stack.enter_context(nc.sbuf_tensor(f"{name}_{uniq[0]}", shape, dt))

        def ps(stack, name, shape, dt):
            uniq[0] += 1
            return stack.enter_context(nc.psum_tensor(f"{name}_{uniq[0]}", shape, dt))

        identf = sb(es, "identf", [128, 128], F32)
        identb = sb(es, "identb", [128, 128], BF16)
        ones_f = sb(es, "ones_f", [128, 128], F32)
        B_const = Buf("const")
        kb.op("pool", lambda g: g.memset(identf[:], 1.0), writes=[B_const])
        kb.op("pool", lambda g: g.affine_select(out=identf[:], in_=identf[:], pattern=[[-1, 128]],
                                                compare_op=ALU.is_equal, fill=0.0, base=0,
                                                channel_multiplier=1), reads=[B_const], writes=[B_const])
        kb.op("dve", lambda v: v.tensor_copy(out=identb[:], in_=identf[:]), reads=[B_const], writes=[B_const], waw=False)
        kb.op("dve", lambda v: v.memset(ones_f[:], 1.0), writes=[B_const], waw=False)

        A_a = sb(es, "A_a", [128, 16], F32)
        S_a = sb(es, "S_a", [128, 16], F32)
        A_m = sb(es, "A_m", [128, 16], F32)
        S_m = sb(es, "S_m", [128, 16], F32)
        B_mod = Buf("modfm")

        dq = [kb.dsem(f"dq{i}") for i in range(12)]
        dp = [kb.dsem(f"dp{i}") for i in range(4)]

        with contextlib.ExitStack() as s0:
            cts = sb(s0, "cts", [128, 16], F32)
            scs = sb(s0, "scs", [128, 16], F32)
            gfs = sb(s0, "gfs", [128, 32], F32)
            scb = sb(s0, "scb", [128, 16, 128], F32)
            wab = [sb(s0, f"wab{i}", [128, 16, 512], F32) for i in range(2)]
            bbc = [sb(s0, f"bbc{i}", [128, 512], F32) for i in range(2)]
            modbc = [sb(s0, f"modbc{i}", [128, D], F32) for i in range(6)]
            tmpd = sb(s0, "tmpd", [128, 128], F32)
            fm = sb(s0, "fm", [128, 4, 16], F32)
            pmod = [ps(s0, f"pmod{i}", [128, 512], F32) for i in range(2)]
            B_c = Buf()
            B_scb = Buf()
            B_wab = [Buf(), Buf()]
            B_bbc = [Buf(), Buf()]
            B_pm = [Buf(), Buf()]
            B_modbc = [Buf() for _ in range(6)]
            B_tmp = Buf()
            B_fm = Buf()
            kb.dma("sp", dq[0], cts[:], cT[:, :], writes=[B_c])
            kb.dma("sp", dq[0], gfs[:], gfm[:, :], writes=[B_c])
            kb.op("act", lambda a: a.activation(out=scs[:], in_=cts[:], func=AF.Silu), reads=[B_c], writes=[B_scb])
            for kc in range(KC):
                kb.op("dve", lambda v, kc=kc: v.tensor_scalar(out=scb[:, kc, :], in0=ones_f[:], scalar1=scs[:, kc:kc + 1],
                                                             scalar2=None, op0=ALU.mult),
                      reads=[B_scb, B_const], writes=[B_c], waw=False)
            wav = w_ada.rearrange("(kc p) n -> p kc n", p=128)
            for j in range(24):
                i = j % 2
                kb.dma("sp", dq[1 + i], wab[i][:, 0:8, :], wav[:, 0:8, j * 512:(j + 1) * 512], writes=[B_wab[i]])
                kb.dma("act", dq[1 + i], wab[i][:, 8:16, :], wav[:, 8:16, j * 512:(j + 1) * 512], writes=[B_wab[i]])
                kb.dma("sp", dq[3 + i], bbc[i][:], b_ada[0, j * 512:(j + 1) * 512].partition_broadcast(128),
                       writes=[B_bbc[i]])

                def mm(pe, i=i):
                    r = None
                    for kc in range(KC):
                        r = pe.matmul(pmod[i][:], lhsT=scb[:, kc, :], rhs=wab[i][:, kc, :], start=(kc == 0), stop=(kc == KC - 1))
                    return r
                kb.op("pe", mm, reads=[B_c, B_wab[i]], writes=[B_pm[i]])
                mi, cb = j // 4, (j % 4) * 512
                kb.op("dve", lambda v, i=i, mi=mi, cb=cb: v.tensor_tensor(out=modbc[mi][:, cb:cb + 512], in0=pmod[i][:],
                                                                           in1=bbc[i][:], op=ALU.add),
                      reads=[B_pm[i], B_bbc[i]], writes=[B_modbc[mi]], waw=False)
            for fi, mi in enumerate((0, 1, 3, 4)):
                for kc in range(KC):
                    kb.op("dve", lambda v, mi=mi, kc=kc: v.tensor_tensor(out=tmpd[:], in0=modbc[mi][:, kc * 128:(kc + 1) * 128],
                                                                         in1=identf[:], op=ALU.mult),
                          reads=[B_modbc[mi], B_const], writes=[B_tmp])
                    kb.op("dve", lambda v, fi=fi, kc=kc: v.reduce_sum(out=fm[:, fi, kc:kc + 1], in_=tmpd[:], axis=AX.X),
                          reads=[B_tmp], writes=[B_fm], waw=False)
            kb.op("dve", lambda v: v.tensor_copy(out=S_a[:], in_=fm[:, 0, :]), reads=[B_fm], writes=[B_mod], waw=False)
            kb.op("dve", lambda v: v.tensor_copy(out=S_m[:], in_=fm[:, 2, :]), reads=[B_fm], writes=[B_mod], waw=False)
            kb.op("dve", lambda v: v.scalar_tensor_tensor(out=A_a[:], in0=fm[:, 1, :], scalar=1.0, in1=gfs[:, 0:16],
                                                          op0=ALU.add, op1=ALU.mult), reads=[B_fm, B_c], writes=[B_mod], waw=False)
            kb.op("dve", lambda v: v.scalar_tensor_tensor(out=A_m[:], in0=fm[:, 3, :], scalar=1.0, in1=gfs[:, 16:32],
                                                          op0=ALU.add, op1=ALU.mult), reads=[B_fm, B_c], writes=[B_mod], waw=False)
            B_ms = Buf()
            kb.dma("sp", dq[5], modsave[0:1, :], modbc[2][0:1, :], reads=[B_modbc[2]], writes=[B_ms], waw=False)
            kb.dma("sp", dq[5], modsave[1:2, :], modbc[5][0:1, :], reads=[B_modbc[5]], writes=[B_ms], waw=False)
            kb.barrier()

        def norm_tile(stack_bufs, src_tile, B_src, tt, A_fm, S_fm, dst_bf, B_dst, dst32=None, B_dst16=None):
            junk, ss, rstd, xh, p4, B_junk, B_ss, B_xh, B_p4 = stack_bufs
            kb.op("act", lambda a: a.activation(out=junk[:], in_=src_tile[:], func=AF.Square, accum_out=ss[:]),
                  reads=[B_src], writes=[B_junk, B_ss])
            kb.op("act", lambda a: a.activation(out=rstd[:], in_=ss[:], func=AF.Sqrt, bias=1e-6, scale=1.0 / D),
                  reads=[B_ss], writes=[B_ss])
            kb.op("dve", lambda v: v.reciprocal(out=rstd[:], in_=rstd[:]), reads=[B_ss], writes=[B_ss])
            kb.op("dve", lambda v: v.tensor_scalar(out=xh[:], in0=src_tile[:], scalar1=rstd[:, 0:1], scalar2=None, op0=ALU.mult),
                  reads=[B_src, B_ss], writes=[B_xh])

            def tr(pe):
                r = None
                for kc in range(KC):
                    r = pe.transpose(out=p4[:, kc * 128:(kc + 1) * 128], in_=xh[:, kc * 128:(kc + 1) * 128], identity=identf[:])
                return r
            kb.op("pe", tr, reads=[B_xh, B_const], writes=[B_p4])
            for kc in range(KC):
                o = dst32(kc) if dst32 is not None else dst_bf(kc)
                if kc < 8:
                    kb.op("act", lambda a, kc=kc, o=o: a.activation(out=o, in_=p4[:, kc * 128:(kc + 1) * 128], func=AF.Identity,
                                                                    bias=S_fm[:, kc:kc + 1], scale=A_fm[:, kc:kc + 1]),
                          reads=[B_p4, B_mod], writes=[B_dst], waw=False)
                else:
                    kb.op("dve", lambda v, kc=kc, o=o: v.tensor_scalar(out=o, in0=p4[:, kc * 128:(kc + 1) * 128],
                                                                       scalar1=A_fm[:, kc:kc + 1], scalar2=S_fm[:, kc:kc + 1],
                                                                       op0=ALU.mult, op1=ALU.add),
                          reads=[B_p4, B_mod], writes=[B_dst], waw=False)
            if dst32 is not None:
                kb.op("pool", lambda g: g.tensor_copy(out=dst_bf(None), in_=dst32(None)), reads=[B_dst], writes=[B_dst16])

        with contextlib.ExitStack() as s1:
            uT = sb(s1, "uT", [128, KC, T], BF16)
            B_uT = Buf("uT")
            phase(1)
            with contextlib.ExitStack() as s1a:
                xt = [sb(s1a, f"xt{i}", [128, D], F32) for i in range(2)]
                junk = sb(s1a, "junk", [128, D], BF16)
                ss = sb(s1a, "ss", [128, 1], F32)
                rstd = sb(s1a, "rstd", [128, 1], F32)
                xh = sb(s1a, "xh", [128, D], F32)
                p4 = ps(s1a, "p4", [128, D], F32)
                B_xt = [Buf(), Buf()]
                nb = (junk, ss, rstd, xh, p4, Buf(), Buf(), Buf(), Buf())
                for tt in range(int(os.environ.get('NT1', '16'))):
                    i = tt % 2
                    kb.dma("sp", dq[i], xt[i][:], x[tt * 128:(tt + 1) * 128, :], writes=[B_xt[i]])
                    norm_tile(nb, xt[i], B_xt[i], tt, A_a, S_a,
                              lambda kc, tt=tt: uT[:, kc, tt * 128:(tt + 1) * 128], B_uT)
                kb.barrier()

            phase(2)
            with contextlib.ExitStack() as s2:
                wb = [sb(s2, f"wb{i}", [128, KC, 512], BF16) for i in range(2)]
                B_wb = [Buf(), Buf()]
                stg = [sb(s2, f"stg{i}", [128, 512], BF16) for i in range(4)]
                B_stg = [Buf() for _ in range(4)]
                pb = [ps(s2, f"pb{i}", [128, 512], F32) for i in range(4)]
                B_pb = [Buf() for _ in range(4)]
                B_scr = Buf("scratchA2")
                wiv = w_in.rearrange("(kc p) n -> p kc n", p=128)
                cnt = [0]

                def evac_store(pi, kind, scale, dst_ap):
                    n = cnt[0]
                    cnt[0] += 1
                    si = n % 4
                    if kind == "sig":
                        kb.op("act", lambda a: a.activation(out=stg[si][:], in_=pb[pi][:], func=AF.Sigmoid),
                              reads=[B_pb[pi]], writes=[B_stg[si]])
                    elif n % 2 == 0:
                        kb.op("act", lambda a: a.activation(out=stg[si][:], in_=pb[pi][:], func=AF.Identity, scale=float(scale)),
                              reads=[B_pb[pi]], writes=[B_stg[si]])
                    else:
                        kb.op("dve", lambda v: v.tensor_scalar(out=stg[si][:], in0=pb[pi][:], scalar1=float(scale), scalar2=None,
                                                               op0=ALU.mult),
                              reads=[B_pb[pi]], writes=[B_stg[si]])
                    kb.dma("sp", dq[4 + si], dst_ap, stg[si][:], reads=[B_stg[si]], writes=[B_scr], waw=False)

                groups = []
                for g in range(20):
                    if g < 2:
                        groups.append(("fm", qkT[0], g * 512, 0.125, "lin"))
                    elif g < 4:
                        groups.append(("fm", qkT[1], (g - 2) * 512, 1.0, "lin"))
                    elif g < 6:
                        groups.append(("tm", vv[0], (g - 4) * 512, 1.0, "lin"))
                    elif g < 8:
                        groups.append(("fm", qkT[2], (g - 6) * 512, 128.0 ** -0.5, "lin"))
                    elif g < 10:
                        groups.append(("fm", qkT[3], (g - 8) * 512, 1.0, "lin"))
                    elif g < 12:
                        groups.append(("tm", vv[1], (g - 10) * 512, 1.0, "lin"))
                    else:
                        groups.append(("fm", gT, (g - 12) * 512, 1.0, "sig"))
                pcount = 0
                for g in range(20):
                    i = g % 2
                    kind, dst, base, scale, ev = groups[g]
                    kb.dma("pool", dp[i], wb[i][:, 0:8, :], wiv[:, 0:8, g * 512:(g + 1) * 512], writes=[B_wb[i]])
                    kb.dma("pool", dp[i], wb[i][:, 8:16, :], wiv[:, 8:16, g * 512:(g + 1) * 512], writes=[B_wb[i]])
                    if kind == "fm":
                        for m in range(4):
                            for tb in range(4):
                                pi = pcount % 4
                                pcount += 1

                                def mm(pe, i=i, m=m, tb=tb, pi=pi):
                                    r = None
                                    for kc in range(KC):
                                        r = pe.matmul(pb[pi][:], lhsT=wb[i][:, kc, m * 128:(m + 1) * 128],
                                                      rhs=uT[:, kc, tb * 512:(tb + 1) * 512], start=(kc == 0), stop=(kc == KC - 1))
                                    return r
                                kb.op("pe", mm, reads=[B_wb[i], B_uT], writes=[B_pb[pi]])
                                evac_store(pi, ev, scale, dst[base + m * 128: base + (m + 1) * 128, tb * 512:(tb + 1) * 512])
                    else:
                        for tt in range(16):
                            pi = pcount % 4
                            pcount += 1

                            def mm(pe, i=i, tt=tt, pi=pi):
                                r = None
                                for kc in range(KC):
                                    r = pe.matmul(pb[pi][:], lhsT=uT[:, kc, tt * 128:(tt + 1) * 128], rhs=wb[i][:, kc, :],
                                                  start=(kc == 0), stop=(kc == KC - 1))
                                return r
                            kb.op("pe", mm, reads=[B_wb[i], B_uT], writes=[B_pb[pi]])
                            evac_store(pi, ev, scale, dst[tt * 128:(tt + 1) * 128, base:base + 512])
                kb.barrier()

        phase(3)
        with contextlib.ExitStack() as s3:
            yT = [sb(s3, f"yT{i}", [128, 8, T], BF16) for i in range(2)]
            B_yT = [Buf(), Buf()]
            with contextlib.ExitStack() as s3a:
                qTh = [sb(s3a, f"qTh{i}", [128, T], BF16) for i in range(2)]
                kTh = [sb(s3a, f"kTh{i}", [128, T], BF16) for i in range(2)]
                vh = [sb(s3a, f"vh{i}", [128, 16, 128], BF16) for i in range(2)]
                Rh = [sb(s3a, f"Rh{i}", [128, 8 * 512], BF16) for i in range(2)]
                B_hd = [Buf(), Buf()]
                cA = sb(s3a, "cA", [128, 8], F32)
                ones_b = sb(s3a, "ones_b", [128, 128], BF16)
                lq = sb(s3a, "lq", [128, 256], F32)
                ltmp = sb(s3a, "ltmp", [128, 64], F32)
                lst = sb(s3a, "lst", [128, 4], F32)
                nl = sb(s3a, "nl", [128, 1], F32)
                B_l = Buf()
                NPB = 3
                PTs = [sb(s3a, f"PTs{i}", [128, 512], BF16) for i in range(NPB)]
                B_PTs = [Buf() for _ in range(NPB)]
                rden = [sb(s3a, f"rden{i}", [128, 512], F32) for i in range(2)]
                B_rden = [Buf(), Buf()]
                t0s = sb(s3a, "t0s", [128, 512], F32)
                oos = sb(s3a, "oos", [128, 512], F32)
                sqs = sb(s3a, "sqs", [128, 512], BF16)
                stds = sb(s3a, "stds", [128, 512], F32)
                B_t0, B_oo, B_sq, B_std = Buf(), Buf(), Buf(), Buf()
                ST = [ps(s3a, f"ST{i}", [128, 512], F32) for i in range(NPB)]
                B_ST = [Buf() for _ in range(NPB)]
                ACC = [ps(s3a, f"ACC{i}", [128, 512], F32) for i in range(2)]
                DEN = [ps(s3a, f"DEN{i}", [128, 512], F32) for i in range(2)]
                B_ACC = [Buf(), Buf()]
                B_DEN = [Buf(), Buf()]
                SSQ = ps(s3a, "SSQ", [128, 512], F32)
                B_SSQ = Buf()

                kb.op("dve", lambda v: v.memset(ones_b[:], 1.0), writes=[B_l])
                kb.dma("sp", dq[8], cA[:], cvecA[:, :], writes=[B_l], waw=False)
                kb.dma("sp", dq[8], lq[:], lam_in[0, :].partition_broadcast(128), writes=[B_l], waw=False)
                for j in range(2):
                    kb.op("dve", lambda v, j=j: v.tensor_tensor(out=ltmp[:], in0=lq[:, j * 128:j * 128 + 64],
                                                                in1=lq[:, j * 128 + 64:j * 128 + 128], op=ALU.mult),
                          reads=[B_l], writes=[B_l])
                    kb.op("dve", lambda v, j=j: v.reduce_sum(out=lst[:, j:j + 1], in_=ltmp[:], axis=AX.X), reads=[B_l], writes=[B_l])
                kb.op("act", lambda a: a.activation(out=lst[:, 2:4], in_=lst[:, 0:2], func=AF.Exp), reads=[B_l], writes=[B_l])
                lam_init = 0.8 - 0.6 * math.exp(-0.3 * 0)
                kb.op("dve", lambda v: v.scalar_tensor_tensor(out=nl[:], in0=lst[:, 3:4], scalar=-lam_init, in1=lst[:, 2:3],
                                                              op0=ALU.add, op1=ALU.subtract), reads=[B_l], writes=[B_l])

                pcnt_ = [0]
                gcnt_ = [0]
                for which in range(2):
                    nmaps = 2 if which == 0 else 1
                    dk = 64 if which == 0 else 128
                    omin = -1 if which == 0 else -4
                    ntile = 5 if which == 0 else 8
                    bsrc = biasAT if which == 0 else biasBT
                    qd, kd, vd = qkT[2 * which], qkT[2 * which + 1], vv[which]
                    vdv = vd.rearrange("(tt p) c -> p tt c", p=128)
                    for h in range(8):
                        hi = (which * 8 + h) % 2
                        kb.dma("sp", dq[hi], qTh[hi][:], qd[h * 128:(h + 1) * 128, :], writes=[B_hd[hi]], waw=False)
                        kb.dma("sp", dq[hi], kTh[hi][:], kd[h * 128:(h + 1) * 128, :], writes=[B_hd[hi]], waw=False)
                        kb.dma("act", dq[hi], vh[hi][:], vdv[:, :, h * 128:(h + 1) * 128], writes=[B_hd[hi]], waw=False)
                        for half_ in range(2):
                            hw_ = ntile * 512 // 2
                            kb.dma("pool", dp[hi], Rh[hi][:, half_ * hw_:(half_ + 1) * hw_], bsrc[h, :, half_ * hw_:(half_ + 1) * hw_],
                                   writes=[B_hd[hi]], waw=False)
                        for g in range(4):
                            for m in range(nmaps):
                                gi = gcnt_[0] % 2
                                gcnt_[0] += 1
                                kb_lo = 0 if which == 0 else max(0, 4 * g - 4)
                                kbs = list(range(kb_lo, 4 * g + 4))
                                pend = []

                                def emit_pv(item, gi=gi, hi=hi):
                                    kbi, pi, c0, first, last = item

                                    def pv(pe):
                                        pe.matmul(ACC[gi][:, c0:512], lhsT=vh[hi][:, kbi, :], rhs=PTs[pi][:, c0:512], start=first, stop=last)
                                        return pe.matmul(DEN[gi][:, c0:512], lhsT=ones_b[:], rhs=PTs[pi][:, c0:512], start=first, stop=last)
                                    kb.op("pe", pv, reads=[B_PTs[pi], B_hd[hi], B_l], writes=[B_ACC[gi], B_DEN[gi]])

                                for idx, kbi in enumerate(kbs):
                                    o = kbi - 4 * g
                                    pi = pcnt_[0] % NPB
                                    pcnt_[0] += 1
                                    c0 = max(0, o) * 128
                                    near = (o >= omin)

                                    def qk(pe, hi=hi, m=m, g=g, kbi=kbi, o=o, pi=pi, c0=c0, near=near):
                                        r = pe.matmul(ST[pi][:, c0:512], lhsT=kTh[hi][m * dk:(m + 1) * dk, kbi * 128:(kbi + 1) * 128],
                                                      rhs=qTh[hi][m * dk:(m + 1) * dk, g * 512 + c0:(g + 1) * 512], start=True, stop=(not near))
                                        if near:
                                            t_ = o - omin
                                            r = pe.matmul(ST[pi][:, c0:512], lhsT=identb[:], rhs=Rh[hi][:, t_ * 512 + c0:(t_ + 1) * 512],
                                                          start=False, stop=True)
                                        return r
                                    kb.op("pe", qk, reads=[B_hd[hi], B_const], writes=[B_ST[pi]])
                                    if near:
                                        kb.op("act", lambda a, pi=pi, c0=c0: a.activation(out=PTs[pi][:, c0:512], in_=ST[pi][:, c0:512], func=AF.Exp),
                                              reads=[B_ST[pi]], writes=[B_PTs[pi]])
                                    else:
                                        kb.op("act", lambda a, pi=pi, c0=c0, h=h: a.activation(out=PTs[pi][:, c0:512], in_=ST[pi][:, c0:512], func=AF.Exp,
                                                                                               bias=cA[:, h:h + 1], scale=1.0),
                                              reads=[B_ST[pi], B_l], writes=[B_PTs[pi]])
                                    pend.append((kbi, pi, c0, idx == 0, idx == len(kbs) - 1))
                                    if len(pend) > 2:
                                        emit_pv(pend.pop(0))
                                while pend:
                                    emit_pv(pend.pop(0))
                                qs = slice(g * 512, (g + 1) * 512)
                                kb.op("dve", lambda v, gi=gi: v.reciprocal(out=rden[gi][:], in_=DEN[gi][:]), reads=[B_DEN[gi]], writes=[B_rden[gi]])
                                if which == 1:
                                    kb.op("dve", lambda v, gi=gi, h=h, qs=qs: v.tensor_tensor(out=yT[1][:, h, qs], in0=ACC[gi][:], in1=rden[gi][:], op=ALU.mult),
                                          reads=[B_ACC[gi], B_rden[gi]], writes=[B_yT[1]], waw=False)
                                elif m == 0:
                                    kb.op("dve", lambda v, gi=gi: v.tensor_tensor(out=t0s[:], in0=ACC[gi][:], in1=rden[gi][:], op=ALU.mult),
                                          reads=[B_ACC[gi], B_rden[gi]], writes=[B_t0])
                                else:
                                    kb.op("dve", lambda v, gi=gi: v.tensor_tensor(out=oos[:], in0=ACC[gi][:], in1=rden[gi][:], op=ALU.mult),
                                          reads=[B_ACC[gi], B_rden[gi]], writes=[B_oo])
                                    kb.op("dve", lambda v: v.scalar_tensor_tensor(out=oos[:], in0=oos[:], scalar=nl[:, 0:1], in1=t0s[:],
                                                                                  op0=ALU.mult, op1=ALU.add),
                                          reads=[B_oo, B_t0, B_l], writes=[B_oo])
                                    kb.op("dve", lambda v: v.tensor_tensor(out=sqs[:], in0=oos[:], in1=oos[:], op=ALU.mult), reads=[B_oo], writes=[B_sq])
                                    kb.op("pe", lambda pe: pe.matmul(SSQ[:], lhsT=ones_b[:], rhs=sqs[:], start=True, stop=True),
                                          reads=[B_sq, B_l], writes=[B_SSQ])
                                    kb.op("act", lambda a: a.activation(out=stds[:], in_=SSQ[:], func=AF.Sqrt, bias=1e-5, scale=1.0 / 128),
                                          reads=[B_SSQ], writes=[B_std])
                                    kb.op("dve", lambda v: v.reciprocal(out=stds[:], in_=stds[:]), reads=[B_std], writes=[B_std])
                                    kb.op("dve", lambda v, h=h, qs=qs: v.scalar_tensor_tensor(out=yT[0][:, h, qs], in0=oos[:], scalar=float(1.0 - lam_init),
                                                                                              in1=stds[:], op0=ALU.mult, op1=ALU.mult),
                                          reads=[B_oo, B_std], writes=[B_yT[0]], waw=False)
                kb.barrier()

            phase(4)
            with contextlib.ExitStack() as s4:
                mT = sb(s4, "mT", [128, KC, T], BF16)
                B_mT = Buf("mT")
                with contextlib.ExitStack() as s4a:
                    wua = [sb(s4a, f"wua{i}", [128, 8, 256], BF16) for i in range(2)]
                    wub = [sb(s4a, f"wub{i}", [128, 8, 256], BF16) for i in range(2)]
                    B_wu = [Buf(), Buf()]
                    gat = [sb(s4a, f"gat{i}", [128, 512], BF16) for i in range(2)]
                    gbt = [sb(s4a, f"gbt{i}", [128, 512], BF16) for i in range(2)]
                    B_gt = [Buf(), Buf()]
                    m1 = [sb(s4a, f"m1{i}", [128, 512], F32) for i in range(2)]
                    m2 = [sb(s4a, f"m2{i}", [128, 512], F32) for i in range(2)]
                    B_m = [Buf(), Buf()]
                    pa = [ps(s4a, f"pa{i}", [128, 512], F32) for i in range(2)]
                    pbb = [ps(s4a, f"pbb{i}", [128, 512], F32) for i in range(2)]
                    B_pab = [Buf(), Buf()]
                    wuav = w_up_a.rearrange("(kc p) n -> p kc n", p=128)
                    wubv = w_up_b.rearrange("(kc p) n -> p kc n", p=128)
                    n = 0
                    for dg in range(8):
                        wi = dg % 2
                        kb.dma("pool", dp[wi], wua[wi][:], wuav[:, :, dg * 256:(dg + 1) * 256], writes=[B_wu[wi]])
                        kb.dma("pool", dp[wi], wub[wi][:], wubv[:, :, dg * 256:(dg + 1) * 256], writes=[B_wu[wi]])
                        for sub in range(2):
                            dc = dg * 2 + sub
                            for tb in range(4):
                                i = n % 2
                                n += 1
                                kb.dma("sp", dq[2 + i], gat[i][:], gT[dc * 128:(dc + 1) * 128, tb * 512:(tb + 1) * 512], writes=[B_gt[i]])
                                kb.dma("sp", dq[2 + i], gbt[i][:], gT[2048 + dc * 128:2048 + (dc + 1) * 128, tb * 512:(tb + 1) * 512],
                                       writes=[B_gt[i]])

                                def mm(pe, wi=wi, sub=sub, tb=tb, i=i):
                                    r = None
                                    for kc in range(8):
                                        pe.matmul(pa[i][:], lhsT=wua[wi][:, kc, sub * 128:(sub + 1) * 128],
                                                  rhs=yT[0][:, kc, tb * 512:(tb + 1) * 512], start=(kc == 0), stop=(kc == 7))
                                    for kc in range(8):
                                        r = pe.matmul(pbb[i][:], lhsT=wub[wi][:, kc, sub * 128:(sub + 1) * 128],
                                                      rhs=yT[1][:, kc, tb * 512:(tb + 1) * 512], start=(kc == 0), stop=(kc == 7))
                                    return r
                                kb.op("pe", mm, reads=[B_wu[wi], B_yT[0], B_yT[1]], writes=[B_pab[i]])
                                kb.op("dve", lambda v, i=i: v.tensor_tensor(out=m1[i][:], in0=pa[i][:], in1=gat[i][:], op=ALU.mult),
                                      reads=[B_pab[i], B_gt[i]], writes=[B_m[i]])
                                kb.op("dve", lambda v, i=i: v.tensor_tensor(out=m2[i][:], in0=pbb[i][:], in1=gbt[i][:], op=ALU.mult),
                                      reads=[B_pab[i], B_gt[i]], writes=[B_m[i]], waw=False)
                                kb.op("dve", lambda v, i=i, dc=dc, tb=tb: v.tensor_tensor(out=mT[:, dc, tb * 512:(tb + 1) * 512], in0=m1[i][:],
                                                                                          in1=m2[i][:], op=ALU.add),
                                      reads=[B_m[i]], writes=[B_mT], waw=False)
                    kb.barrier()

                phase(5)
                with contextlib.ExitStack() as s4b:
                    wo = [sb(s4b, f"wo{i}", [128, KC, 512], BF16) for i in range(2)]
                    B_wo = [Buf(), Buf()]
                    gtA = sb(s4b, "gtA", [128, D], F32)
                    B_gtA = Buf()
                    xp = [sb(s4b, f"xp{i}", [128, 512], F32) for i in range(3)]
                    B_xp = [Buf() for _ in range(3)]
                    hp = [sb(s4b, f"hp{i}", [128, 512], F32) for i in range(3)]
                    B_hp = [Buf() for _ in range(3)]
                    po = [ps(s4b, f"po{i}", [128, 512], F32) for i in range(3)]
                    B_po = [Buf() for _ in range(3)]
                    B_h1 = Buf("h1d")
                    kb.dma("sp", dq[8], gtA[:], modsave[0, :].partition_broadcast(128), writes=[B_gtA])
                    wov = w_o.rearrange("(kc p) n -> p kc n", p=128)
                    n = 0
                    for ob in range(4):
                        wi = ob % 2
                        kb.dma("pool", dp[wi], wo[wi][:, 0:8, :], wov[:, 0:8, ob * 512:(ob + 1) * 512], writes=[B_wo[wi]])
                        kb.dma("pool", dp[wi], wo[wi][:, 8:16, :], wov[:, 8:16, ob * 512:(ob + 1) * 512], writes=[B_wo[wi]])
                        for tt in range(16):
                            i = n % 3
                            n += 1
                            kb.dma("sp", dq[2 + i], xp[i][:], x[tt * 128:(tt + 1) * 128, ob * 512:(ob + 1) * 512], writes=[B_xp[i]])

                            def mm(pe, wi=wi, tt=tt, i=i):
                                r = None
                                for kc in range(KC):
                                    r = pe.matmul(po[i][:], lhsT=mT[:, kc, tt * 128:(tt + 1) * 128], rhs=wo[wi][:, kc, :],
                                                  start=(kc == 0), stop=(kc == KC - 1))
                                return r
                            kb.op("pe", mm, reads=[B_wo[wi], B_mT], writes=[B_po[i]])
                            kb.op("dve", lambda v, i=i, ob=ob: v.tensor_tensor(out=hp[i][:], in0=po[i][:], in1=gtA[:, ob * 512:(ob + 1) * 512],
                                                                               op=ALU.mult),
                                  reads=[B_po[i], B_gtA], writes=[B_hp[i]])
                            kb.op("dve", lambda v, i=i: v.tensor_tensor(out=hp[i][:], in0=hp[i][:], in1=xp[i][:], op=ALU.add),
                                  reads=[B_hp[i], B_xp[i]], writes=[B_hp[i]])
                            kb.dma("sp", dq[5 + i], h1d[tt * 128:(tt + 1) * 128, ob * 512:(ob + 1) * 512], hp[i][:],
                                   reads=[B_hp[i]], writes=[B_h1], waw=False)
                    kb.barrier()

        phase(6)
        with contextlib.ExitStack() as s5:
            h1t = [sb(s5, f"h1t{i}", [128, D], F32) for i in range(2)]
            B_h1t = [Buf(), Buf()]
            junk = sb(s5, "junk5", [128, D], BF16)
            ss = sb(s5, "ss5", [128, 1], F32)
            rstd = sb(s5, "rstd5", [128, 1], F32)
            xh = sb(s5, "xh5", [128, D], F32)
            p4 = ps(s5, "p45", [128, D], F32)
            nb = (junk, ss, rstd, xh, p4, Buf(), Buf(), Buf(), Buf())
            v16 = [sb(s5, f"v16{i}", [128, KC, 128], BF16) for i in range(2)]
            v32 = [sb(s5, f"v32{i}", [128, KC, 128], F32) for i in range(2)]
            B_v = [Buf(), Buf()]
            B_v16 = [Buf(), Buf()]
            wr = sb(s5, "wr", [128, KC, NE], F32)
            rb = sb(s5, "rb", [128, NE], F32)
            B_wr = Buf()
            plg_ = ps(s5, "plg", [128, 512], F32)
            plg = plg_[:, 0:NE]
            pwt_ = ps(s5, "pwt", [128, 512], F32)
            pwt = pwt_[0:NE, 0:128]
            B_plg, B_pwt = Buf(), Buf()
            sc = sb(s5, "sc", [128, NE], F32)
            bi = sb(s5, "bi", [128, NE], F32)
            t8 = sb(s5, "t8", [128, 8, 8], F32)
            gs = sb(s5, "gs", [128, 8], F32)
            g8 = sb(s5, "g8", [128, 8], F32)
            gm = sb(s5, "gm", [128, 8], F32)
            mb = sb(s5, "mb", [128, 8], F32)
            msk = sb(s5, "msk", [128, NE], F32)
            m8 = sb(s5, "m8", [128, 8], F32)
            sel = sb(s5, "sel", [128, NE], F32)
            den = sb(s5, "den", [128, 1], F32)
            wc = sb(s5, "wc", [128, NE], F32)
            wcs = [sb(s5, f"wcs{i}", [NE, 128], F32) for i in range(2)]
            B_wcs = [Buf(), Buf()]
            B_rt = Buf()
            B_VTd, B_wcTd = Buf(), Buf()
            kb.dma("sp", dq[8], wr[:], w_router.rearrange("(kc p) n -> p kc n", p=128), writes=[B_wr])
            kb.dma("sp", dq[8], rb[:], rbias[0, :].partition_broadcast(128), writes=[B_wr])
            for tt in range(16):
                i = tt % 2
                kb.dma("sp", dq[i], h1t[i][:], h1d[tt * 128:(tt + 1) * 128, :], writes=[B_h1t[i]])
                norm_tile(nb, h1t[i], B_h1t[i], tt, A_m, S_m,
                          lambda kc, i=i: (v16[i][:] if kc is None else v16[i][:, kc, :]), B_v[i],
                          dst32=lambda kc, i=i: (v32[i][:] if kc is None else v32[i][:, kc, :]), B_dst16=B_v16[i])
                kb.dma("sp", dq[2 + i], VTd[:, :, tt * 128:(tt + 1) * 128].rearrange("k p t -> p k t"), v16[i][:],
                       reads=[B_v16[i]], writes=[B_VTd], waw=False)

                def mm(pe, i=i):
                    r = None
                    for kc in range(KC):
                        r = pe.matmul(plg[:], lhsT=v32[i][:, kc, :], rhs=wr[:, kc, :], start=(kc == 0), stop=(kc == KC - 1))
                    return r
                kb.op("pe", mm, reads=[B_v[i], B_wr], writes=[B_plg])
                R = [B_rt]
                kb.op("act", lambda a: a.activation(out=sc[:], in_=plg[:], func=AF.Sigmoid), reads=[B_plg], writes=R)
                kb.op("dve", lambda v: v.tensor_tensor(out=bi[:], in0=sc[:], in1=rb[:], op=ALU.add), reads=R + [B_wr], writes=R)
                for g in range(8):
                    kb.op("dve", lambda v, g=g: v.max(out=t8[:, g, :], in_=bi[:, g * 8:(g + 1) * 8]), reads=R, writes=R)
                kb.op("dve", lambda v: v.tensor_tensor(out=gs[:], in0=t8[:, :, 0], in1=t8[:, :, 1], op=ALU.add), reads=R, writes=R)
                kb.op("dve", lambda v: v.max(out=g8[:], in_=gs[:]), reads=R, writes=R)
                kb.op("dve", lambda v: v.tensor_scalar(out=gm[:], in0=gs[:], scalar1=g8[:, 3:4], scalar2=None, op0=ALU.is_ge),
                      reads=R, writes=R)
                kb.op("dve", lambda v: v.tensor_scalar(out=mb[:], in0=gm[:], scalar1=-1.0, scalar2=1e9, op0=ALU.add, op1=ALU.mult),
                      reads=R, writes=R)
                for g in range(8):
                    kb.op("dve", lambda v, g=g: v.tensor_scalar(out=msk[:, g * 8:(g + 1) * 8], in0=bi[:, g * 8:(g + 1) * 8],
                                                               scalar1=gm[:, g:g + 1], scalar2=mb[:, g:g + 1], op0=ALU.mult, op1=ALU.add),
                          reads=R, writes=R)
                kb.op("dve", lambda v: v.max(out=m8[:], in_=msk[:]), reads=R, writes=R)
                kb.op("dve", lambda v: v.tensor_scalar(out=sel[:], in0=msk[:], scalar1=m8[:, 7:8], scalar2=None, op0=ALU.is_ge),
                      reads=R, writes=R)
                kb.op("dve", lambda v: v.tensor_tensor(out=sel[:], in0=sel[:], in1=sc[:], op=ALU.mult), reads=R, writes=R)
                kb.op("dve", lambda v: v.reduce_sum(out=den[:], in_=sel[:], axis=AX.X), reads=R, writes=R)
                kb.op("dve", lambda v: v.reciprocal(out=den[:], in_=den[:]), reads=R, writes=R)
                kb.op("dve", lambda v: v.tensor_scalar(out=wc[:], in0=sel[:], scalar1=den[:, 0:1], scalar2=2.5, op0=ALU.mult, op1=ALU.mult),
                      reads=R, writes=R)
                kb.op("pe", lambda pe: pe.transpose(out=pwt[:], in_=wc[:], identity=identf[:]), reads=R + [B_const], writes=[B_pwt])
                kb.op("act", lambda a, i=i: a.copy(out=wcs[i][:], in_=pwt[:]), reads=[B_pwt], writes=[B_wcs[i]])
                kb.dma("sp", dq[4 + i], wcTd[:, tt * 128:(tt + 1) * 128], wcs[i][:], reads=[B_wcs[i]], writes=[B_wcTd], waw=False)
            kb.barrier()

        phase(7)
        for pz in range(2):
            t0 = pz * 1024
            with contextlib.ExitStack() as s6:
                acc = sb(s6, "acc", [128, 8, D], F32)
                B_acc = [Buf() for _ in range(8)]
                with contextlib.ExitStack() as s6a:
                    vt = sb(s6a, "vt", [128, KC, 1024], BF16)
                    B_vt = Buf()
                    wct = sb(s6a, "wct", [NE, 1024], F32)
                    B_wct = Buf()
                    gu = [sb(s6a, f"gu{i}", [128, 2, KC, 128], BF16) for i in range(3)]
                    B_gu = [Buf() for _ in range(3)]
                    dd = sb(s6a, "dd", [128, 4, D], BF16)
                    B_dd = Buf()
                    hT = sb(s6a, "hT", [128, 4, 1024], BF16)
                    B_hT = Buf()
                    sg = [sb(s6a, f"sg{i}", [128, 512], F32) for i in range(2)]
                    sgw = [sb(s6a, f"sgw{i}", [128, 512], F32) for i in range(2)]
                    B_sg = [Buf(), Buf()]
                    B_sgw = [Buf(), Buf()]
                    wbc = sb(s6a, "wbc", [128, 1024], F32)
                    B_wbc = Buf()
                    sl = [sb(s6a, f"sl{i}", [NE, 128], F32) for i in range(2)]
                    B_sl = [Buf(), Buf()]
                    pg = [ps(s6a, f"pg{i}", [128, 512], F32) for i in range(2)]
                    pu = [ps(s6a, f"pu{i}", [128, 512], F32) for i in range(2)]
                    B_pgu = [Buf(), Buf()]
                    py = [ps(s6a, f"py{i}", [128, 512], F32) for i in range(3)]
                    B_py = [Buf() for _ in range(3)]
                    pw = ps(s6a, "pw", [128, 512], F32)
                    B_pw = Buf()

                    for kc4 in range(4):
                        kb.dma("sp", dq[8], vt[:, kc4 * 4:(kc4 + 1) * 4, :],
                               VTd[kc4 * 4:(kc4 + 1) * 4, :, t0:t0 + 1024].rearrange("k p t -> p k t"), writes=[B_vt])
                    kb.dma("sp", dq[9], wct[:], wcTd[:, t0:t0 + 1024], writes=[B_wct])

                    NU = (NE + 1) * 4

                    def load_gu(u):
                        e, c = divmod(u, 4)
                        bi_ = u % 3
                        kb.dma("pool", dp[bi_], gu[bi_][:].rearrange("p g k m -> p g (k m)"),
                               wgu[e, c].rearrange("p (g j) -> p g j", g=2), writes=[B_gu[bi_]])

                    def load_d(e):
                        for c in range(4):
                            kb.dma("pool", dp[3], dd[:, c, :], wd[e, c * 128:(c + 1) * 128, :], writes=[B_dd])

                    load_gu(0)
                    load_gu(1)
                    load_d(0)
                    ny = 0
                    for e in range(NE + 1):
                        if e < NE:
                            si = e % 2
                            kb.op("dve", lambda v, si=si, e=e: v.tensor_scalar(out=sl[si][:], in0=ones_f[0:NE, :], scalar1=identf[0:NE, e:e + 1],
                                                                               scalar2=None, op0=ALU.mult),
                                  reads=[B_const], writes=[B_sl[si]])
                            for j in range(2):
                                kb.op("pe", lambda pe, si=si, j=j: pe.matmul(pw[:], lhsT=sl[si][:], rhs=wct[:, j * 512:(j + 1) * 512],
                                                                             start=True, stop=True),
                                      reads=[B_sl[si], B_wct], writes=[B_pw])
                                kb.op("act", lambda a, j=j: a.copy(out=wbc[:, j * 512:(j + 1) * 512], in_=pw[:]),
                                      reads=[B_pw], writes=[B_wbc], waw=(j == 0))
                        else:
                            kb.op("dve", lambda v: v.memset(wbc[:], 1.0), writes=[B_wbc])
                        for c in range(4):
                            u = e * 4 + c
                            if u + 2 < NU:
                                load_gu(u + 2)
                            bi_ = u % 3
                            for tb in range(2):
                                i = (u * 2 + tb) % 2

                                def mm(pe, bi_=bi_, tb=tb, i=i):
                                    r = None
                                    for kc in range(KC):
                                        pe.matmul(pg[i][:], lhsT=gu[bi_][:, 0, kc, :], rhs=vt[:, kc, tb * 512:(tb + 1) * 512],
                                                  start=(kc == 0), stop=(kc == KC - 1))
                                    for kc in range(KC):
                                        r = pe.matmul(pu[i][:], lhsT=gu[bi_][:, 1, kc, :], rhs=vt[:, kc, tb * 512:(tb + 1) * 512],
                                                      start=(kc == 0), stop=(kc == KC - 1))
                                    return r
                                kb.op("pe", mm, reads=[B_gu[bi_], B_vt], writes=[B_pgu[i]])
                                kb.op("act", lambda a, i=i: a.activation(out=sg[i][:], in_=pg[i][:], func=AF.Silu),
                                      reads=[B_pgu[i]], writes=[B_sg[i]])
                                kb.op("dve", lambda v, i=i, tb=tb: v.tensor_tensor(out=sgw[i][:], in0=sg[i][:], in1=wbc[:, tb * 512:(tb + 1) * 512],
                                                                                   op=ALU.mult),
                                      reads=[B_sg[i], B_wbc], writes=[B_sgw[i]])
                                kb.op("dve", lambda v, i=i, c=c, tb=tb: v.tensor_tensor(out=hT[:, c, tb * 512:(tb + 1) * 512], in0=pu[i][:],
                                                                                        in1=sgw[i][:], op=ALU.mult),
                                      reads=[B_pgu[i], B_sgw[i]], writes=[B_hT], waw=False)
                        for tt in range(8):
                            for ob in range(4):
                                yi = ny % 3
                                ny += 1

                                def mmd(pe, tt=tt, ob=ob, yi=yi):
                                    r = None
                                    for c in range(4):
                                        r = pe.matmul(py[yi][:], lhsT=hT[:, c, tt * 128:(tt + 1) * 128], rhs=dd[:, c, ob * 512:(ob + 1) * 512],
                                                      start=(c == 0), stop=(c == 3))
                                    return r
                                kb.op("pe", mmd, reads=[B_hT, B_dd], writes=[B_py[yi]])
                                if e == 0:
                                    kb.op("dve", lambda v, tt=tt, ob=ob, yi=yi: v.tensor_copy(out=acc[:, tt, ob * 512:(ob + 1) * 512], in_=py[yi][:]),
                                          reads=[B_py[yi]], writes=[B_acc[tt]], waw=False)
                                else:
                                    kb.op("dve", lambda v, tt=tt, ob=ob, yi=yi: v.tensor_tensor(out=acc[:, tt, ob * 512:(ob + 1) * 512],
                                                                                                in0=py[yi][:], in1=acc[:, tt, ob * 512:(ob + 1) * 512],
                                                                                                op=ALU.add),
                                          reads=[B_py[yi], B_acc[tt]], writes=[B_acc[tt]], waw=False)
                        if e + 1 <= NE:
                            load_d(e + 1)
                    kb.barrier()

                with contextlib.ExitStack() as s6b:
                    gtM = sb(s6b, "gtM", [128, D], F32)
                    gF = sb(s6b, "gF", [128, D], F32)
                    B_gc = Buf()
                    h1f = [sb(s6b, f"h1f{i}", [128, D], F32) for i in range(2)]
                    B_h1f = [Buf(), Buf()]
                    ot = [sb(s6b, f"ot{i}", [128, D], F32) for i in range(2)]
                    B_ot = [Buf(), Buf()]
                    jk = sb(s6b, "jk6", [128, D], BF16)
                    fs = [sb(s6b, f"fs{i}", [128, 1], F32) for i in range(2)]
                    B_fs = [Buf(), Buf()]
                    B_jk = Buf()
                    B_out = Buf()
                    kb.dma("sp", dq[8], gtM[:], modsave[1, :].partition_broadcast(128), writes=[B_gc])
                    kb.dma("sp", dq[8], gF[:], g_final[0, :].partition_broadcast(128), writes=[B_gc])
                    for tt in range(8):
                        i = tt % 2
                        r0 = t0 + tt * 128
                        kb.dma("sp", dq[i], h1f[i][:], h1d[r0:r0 + 128, :], writes=[B_h1f[i]])
                        kb.op("dve", lambda v, tt=tt: v.tensor_tensor(out=acc[:, tt, :], in0=acc[:, tt, :], in1=gtM[:], op=ALU.mult),
                              reads=[B_acc[tt], B_gc], writes=[B_acc[tt]])
                        kb.op("dve", lambda v, tt=tt, i=i: v.tensor_tensor(out=h1f[i][:], in0=h1f[i][:], in1=acc[:, tt, :], op=ALU.add),
                              reads=[B_acc[tt], B_h1f[i]], writes=[B_h1f[i]])
                        kb.op("act", lambda a, i=i: a.activation(out=jk[:], in_=h1f[i][:], func=AF.Square, accum_out=fs[i][:]),
                              reads=[B_h1f[i]], writes=[B_jk, B_fs[i]])
                        kb.op("act", lambda a, i=i: a.activation(out=fs[i][:], in_=fs[i][:], func=AF.Sqrt, bias=1e-6, scale=1.0 / D),
                              reads=[B_fs[i]], writes=[B_fs[i]])
                        kb.op("dve", lambda v, i=i: v.reciprocal(out=fs[i][:], in_=fs[i][:]), reads=[B_fs[i]], writes=[B_fs[i]])
                        kb.op("dve", lambda v, i=i: v.scalar_tensor_tensor(out=ot[i][:], in0=h1f[i][:], scalar=fs[i][:, 0:1], in1=gF[:],
                                                                           op0=ALU.mult, op1=ALU.mult),
                              reads=[B_h1f[i], B_fs[i], B_gc], writes=[B_ot[i]])
                        kb.dma("sp", dq[2 + i], out[r0:r0 + 128, :], ot[i][:], reads=[B_ot[i]], writes=[B_out], waw=False)
                    kb.barrier()
    return nc


_NC = None


def t5_bucket(rel):
    nb = 16
    ret = (rel > 0).astype(np.int32) * nb
    n = np.abs(rel)
    max_exact = nb // 2
    large = max_exact + (np.log(np.maximum(n, 1) / max_exact) / math.log(128 / max_exact) * (nb - max_exact)).astype(np.int32)
    large = np.minimum(large, nb - 1)
    return (ret + np.where(n < max_exact, n, large)).astype(np.int32)


def attn_bias_tiles(t5, relb):
    k = np.arange(128)[:, None]
    q = np.arange(512)[None, :]
    tilesA = []
    for o in range(-1, 4):
        kpos = (4 + o) * 128 + k
        qpos = 4 * 128 + q
        idx = t5_bucket(kpos - qpos)
        allowed = (kpos // 64) <= (qpos // 64)
        tilesA.append(np.stack([np.where(allowed, t5[idx, h], np.float32(NEG)) for h in range(8)]))
    biasAT = np.concatenate(tilesA, axis=2).astype(np.float32)
    tilesB = []
    for o in range(-4, 4):
        kpos = (4 + o) * 128 + k
        qpos = 4 * 128 + q
        rel = np.clip(kpos - qpos, -256, 256) + 256
        kc, qc = kpos // 64, qpos // 64
        allowed = (kc <= qc) & (kc >= qc - 8)
        tilesB.append(np.stack([np.where(allowed, relb[rel, h], np.float32(NEG)) for h in range(8)]))
    biasBT = np.concatenate(tilesB, axis=2).astype(np.float32)
    cvecA = np.ascontiguousarray(np.broadcast_to(t5[15, :][None, :], (128, 8))).astype(np.float32)
    return np.ascontiguousarray(biasAT), np.ascontiguousarray(biasBT), cvecA


def kernel(x, c, w_ada, b_ada, g_attn, w_in, lambda_qk, t5_bias, rel_bias_b, w_up_a, w_up_b, w_o,
           g_moe, w_router, router_bias, w_exp_gate, w_exp_up, w_exp_down,
           w_sh_gate, w_sh_up, w_sh_down, g_final):
    global _NC
    f = lambda a: np.ascontiguousarray(np.asarray(a, dtype=np.float32))
    x = f(x); c = f(c)
    gfm = np.concatenate([f(g_attn)[0].reshape(16, 128).T, f(g_moe)[0].reshape(16, 128).T], axis=1)
    biasAT, biasBT, cvecA = attn_bias_tiles(f(t5_bias), f(rel_bias_b)[0])

    def lay(w):
        return w.reshape(16, 128, 4, 128).transpose(2, 1, 0, 3).reshape(4, 128, 2048)
    wg = f(w_exp_gate)[0]; wu = f(w_exp_up)[0]
    wgu = np.empty((NE + 1, 4, 128, 4096), np.float32)
    for e in range(NE):
        wgu[e, :, :, 0:2048] = lay(wg[e])
        wgu[e, :, :, 2048:4096] = lay(wu[e])
    wgu[NE, :, :, 0:2048] = lay(f(w_sh_gate)[0])
    wgu[NE, :, :, 2048:4096] = lay(f(w_sh_up)[0])
    wdn = np.concatenate([f(w_exp_down)[0], f(w_sh_down)], axis=0)

    shared = {
        "w_ada": f(w_ada)[0], "b_ada": f(b_ada), "gfm": np.ascontiguousarray(gfm), "g_final": f(g_final).reshape(1, D),
        "w_in": f(w_in)[0], "lam": f(lambda_qk).reshape(1, 256), "biasAT": biasAT, "biasBT": biasBT, "cvecA": cvecA,
        "w_up_a": f(w_up_a)[0], "w_up_b": f(w_up_b)[0], "w_o": f(w_o)[0], "w_router": f(w_router)[0],
        "rbias": f(router_bias), "wgu": wgu, "wd": np.ascontiguousarray(wdn),
    }
    in_maps = []
    for b in range(8):
        m = dict(shared)
        m["x"] = x[b]
        m["cT"] = np.ascontiguousarray(c[b].reshape(16, 128).T)
        in_maps.append(m)
    if _NC is None:
        _NC = build()
    res = run_bass_kernel_spmd(_NC, in_maps, core_ids=list(range(8)))
    return np.stack([np.asarray(r["out"], dtype=np.float32) for r in res.results], axis=0)
```

```python
import contextlib
import os
import math
import numpy as np
import concourse.bass as bass
import concourse.mybir as mybir
from concourse.bass_utils import run_bass_kernel_spmd

F32 = mybir.dt.float32
BF16 = mybir.dt.bfloat16
AF = mybir.ActivationFunctionType
ALU = mybir.AluOpType
AX = mybir.AxisListType

T = 2048
D = 2048
KC = 16
NE = 64
NEG = -1e30


class _Stop(Exception):
    pass


class Tk:
    __slots__ = ("sem", "val", "eng")

    def __init__(self, sem, val, eng):
        self.sem = sem
        self.val = val
        self.eng = eng


class Buf:
    def __init__(self, name=""):
        self.name = name
        self.w = {}
        self.r = {}


class DSem:
    def __init__(self, h):
        self.h = h
        self.cnt = 0


class KB:
    def __init__(self, nc, es):
        self.nc = nc
        self.es = es
        self.E = {"pe": nc.tensor, "act": nc.scalar, "dve": nc.vector, "pool": nc.gpsimd, "sp": nc.sync}
        self.psem = {e: es.enter_context(nc.semaphore("prog_" + e)) for e in ("pe", "act", "dve", "pool")}
        self.pcnt = {e: 0 for e in self.psem}
        self.waited = {e: {} for e in self.E}
        self.dsems = []

    def dsem(self, name):
        d = DSem(self.es.enter_context(self.nc.semaphore(name)))
        self.dsems.append(d)
        return d

    def wait(self, e, tk):
        if tk is None:
            return
        if tk.eng == "pe" and e == "pe":
            return
        k = id(tk.sem)
        if self.waited[e].get(k, 0) >= tk.val:
            return
        self.E[e].wait_ge(tk.sem, tk.val)
        self.waited[e][k] = tk.val

    def _deps(self, e, reads, writes, waw, skipsem=None):
        for b in reads:
            for t in b.w.values():
                self.wait(e, t)
        for b in writes:
            if waw:
                for t in b.w.values():
                    if skipsem is not None and t.sem is skipsem:
                        continue
                    self.wait(e, t)
            for t in b.r.values():
                self.wait(e, t)

    def _commit(self, tk, reads, writes, waw):
        k = id(tk.sem)
        for b in reads:
            o = b.r.get(k)
            if o is None or o.val < tk.val:
                b.r[k] = tk
        for b in writes:
            o = b.w.get(k)
            if o is None or o.val < tk.val:
                b.w[k] = tk

    def op(self, e, fn, reads=(), writes=(), waw=True):
        self._deps(e, reads, writes, waw)
        ins = fn(self.E[e])
        self.pcnt[e] += 1
        ins.then_inc(self.psem[e], 1)
        tk = Tk(self.psem[e], self.pcnt[e], e)
        self._commit(tk, reads, writes, waw)
        return tk

    def dma(self, q, ds, out, in_, reads=(), writes=(), waw=True):
        self._deps(q, reads, writes, waw, skipsem=ds.h)
        ins = self.E[q].dma_start(out=out, in_=in_)
        ds.cnt += 16
        ins.then_inc(ds.h, 16)
        tk = Tk(ds.h, ds.cnt, "dma")
        self._commit(tk, reads, writes, waw)
        return tk

    def barrier(self):
        for e in self.E:
            for p in self.psem:
                if self.pcnt[p] > 0:
                    self.wait(e, Tk(self.psem[p], self.pcnt[p], "x"))
            for d in self.dsems:
                if d.cnt > 0:
                    self.wait(e, Tk(d.h, d.cnt, "dma"))


def build(upto=9, dbg=False):
    nc = bass.Bass("TRN2", target_bir_lowering=False)

    def din(name, shape, dt=F32):
        return nc.dram_tensor(name, shape, dt, kind="ExternalInput").ap()

    def dscr(name, shape, dt):
        return nc.dram_tensor(name, shape, dt, kind=("ExternalOutput" if dbg else "Internal")).ap()

    x = din("x", [T, D])
    cT = din("cT", [128, 16])
    w_ada = din("w_ada", [D, 6 * D])
    b_ada = din("b_ada", [1, 6 * D])
    gfm = din("gfm", [128, 32])
    g_final = din("g_final", [1, D])
    w_in = din("w_in", [D, 10240])
    lam_in = din("lam", [1, 256])
    biasAT = din("biasAT", [8, 128, 5 * 512])
    biasBT = din("biasBT", [8, 128, 8 * 512])
    cvecA = din("cvecA", [128, 8])
    w_up_a = din("w_up_a", [1024, D])
    w_up_b = din("w_up_b", [1024, D])
    w_o = din("w_o", [D, D])
    w_router = din("w_router", [D, NE])
    rbias = din("rbias", [1, NE])
    if upto >= 7:
        wgu = din("wgu", [NE + 1, 4, 128, 4096])
        wd = din("wd", [NE + 1, 512, D])
    out = nc.dram_tensor("out", [T, D], F32, kind="ExternalOutput").ap()

    qkT = [dscr(f"qkT{i}", [1024, T], BF16) for i in range(4)]
    vv = [dscr(f"vv{i}", [T, 1024], BF16) for i in range(2)]
    gT = dscr("gT", [4096, T], BF16)
    h1d = dscr("h1d", [T, D], F32)
    VTd = dscr("VTd", [KC, 128, T], BF16)
    wcTd = dscr("wcTd", [NE, T], F32)
    modsave = dscr("modsave", [2, D], F32)

    with contextlib.suppress(_Stop), contextlib.ExitStack() as es:
        kb = KB(nc, es)

        def phase(n):
            if n > upto:
                kb.barrier()
                raise _Stop()

        uniq = [0]

        def sb(stack, name, shape, dt):
            uniq[0] += 1
            return stack.enter_context(nc.sbuf_tensor(f"{name}_{uniq[0]}", shape, dt))

        def ps(stack, name, shape, dt):
            uniq[0] += 1
            return stack.enter_context(nc.psum_tensor(f"{name}_{uniq[0]}", shape, dt))

        identf = sb(es, "identf", [128, 128], F32)
        identb = sb(es, "identb", [128, 128], BF16)
        ones_f = sb(es, "ones_f", [128, 128], F32)
        B_const = Buf("const")
        kb.op("pool", lambda g: g.memset(identf[:], 1.0), writes=[B_const])
        kb.op("pool", lambda g: g.affine_select(out=identf[:], in_=identf[:], pattern=[[-1, 128]],
                                                compare_op=ALU.is_equal, fill=0.0, base=0,
                                                channel_multiplier=1), reads=[B_const], writes=[B_const])
        kb.op("dve", lambda v: v.tensor_copy(out=identb[:], in_=identf[:]), reads=[B_const], writes=[B_const], waw=False)
        kb.op("dve", lambda v: v.memset(ones_f[:], 1.0), writes=[B_const], waw=False)

        A_a = sb(es, "A_a", [128, 16], F32)
        S_a = sb(es, "S_a", [128, 16], F32)
        A_m = sb(es, "A_m", [128, 16], F32)
        S_m = sb(es, "S_m", [128, 16], F32)
        B_mod = Buf("modfm")

        dq = [kb.dsem(f"dq{i}") for i in range(12)]
        dp = [kb.dsem(f"dp{i}") for i in range(4)]

        with contextlib.ExitStack() as s0:
            cts = sb(s0, "cts", [128, 16], F32)
            scs = sb(s0, "scs", [128, 16], F32)
            gfs = sb(s0, "gfs", [128, 32], F32)
            scb = sb(s0, "scb", [128, 16, 128], F32)
            wab = [sb(s0, f"wab{i}", [128, 16, 512], F32) for i in range(2)]
            bbc = [sb(s0, f"bbc{i}", [128, 512], F32) for i in range(2)]
            modbc = [sb(s0, f"modbc{i}", [128, D], F32) for i in range(6)]
            tmpd = sb(s0, "tmpd", [128, 128], F32)
            fm = sb(s0, "fm", [128, 4, 16], F32)
            pmod = [ps(s0, f"pmod{i}", [128, 512], F32) for i in range(2)]
            B_c = Buf()
            B_scb = Buf()
            B_wab = [Buf(), Buf()]
            B_bbc = [Buf(), Buf()]
            B_pm = [Buf(), Buf()]
            B_modbc = [Buf() for _ in range(6)]
            B_tmp = Buf()
            B_fm = Buf()
            kb.dma("sp", dq[0], cts[:], cT[:, :], writes=[B_c])
            kb.dma("sp", dq[0], gfs[:], gfm[:, :], writes=[B_c])
            kb.op("act", lambda a: a.activation(out=scs[:], in_=cts[:], func=AF.Silu), reads=[B_c], writes=[B_scb])
            for kc in range(KC):
                kb.op("dve", lambda v, kc=kc: v.tensor_scalar(out=scb[:, kc, :], in0=ones_f[:], scalar1=scs[:, kc:kc + 1],
                                                             scalar2=None, op0=ALU.mult),
                      reads=[B_scb, B_const], writes=[B_c], waw=False)
            wav = w_ada.rearrange("(kc p) n -> p kc n", p=128)
            for j in range(24):
                i = j % 2
                kb.dma("sp", dq[1 + i], wab[i][:, 0:8, :], wav[:, 0:8, j * 512:(j + 1) * 512], writes=[B_wab[i]])
                kb.dma("act", dq[1 + i], wab[i][:, 8:16, :], wav[:, 8:16, j * 512:(j + 1) * 512], writes=[B_wab[i]])
                kb.dma("sp", dq[3 + i], bbc[i][:], b_ada[0, j * 512:(j + 1) * 512].partition_broadcast(128),
                       writes=[B_bbc[i]])

                def mm(pe, i=i):
                    r = None
                    for kc in range(KC):
                        r = pe.matmul(pmod[i][:], lhsT=scb[:, kc, :], rhs=wab[i][:, kc, :], start=(kc == 0), stop=(kc == KC - 1))
                    return r
                kb.op("pe", mm, reads=[B_c, B_wab[i]], writes=[B_pm[i]])
                mi, cb = j // 4, (j % 4) * 512
                kb.op("dve", lambda v, i=i, mi=mi, cb=cb: v.tensor_tensor(out=modbc[mi][:, cb:cb + 512], in0=pmod[i][:],
                                                                           in1=bbc[i][:], op=ALU.add),
                      reads=[B_pm[i], B_bbc[i]], writes=[B_modbc[mi]], waw=False)
            for fi, mi in enumerate((0, 1, 3, 4)):
                for kc in range(KC):
                    kb.op("dve", lambda v, mi=mi, kc=kc: v.tensor_tensor(out=tmpd[:], in0=modbc[mi][:, kc * 128:(kc + 1) * 128],
                                                                         in1=identf[:], op=ALU.mult),
                          reads=[B_modbc[mi], B_const], writes=[B_tmp])
                    kb.op("dve", lambda v, fi=fi, kc=kc: v.reduce_sum(out=fm[:, fi, kc:kc + 1], in_=tmpd[:], axis=AX.X),
                          reads=[B_tmp], writes=[B_fm], waw=False)
            kb.op("dve", lambda v: v.tensor_copy(out=S_a[:], in_=fm[:, 0, :]), reads=[B_fm], writes=[B_mod], waw=False)
            kb.op("dve", lambda v: v.tensor_copy(out=S_m[:], in_=fm[:, 2, :]), reads=[B_fm], writes=[B_mod], waw=False)
            kb.op("dve", lambda v: v.scalar_tensor_tensor(out=A_a[:], in0=fm[:, 1, :], scalar=1.0, in1=gfs[:, 0:16],
                                                          op0=ALU.add, op1=ALU.mult), reads=[B_fm, B_c], writes=[B_mod], waw=False)
            kb.op("dve", lambda v: v.scalar_tensor_tensor(out=A_m[:], in0=fm[:, 3, :], scalar=1.0, in1=gfs[:, 16:32],
                                                          op0=ALU.add, op1=ALU.mult), reads=[B_fm, B_c], writes=[B_mod], waw=False)
            B_ms = Buf()
            kb.dma("sp", dq[5], modsave[0:1, :], modbc[2][0:1, :], reads=[B_modbc[2]], writes=[B_ms], waw=False)
            kb.dma("sp", dq[5], modsave[1:2, :], modbc[5][0:1, :], reads=[B_modbc[5]], writes=[B_ms], waw=False)
            kb.barrier()

        def norm_tile(stack_bufs, src_tile, B_src, tt, A_fm, S_fm, dst_bf, B_dst, dst32=None, B_dst16=None):
            junk, ss, rstd, xh, p4, B_junk, B_ss, B_xh, B_p4 = stack_bufs
            kb.op("act", lambda a: a.activation(out=junk[:], in_=src_tile[:], func=AF.Square, accum_out=ss[:]),
                  reads=[B_src], writes=[B_junk, B_ss])
            kb.op("act", lambda a: a.activation(out=rstd[:], in_=ss[:], func=AF.Sqrt, bias=1e-6, scale=1.0 / D),
                  reads=[B_ss], writes=[B_ss])
            kb.op("dve", lambda v: v.reciprocal(out=rstd[:], in_=rstd[:]), reads=[B_ss], writes=[B_ss])
            kb.op("dve", lambda v: v.tensor_scalar(out=xh[:], in0=src_tile[:], scalar1=rstd[:, 0:1], scalar2=None, op0=ALU.mult),
                  reads=[B_src, B_ss], writes=[B_xh])

            def tr(pe):
                r = None
                for kc in range(KC):
                    r = pe.transpose(out=p4[:, kc * 128:(kc + 1) * 128], in_=xh[:, kc * 128:(kc + 1) * 128], identity=identf[:])
                return r
            kb.op("pe", tr, reads=[B_xh, B_const], writes=[B_p4])
            for kc in range(KC):
                o = dst32(kc) if dst32 is not None else dst_bf(kc)
                if kc < 8:
                    kb.op("act", lambda a, kc=kc, o=o: a.activation(out=o, in_=p4[:, kc * 128:(kc + 1) * 128], func=AF.Identity,
                                                                    bias=S_fm[:, kc:kc + 1], scale=A_fm[:, kc:kc + 1]),
                          reads=[B_p4, B_mod], writes=[B_dst], waw=False)
                else:
                    kb.op("dve", lambda v, kc=kc, o=o: v.tensor_scalar(out=o, in0=p4[:, kc * 128:(kc + 1) * 128],
                                                                       scalar1=A_fm[:, kc:kc + 1], scalar2=S_fm[:, kc:kc + 1],
                                                                       op0=ALU.mult, op1=ALU.add),
                          reads=[B_p4, B_mod], writes=[B_dst], waw=False)
            if dst32 is not None:
                kb.op("pool", lambda g: g.tensor_copy(out=dst_bf(None), in_=dst32(None)), reads=[B_dst], writes=[B_dst16])

        with contextlib.ExitStack() as s1:
            uT = sb(s1, "uT", [128, KC, T], BF16)
            B_uT = Buf("uT")
            phase(1)
            with contextlib.ExitStack() as s1a:
                xt = [sb(s1a, f"xt{i}", [128, D], F32) for i in range(2)]
                junk = sb(s1a, "junk", [128, D], BF16)
                ss = sb(s1a, "ss", [128, 1], F32)
                rstd = sb(s1a, "rstd", [128, 1], F32)
                xh = sb(s1a, "xh", [128, D], F32)
                p4 = ps(s1a, "p4", [128, D], F32)
                B_xt = [Buf(), Buf()]
                nb = (junk, ss, rstd, xh, p4, Buf(), Buf(), Buf(), Buf())
                for tt in range(int(os.environ.get('NT1', '16'))):
                    i = tt % 2
                    kb.dma("sp", dq[i], xt[i][:], x[tt * 128:(tt + 1) * 128, :], writes=[B_xt[i]])
                    norm_tile(nb, xt[i], B_xt[i], tt, A_a, S_a,
                              lambda kc, tt=tt: uT[:, kc, tt * 128:(tt + 1) * 128], B_uT)
                kb.barrier()

            phase(2)
            with contextlib.ExitStack() as s2:
                wb = [sb(s2, f"wb{i}", [128, KC, 512], BF16) for i in range(2)]
                B_wb = [Buf(), Buf()]
                stg = [sb(s2, f"stg{i}", [128, 512], BF16) for i in range(4)]
                B_stg = [Buf() for _ in range(4)]
                pb = [ps(s2, f"pb{i}", [128, 512], F32) for i in range(4)]
                B_pb = [Buf() for _ in range(4)]
                B_scr = Buf("scratchA2")
                wiv = w_in.rearrange("(kc p) n -> p kc n", p=128)
                cnt = [0]

                def evac_store(pi, kind, scale, dst_ap):
                    n = cnt[0]
                    cnt[0] += 1
                    si = n % 4
                    if kind == "sig":
                        kb.op("act", lambda a: a.activation(out=stg[si][:], in_=pb[pi][:], func=AF.Sigmoid),
                              reads=[B_pb[pi]], writes=[B_stg[si]])
                    elif n % 2 == 0:
                        kb.op("act", lambda a: a.activation(out=stg[si][:], in_=pb[pi][:], func=AF.Identity, scale=float(scale)),
                              reads=[B_pb[pi]], writes=[B_stg[si]])
                    else:
                        kb.op("dve", lambda v: v.tensor_scalar(out=stg[si][:], in0=pb[pi][:], scalar1=float(scale), scalar2=None,
                                                               op0=ALU.mult),
                              reads=[B_pb[pi]], writes=[B_stg[si]])
                    kb.dma("sp", dq[4 + si], dst_ap, stg[si][:], reads=[B_stg[si]], writes=[B_scr], waw=False)

                groups = []
                for g in range(20):
                    if g < 2:
                        groups.append(("fm", qkT[0], g * 512, 0.125, "lin"))
                    elif g < 4:
                        groups.append(("fm", qkT[1], (g - 2) * 512, 1.0, "lin"))
                    elif g < 6:
                        groups.append(("tm", vv[0], (g - 4) * 512, 1.0, "lin"))
                    elif g < 8:
                        groups.append(("fm", qkT[2], (g - 6) * 512, 128.0 ** -0.5, "lin"))
                    elif g < 10:
                        groups.append(("fm", qkT[3], (g - 8) * 512, 1.0, "lin"))
                    elif g < 12:
                        groups.append(("tm", vv[1], (g - 10) * 512, 1.0, "lin"))
                    else:
                        groups.append(("fm", gT, (g - 12) * 512, 1.0, "sig"))
                pcount = 0
                for g in range(20):
                    i = g % 2
                    kind, dst, base, scale, ev = groups[g]
                    kb.dma("pool", dp[i], wb[i][:, 0:8, :], wiv[:, 0:8, g * 512:(g + 1) * 512], writes=[B_wb[i]])
                    kb.dma("pool", dp[i], wb[i][:, 8:16, :], wiv[:, 8:16, g * 512:(g + 1) * 512], writes=[B_wb[i]])
                    if kind == "fm":
                        for m in range(4):
                            for tb in range(4):
                                pi = pcount % 4
                                pcount += 1

                                def mm(pe, i=i, m=m, tb=tb, pi=pi):
                                    r = None
                                    for kc in range(KC):
                                        r = pe.matmul(pb[pi][:], lhsT=wb[i][:, kc, m * 128:(m + 1) * 128],
                                                      rhs=uT[:, kc, tb * 512:(tb + 1) * 512], start=(kc == 0), stop=(kc == KC - 1))
                                    return r
                                kb.op("pe", mm, reads=[B_wb[i], B_uT], writes=[B_pb[pi]])
                                evac_store(pi, ev, scale, dst[base + m * 128: base + (m + 1) * 128, tb * 512:(tb + 1) * 512])
                    else:
                        for tt in range(16):
                            pi = pcount % 4
                            pcount += 1

                            def mm(pe, i=i, tt=tt, pi=pi):
                                r = None
                                for kc in range(KC):
                                    r = pe.matmul(pb[pi][:], lhsT=uT[:, kc, tt * 128:(tt + 1) * 128], rhs=wb[i][:, kc, :],
                                                  start=(kc == 0), stop=(kc == KC - 1))
                                return r
                            kb.op("pe", mm, reads=[B_wb[i], B_uT], writes=[B_pb[pi]])
                            evac_store(pi, ev, scale, dst[tt * 128:(tt + 1) * 128, base:base + 512])
                kb.barrier()

        phase(3)
        with contextlib.ExitStack() as s3:
            yT = [sb(s3, f"yT{i}", [128, 8, T], BF16) for i in range(2)]
            B_yT = [Buf(), Buf()]
            with contextlib.ExitStack() as s3a:
                qTh = [sb(s3a, f"qTh{i}", [128, T], BF16) for i in range(2)]
                kTh = [sb(s3a, f"kTh{i}", [128, T], BF16) for i in range(2)]
                vh = [sb(s3a, f"vh{i}", [128, 16, 128], BF16) for i in range(2)]
                Rh = [sb(s3a, f"Rh{i}", [128, 8 * 512], BF16) for i in range(2)]
                B_hd = [Buf(), Buf()]
                cA = sb(s3a, "cA", [128, 8], F32)
                ones_b = sb(s3a, "ones_b", [128, 128], BF16)
                lq = sb(s3a, "lq", [128, 256], F32)
                ltmp = sb(s3a, "ltmp", [128, 64], F32)
                lst = sb(s3a, "lst", [128, 4], F32)
                nl = sb(s3a, "nl", [128, 1], F32)
                B_l = Buf()
                NPB = 3
                PTs = [sb(s3a, f"PTs{i}", [128, 512], BF16) for i in range(NPB)]
                B_PTs = [Buf() for _ in range(NPB)]
                rden = [sb(s3a, f"rden{i}", [128, 512], F32) for i in range(2)]
                B_rden = [Buf(), Buf()]
                t0s = sb(s3a, "t0s", [128, 512], F32)
                oos = sb(s3a, "oos", [128, 512], F32)
                sqs = sb(s3a, "sqs", [128, 512], BF16)
                stds = sb(s3a, "stds", [128, 512], F32)
                B_t0, B_oo, B_sq, B_std = Buf(), Buf(), Buf(), Buf()
                ST = [ps(s3a, f"ST{i}", [128, 512], F32) for i in range(NPB)]
                B_ST = [Buf() for _ in range(NPB)]
                ACC = [ps(s3a, f"ACC{i}", [128, 512], F32) for i in range(2)]
                DEN = [ps(s3a, f"DEN{i}", [128, 512], F32) for i in range(2)]
                B_ACC = [Buf(), Buf()]
                B_DEN = [Buf(), Buf()]
                SSQ = ps(s3a, "SSQ", [128, 512], F32)
                B_SSQ = Buf()

                kb.op("dve", lambda v: v.memset(ones_b[:], 1.0), writes=[B_l])
                kb.dma("sp", dq[8], cA[:], cvecA[:, :], writes=[B_l], waw=False)
                kb.dma("sp", dq[8], lq[:], lam_in[0, :].partition_broadcast(128), writes=[B_l], waw=False)
                for j in range(2):
                    kb.op("dve", lambda v, j=j: v.tensor_tensor(out=ltmp[:], in0=lq[:, j * 128:j * 128 + 64],
                                                                in1=lq[:, j * 128 + 64:j * 128 + 128], op=ALU.mult),
                          reads=[B_l], writes=[B_l])
                    kb.op("dve", lambda v, j=j: v.reduce_sum(out=lst[:, j:j + 1], in_=ltmp[:], axis=AX.X), reads=[B_l], writes=[B_l])
                kb.op("act", lambda a: a.activation(out=lst[:, 2:4], in_=lst[:, 0:2], func=AF.Exp), reads=[B_l], writes=[B_l])
                lam_init = 0.8 - 0.6 * math.exp(-0.3 * 0)
                kb.op("dve", lambda v: v.scalar_tensor_tensor(out=nl[:], in0=lst[:, 3:4], scalar=-lam_init, in1=lst[:, 2:3],
                                                              op0=ALU.add, op1=ALU.subtract), reads=[B_l], writes=[B_l])

                pcnt_ = [0]
                gcnt_ = [0]
                for which in range(2):
                    nmaps = 2 if which == 0 else 1
                    dk = 64 if which == 0 else 128
                    omin = -1 if which == 0 else -4
                    ntile = 5 if which == 0 else 8
                    bsrc = biasAT if which == 0 else biasBT
                    qd, kd, vd = qkT[2 * which], qkT[2 * which + 1], vv[which]
                    vdv = vd.rearrange("(tt p) c -> p tt c", p=128)
                    for h in range(8):
                        hi = (which * 8 + h) % 2
                        kb.dma("sp", dq[hi], qTh[hi][:], qd[h * 128:(h + 1) * 128, :], writes=[B_hd[hi]], waw=False)
                        kb.dma("sp", dq[hi], kTh[hi][:], kd[h * 128:(h + 1) * 128, :], writes=[B_hd[hi]], waw=False)
                        kb.dma("act", dq[hi], vh[hi][:], vdv[:, :, h * 128:(h + 1) * 128], writes=[B_hd[hi]], waw=False)
                        for half_ in range(2):
                            hw_ = ntile * 512 // 2
                            kb.dma("pool", dp[hi], Rh[hi][:, half_ * hw_:(half_ + 1) * hw_], bsrc[h, :, half_ * hw_:(half_ + 1) * hw_],
                                   writes=[B_hd[hi]], waw=False)
                        for g in range(4):
                            for m in range(nmaps):
                                gi = gcnt_[0] % 2
                                gcnt_[0] += 1
                                kb_lo = 0 if which == 0 else max(0, 4 * g - 4)
                                kbs = list(range(kb_lo, 4 * g + 4))
                                pend = []

                                def emit_pv(item, gi=gi, hi=hi):
                                    kbi, pi, c0, first, last = item

                                    def pv(pe):
                                        pe.matmul(ACC[gi][:, c0:512], lhsT=vh[hi][:, kbi, :], rhs=PTs[pi][:, c0:512], start=first, stop=last)
                                        return pe.matmul(DEN[gi][:, c0:512], lhsT=ones_b[:], rhs=PTs[pi][:, c0:512], start=first, stop=last)
                                    kb.op("pe", pv, reads=[B_PTs[pi], B_hd[hi], B_l], writes=[B_ACC[gi], B_DEN[gi]])

                                for idx, kbi in enumerate(kbs):
                                    o = kbi - 4 * g
                                    pi = pcnt_[0] % NPB
                                    pcnt_[0] += 1
                                    c0 = max(0, o) * 128
                                    near = (o >= omin)

                                    def qk(pe, hi=hi, m=m, g=g, kbi=kbi, o=o, pi=pi, c0=c0, near=near):
                                        r = pe.matmul(ST[pi][:, c0:512], lhsT=kTh[hi][m * dk:(m + 1) * dk, kbi * 128:(kbi + 1) * 128],
                                                      rhs=qTh[hi][m * dk:(m + 1) * dk, g * 512 + c0:(g + 1) * 512], start=True, stop=(not near))
                                        if near:
                                            t_ = o - omin
                                            r = pe.matmul(ST[pi][:, c0:512], lhsT=identb[:], rhs=Rh[hi][:, t_ * 512 + c0:(t_ + 1) * 512],
                                                          start=False, stop=True)
                                        return r
                                    kb.op("pe", qk, reads=[B_hd[hi], B_const], writes=[B_ST[pi]])
                                    if near:
                                        kb.op("act", lambda a, pi=pi, c0=c0: a.activation(out=PTs[pi][:, c0:512], in_=ST[pi][:, c0:512], func=AF.Exp),
                                              reads=[B_ST[pi]], writes=[B_PTs[pi]])
                                    else:
                                        kb.op("act", lambda a, pi=pi, c0=c0, h=h: a.activation(out=PTs[pi][:, c0:512], in_=ST[pi][:, c0:512], func=AF.Exp,
                                                                                               bias=cA[:, h:h + 1], scale=1.0),
                                              reads=[B_ST[pi], B_l], writes=[B_PTs[pi]])
                                    pend.append((kbi, pi, c0, idx == 0, idx == len(kbs) - 1))
                                    if len(pend) > 2:
                                        emit_pv(pend.pop(0))
                                while pend:
                                    emit_pv(pend.pop(0))
                                qs = slice(g * 512, (g + 1) * 512)
                                kb.op("dve", lambda v, gi=gi: v.reciprocal(out=rden[gi][:], in_=DEN[gi][:]), reads=[B_DEN[gi]], writes=[B_rden[gi]])
                                if which == 1:
                                    kb.op("dve", lambda v, gi=gi, h=h, qs=qs: v.tensor_tensor(out=yT[1][:, h, qs], in0=ACC[gi][:], in1=rden[gi][:], op=ALU.mult),
                                          reads=[B_ACC[gi], B_rden[gi]], writes=[B_yT[1]], waw=False)
                                elif m == 0:
                                    kb.op("dve", lambda v, gi=gi: v.tensor_tensor(out=t0s[:], in0=ACC[gi][:], in1=rden[gi][:], op=ALU.mult),
                                          reads=[B_ACC[gi], B_rden[gi]], writes=[B_t0])
                                else:
                                    kb.op("dve", lambda v, gi=gi: v.tensor_tensor(out=oos[:], in0=ACC[gi][:], in1=rden[gi][:], op=ALU.mult),
                                          reads=[B_ACC[gi], B_rden[gi]], writes=[B_oo])
                                    kb.op("dve", lambda v: v.scalar_tensor_tensor(out=oos[:], in0=oos[:], scalar=nl[:, 0:1], in1=t0s[:],
                                                                                  op0=ALU.mult, op1=ALU.add),
                                          reads=[B_oo, B_t0, B_l], writes=[B_oo])
                                    kb.op("dve", lambda v: v.tensor_tensor(out=sqs[:], in0=oos[:], in1=oos[:], op=ALU.mult), reads=[B_oo], writes=[B_sq])
                                    kb.op("pe", lambda pe: pe.matmul(SSQ[:], lhsT=ones_b[:], rhs=sqs[:], start=True, stop=True),
                                          reads=[B_sq, B_l], writes=[B_SSQ])
                                    kb.op("act", lambda a: a.activation(out=stds[:], in_=SSQ[:], func=AF.Sqrt, bias=1e-5, scale=1.0 / 128),
                                          reads=[B_SSQ], writes=[B_std])
                                    kb.op("dve", lambda v: v.reciprocal(out=stds[:], in_=stds[:]), reads=[B_std], writes=[B_std])
                                    kb.op("dve", lambda v, h=h, qs=qs: v.scalar_tensor_tensor(out=yT[0][:, h, qs], in0=oos[:], scalar=float(1.0 - lam_init),
                                                                                              in1=stds[:], op0=ALU.mult, op1=ALU.mult),
                                          reads=[B_oo, B_std], writes=[B_yT[0]], waw=False)
                kb.barrier()

            phase(4)
            with contextlib.ExitStack() as s4:
                mT = sb(s4, "mT", [128, KC, T], BF16)
                B_mT = Buf("mT")
                with contextlib.ExitStack() as s4a:
                    wua = [sb(s4a, f"wua{i}", [128, 8, 256], BF16) for i in range(2)]
                    wub = [sb(s4a, f"wub{i}", [128, 8, 256], BF16) for i in range(2)]
                    B_wu = [Buf(), Buf()]
                    gat = [sb(s4a, f"gat{i}", [128, 512], BF16) for i in range(2)]
                    gbt = [sb(s4a, f"gbt{i}", [128, 512], BF16) for i in range(2)]
                    B_gt = [Buf(), Buf()]
                    m1 = [sb(s4a, f"m1{i}", [128, 512], F32) for i in range(2)]
                    m2 = [sb(s4a, f"m2{i}", [128, 512], F32) for i in range(2)]
                    B_m = [Buf(), Buf()]
                    pa = [ps(s4a, f"pa{i}", [128, 512], F32) for i in range(2)]
                    pbb = [ps(s4a, f"pbb{i}", [128, 512], F32) for i in range(2)]
                    B_pab = [Buf(), Buf()]
                    wuav = w_up_a.rearrange("(kc p) n -> p kc n", p=128)
                    wubv = w_up_b.rearrange("(kc p) n -> p kc n", p=128)
                    n = 0
                    for dg in range(8):
                        wi = dg % 2
                        kb.dma("pool", dp[wi], wua[wi][:], wuav[:, :, dg * 256:(dg + 1) * 256], writes=[B_wu[wi]])
                        kb.dma("pool", dp[wi], wub[wi][:], wubv[:, :, dg * 256:(dg + 1) * 256], writes=[B_wu[wi]])
                        for sub in range(2):
                            dc = dg * 2 + sub
                            for tb in range(4):
                                i = n % 2
                                n += 1
                                kb.dma("sp", dq[2 + i], gat[i][:], gT[dc * 128:(dc + 1) * 128, tb * 512:(tb + 1) * 512], writes=[B_gt[i]])
                                kb.dma("sp", dq[2 + i], gbt[i][:], gT[2048 + dc * 128:2048 + (dc + 1) * 128, tb * 512:(tb + 1) * 512],
                                       writes=[B_gt[i]])

                                def mm(pe, wi=wi, sub=sub, tb=tb, i=i):
                                    r = None
                                    for kc in range(8):
                                        pe.matmul(pa[i][:], lhsT=wua[wi][:, kc, sub * 128:(sub + 1) * 128],
                                                  rhs=yT[0][:, kc, tb * 512:(tb + 1) * 512], start=(kc == 0), stop=(kc == 7))
                                    for kc in range(8):
                                        r = pe.matmul(pbb[i][:], lhsT=wub[wi][:, kc, sub * 128:(sub + 1) * 128],
                                                      rhs=yT[1][:, kc, tb * 512:(tb + 1) * 512], start=(kc == 0), stop=(kc == 7))
                                    return r
                                kb.op("pe", mm, reads=[B_wu[wi], B_yT[0], B_yT[1]], writes=[B_pab[i]])
                                kb.op("dve", lambda v, i=i: v.tensor_tensor(out=m1[i][:], in0=pa[i][:], in1=gat[i][:], op=ALU.mult),
                                      reads=[B_pab[i], B_gt[i]], writes=[B_m[i]])
                                kb.op("dve", lambda v, i=i: v.tensor_tensor(out=m2[i][:], in0=pbb[i][:], in1=gbt[i][:], op=ALU.mult),
                                      reads=[B_pab[i], B_gt[i]], writes=[B_m[i]], waw=False)
                                kb.op("dve", lambda v, i=i, dc=dc, tb=tb: v.tensor_tensor(out=mT[:, dc, tb * 512:(tb + 1) * 512], in0=m1[i][:],
                                                                                          in1=m2[i][:], op=ALU.add),
                                      reads=[B_m[i]], writes=[B_mT], waw=False)
                    kb.barrier()

                phase(5)
                with contextlib.ExitStack() as s4b:
                    wo = [sb(s4b, f"wo{i}", [128, KC, 512], BF16) for i in range(2)]
                    B_wo = [Buf(), Buf()]
                    gtA = sb(s4b, "gtA", [128, D], F32)
                    B_gtA = Buf()
                    xp = [sb(s4b, f"xp{i}", [128, 512], F32) for i in range(3)]
                    B_xp = [Buf() for _ in range(3)]
                    hp = [sb(s4b, f"hp{i}", [128, 512], F32) for i in range(3)]
                    B_hp = [Buf() for _ in range(3)]
                    po = [ps(s4b, f"po{i}", [128, 512], F32) for i in range(3)]
                    B_po = [Buf() for _ in range(3)]
                    B_h1 = Buf("h1d")
                    kb.dma("sp", dq[8], gtA[:], modsave[0, :].partition_broadcast(128), writes=[B_gtA])
                    wov = w_o.rearrange("(kc p) n -> p kc n", p=128)
                    n = 0
                    for ob in range(4):
                        wi = ob % 2
                        kb.dma("pool", dp[wi], wo[wi][:, 0:8, :], wov[:, 0:8, ob * 512:(ob + 1) * 512], writes=[B_wo[wi]])
                        kb.dma("pool", dp[wi], wo[wi][:, 8:16, :], wov[:, 8:16, ob * 512:(ob + 1) * 512], writes=[B_wo[wi]])
                        for tt in range(16):
                            i = n % 3
                            n += 1
                            kb.dma("sp", dq[2 + i], xp[i][:], x[tt * 128:(tt + 1) * 128, ob * 512:(ob + 1) * 512], writes=[B_xp[i]])

                            def mm(pe, wi=wi, tt=tt, i=i):
                                r = None
                                for kc in range(KC):
                                    r = pe.matmul(po[i][:], lhsT=mT[:, kc, tt * 128:(tt + 1) * 128], rhs=wo[wi][:, kc, :],
                                                  start=(kc == 0), stop=(kc == KC - 1))
                                return r
                            kb.op("pe", mm, reads=[B_wo[wi], B_mT], writes=[B_po[i]])
                            kb.op("dve", lambda v, i=i, ob=ob: v.tensor_tensor(out=hp[i][:], in0=po[i][:], in1=gtA[:, ob * 512:(ob + 1) * 512],
                                                                               op=ALU.mult),
                                  reads=[B_po[i], B_gtA], writes=[B_hp[i]])
                            kb.op("dve", lambda v, i=i: v.tensor_tensor(out=hp[i][:], in0=hp[i][:], in1=xp[i][:], op=ALU.add),
                                  reads=[B_hp[i], B_xp[i]], writes=[B_hp[i]])
                            kb.dma("act", dq[5 + i], h1d[tt * 128:(tt + 1) * 128, ob * 512:(ob + 1) * 512], hp[i][:],
                                   reads=[B_hp[i]], writes=[B_h1], waw=False)
                    kb.barrier()

        phase(6)
        with contextlib.ExitStack() as s5:
            h1t = [sb(s5, f"h1t{i}", [128, D], F32) for i in range(2)]
            B_h1t = [Buf(), Buf()]
            junk = sb(s5, "junk5", [128, D], BF16)
            ss = sb(s5, "ss5", [128, 1], F32)
            rstd = sb(s5, "rstd5", [128, 1], F32)
            xh = sb(s5, "xh5", [128, D], F32)
            p4 = ps(s5, "p45", [128, D], F32)
            nb = (junk, ss, rstd, xh, p4, Buf(), Buf(), Buf(), Buf())
            v16 = [sb(s5, f"v16{i}", [128, KC, 128], BF16) for i in range(2)]
            v32 = [sb(s5, f"v32{i}", [128, KC, 128], F32) for i in range(2)]
            B_v = [Buf(), Buf()]
            B_v16 = [Buf(), Buf()]
            wr = sb(s5, "wr", [128, KC, NE], F32)
            rb = sb(s5, "rb", [128, NE], F32)
            B_wr = Buf()
            plg_ = ps(s5, "plg", [128, 512], F32)
            plg = plg_[:, 0:NE]
            pwt_ = ps(s5, "pwt", [128, 512], F32)
            pwt = pwt_[0:NE, 0:128]
            B_plg, B_pwt = Buf(), Buf()
            sc = sb(s5, "sc", [128, NE], F32)
            bi = sb(s5, "bi", [128, NE], F32)
            t8 = sb(s5, "t8", [128, 8, 8], F32)
            gs = sb(s5, "gs", [128, 8], F32)
            g8 = sb(s5, "g8", [128, 8], F32)
            gm = sb(s5, "gm", [128, 8], F32)
            mb = sb(s5, "mb", [128, 8], F32)
            msk = sb(s5, "msk", [128, NE], F32)
            m8 = sb(s5, "m8", [128, 8], F32)
            sel = sb(s5, "sel", [128, NE], F32)
            den = sb(s5, "den", [128, 1], F32)
            wc = sb(s5, "wc", [128, NE], F32)
            wcs = [sb(s5, f"wcs{i}", [NE, 128], F32) for i in range(2)]
            B_wcs = [Buf(), Buf()]
            B_rt = Buf()
            B_VTd, B_wcTd = Buf(), Buf()
            kb.dma("sp", dq[8], wr[:], w_router.rearrange("(kc p) n -> p kc n", p=128), writes=[B_wr])
            kb.dma("sp", dq[8], rb[:], rbias[0, :].partition_broadcast(128), writes=[B_wr])
            kb.dma("sp", dq[0], h1t[0][:], h1d[0:128, :], writes=[B_h1t[0]])
            for tt in range(16):
                i = tt % 2
                if tt + 1 < 16:
                    kb.dma("sp", dq[1 - i], h1t[1 - i][:], h1d[(tt + 1) * 128:(tt + 2) * 128, :], writes=[B_h1t[1 - i]])
                norm_tile(nb, h1t[i], B_h1t[i], tt, A_m, S_m,
                          lambda kc, i=i: (v16[i][:] if kc is None else v16[i][:, kc, :]), B_v[i],
                          dst32=lambda kc, i=i: (v32[i][:] if kc is None else v32[i][:, kc, :]), B_dst16=B_v16[i])
                kb.dma("pool", dp[i], VTd[:, :, tt * 128:(tt + 1) * 128].rearrange("k p t -> p k t"), v16[i][:],
                       reads=[B_v16[i]], writes=[B_VTd], waw=False)

                def mm(pe, i=i):
                    r = None
                    for kc in range(KC):
                        r = pe.matmul(plg[:], lhsT=v32[i][:, kc, :], rhs=wr[:, kc, :], start=(kc == 0), stop=(kc == KC - 1))
                    return r
                kb.op("pe", mm, reads=[B_v[i], B_wr], writes=[B_plg])
                R = [B_rt]
                kb.op("act", lambda a: a.activation(out=sc[:], in_=plg[:], func=AF.Sigmoid), reads=[B_plg], writes=R)
                kb.op("dve", lambda v: v.tensor_tensor(out=bi[:], in0=sc[:], in1=rb[:], op=ALU.add), reads=R + [B_wr], writes=R)
                for g in range(8):
                    kb.op("dve", lambda v, g=g: v.max(out=t8[:, g, :], in_=bi[:, g * 8:(g + 1) * 8]), reads=R, writes=R)
                kb.op("dve", lambda v: v.tensor_tensor(out=gs[:], in0=t8[:, :, 0], in1=t8[:, :, 1], op=ALU.add), reads=R, writes=R)
                kb.op("dve", lambda v: v.max(out=g8[:], in_=gs[:]), reads=R, writes=R)
                kb.op("dve", lambda v: v.tensor_scalar(out=gm[:], in0=gs[:], scalar1=g8[:, 3:4], scalar2=None, op0=ALU.is_ge),
                      reads=R, writes=R)
                kb.op("dve", lambda v: v.tensor_scalar(out=mb[:], in0=gm[:], scalar1=-1.0, scalar2=1e9, op0=ALU.add, op1=ALU.mult),
                      reads=R, writes=R)
                for g in range(8):
                    kb.op("dve", lambda v, g=g: v.tensor_scalar(out=msk[:, g * 8:(g + 1) * 8], in0=bi[:, g * 8:(g + 1) * 8],
                                                               scalar1=gm[:, g:g + 1], scalar2=mb[:, g:g + 1], op0=ALU.mult, op1=ALU.add),
                          reads=R, writes=R)
                kb.op("dve", lambda v: v.max(out=m8[:], in_=msk[:]), reads=R, writes=R)
                kb.op("dve", lambda v: v.tensor_scalar(out=sel[:], in0=msk[:], scalar1=m8[:, 7:8], scalar2=None, op0=ALU.is_ge),
                      reads=R, writes=R)
                kb.op("dve", lambda v: v.tensor_tensor(out=sel[:], in0=sel[:], in1=sc[:], op=ALU.mult), reads=R, writes=R)
                kb.op("dve", lambda v: v.reduce_sum(out=den[:], in_=sel[:], axis=AX.X), reads=R, writes=R)
                kb.op("dve", lambda v: v.reciprocal(out=den[:], in_=den[:]), reads=R, writes=R)
                kb.op("dve", lambda v: v.tensor_scalar(out=wc[:], in0=sel[:], scalar1=den[:, 0:1], scalar2=2.5, op0=ALU.mult, op1=ALU.mult),
                      reads=R, writes=R)
                kb.op("pe", lambda pe: pe.transpose(out=pwt[:], in_=wc[:], identity=identf[:]), reads=R + [B_const], writes=[B_pwt])
                kb.op("act", lambda a, i=i: a.copy(out=wcs[i][:], in_=pwt[:]), reads=[B_pwt], writes=[B_wcs[i]])
                kb.dma("pool", dp[2 + i], wcTd[:, tt * 128:(tt + 1) * 128], wcs[i][:], reads=[B_wcs[i]], writes=[B_wcTd], waw=False)
            kb.barrier()

        phase(7)
        for pz in range(2):
            t0 = pz * 1024
            with contextlib.ExitStack() as s6:
                acc = sb(s6, "acc", [128, 8, D], F32)
                B_acc = [Buf() for _ in range(8)]
                with contextlib.ExitStack() as s6a:
                    vt = sb(s6a, "vt", [128, KC, 1024], BF16)
                    B_vt = Buf()
                    wct = sb(s6a, "wct", [NE, 1024], F32)
                    B_wct = Buf()
                    gu = [sb(s6a, f"gu{i}", [128, 2, KC, 128], BF16) for i in range(3)]
                    B_gu = [Buf() for _ in range(3)]
                    dd = sb(s6a, "dd", [128, 4, D], BF16)
                    B_dd = Buf()
                    hT = [sb(s6a, f"hT{i}", [128, 4, 1024], BF16) for i in range(2)]
                    B_hT = [Buf(), Buf()]
                    sg = [sb(s6a, f"sg{i}", [128, 512], F32) for i in range(2)]
                    sgw = [sb(s6a, f"sgw{i}", [128, 512], F32) for i in range(2)]
                    B_sg = [Buf(), Buf()]
                    B_sgw = [Buf(), Buf()]
                    wbc = [sb(s6a, f"wbc{i}", [128, 1024], F32) for i in range(2)]
                    B_wbc = [Buf(), Buf()]
                    sl = [sb(s6a, f"sl{i}", [NE, 128], F32) for i in range(2)]
                    B_sl = [Buf(), Buf()]
                    pg = [ps(s6a, f"pg{i}", [128, 512], F32) for i in range(2)]
                    pu = [ps(s6a, f"pu{i}", [128, 512], F32) for i in range(2)]
                    B_pgu = [Buf(), Buf()]
                    py = [ps(s6a, f"py{i}", [128, 512], F32) for i in range(3)]
                    B_py = [Buf() for _ in range(3)]
                    pw = ps(s6a, "pw", [128, 512], F32)
                    B_pw = Buf()

                    for kc4 in range(4):
                        kb.dma("sp", dq[8], vt[:, kc4 * 4:(kc4 + 1) * 4, :],
                               VTd[kc4 * 4:(kc4 + 1) * 4, :, t0:t0 + 1024].rearrange("k p t -> p k t"), writes=[B_vt])
                    kb.dma("sp", dq[9], wct[:], wcTd[:, t0:t0 + 1024], writes=[B_wct])

                    NU = (NE + 1) * 4

                    def load_gu(u):
                        e, c = divmod(u, 4)
                        bi_ = u % 3
                        kb.dma("pool", dp[bi_], gu[bi_][:].rearrange("p g k m -> p g (k m)"),
                               wgu[e, c].rearrange("p (g j) -> p g j", g=2), writes=[B_gu[bi_]])

                    def load_d(e):
                        for c in range(4):
                            kb.dma("pool", dp[3], dd[:, c, :], wd[e, c * 128:(c + 1) * 128, :], writes=[B_dd])

                    load_gu(0)
                    load_gu(1)
                    load_d(0)
                    ny = [0]

                    def emit_sl(e):
                        si = e % 2
                        if e < NE:
                            kb.op("dve", lambda v: v.tensor_scalar(out=sl[si][:], in0=ones_f[0:NE, :], scalar1=identf[0:NE, e:e + 1],
                                                                   scalar2=None, op0=ALU.mult),
                                  reads=[B_const], writes=[B_sl[si]])
                        else:
                            kb.op("dve", lambda v: v.memset(wbc[si][:], 1.0), writes=[B_wbc[si]])

                    def emit_wbc(e, j):
                        si = e % 2
                        if e >= NE:
                            return
                        kb.op("pe", lambda pe: pe.matmul(pw[:], lhsT=sl[si][:], rhs=wct[:, j * 512:(j + 1) * 512], start=True, stop=True),
                              reads=[B_sl[si], B_wct], writes=[B_pw])
                        kb.op("act", lambda a: a.copy(out=wbc[si][:, j * 512:(j + 1) * 512], in_=pw[:]),
                              reads=[B_pw], writes=[B_wbc[si]], waw=(j == 0))

                    def emit_gu(e, c):
                        u = e * 4 + c
                        bi_ = u % 3
                        wi = e % 2
                        for tb in range(2):
                            i = (u * 2 + tb) % 2

                            def mm(pe, bi_=bi_, tb=tb, i=i):
                                r = None
                                for kc in range(KC):
                                    pe.matmul(pg[i][:], lhsT=gu[bi_][:, 0, kc, :], rhs=vt[:, kc, tb * 512:(tb + 1) * 512],
                                              start=(kc == 0), stop=(kc == KC - 1))
                                for kc in range(KC):
                                    r = pe.matmul(pu[i][:], lhsT=gu[bi_][:, 1, kc, :], rhs=vt[:, kc, tb * 512:(tb + 1) * 512],
                                                  start=(kc == 0), stop=(kc == KC - 1))
                                return r
                            kb.op("pe", mm, reads=[B_gu[bi_], B_vt], writes=[B_pgu[i]])
                            kb.op("act", lambda a, i=i: a.activation(out=sg[i][:], in_=pg[i][:], func=AF.Silu),
                                  reads=[B_pgu[i]], writes=[B_sg[i]])
                            kb.op("dve", lambda v, i=i, tb=tb: v.tensor_tensor(out=sgw[i][:], in0=sg[i][:], in1=wbc[wi][:, tb * 512:(tb + 1) * 512],
                                                                               op=ALU.mult),
                                  reads=[B_sg[i], B_wbc[wi]], writes=[B_sgw[i]])
                            kb.op("dve", lambda v, i=i, tb=tb: v.tensor_tensor(out=hT[wi][:, c, tb * 512:(tb + 1) * 512], in0=pu[i][:],
                                                                               in1=sgw[i][:], op=ALU.mult),
                                  reads=[B_pgu[i], B_sgw[i]], writes=[B_hT[wi]], waw=False)

                    def emit_down(e):
                        wi = e % 2
                        for tt in range(8):
                            for ob in range(4):
                                yi = ny[0] % 3
                                ny[0] += 1

                                def mmd(pe, tt=tt, ob=ob, yi=yi):
                                    r = None
                                    for c in range(4):
                                        r = pe.matmul(py[yi][:], lhsT=hT[wi][:, c, tt * 128:(tt + 1) * 128], rhs=dd[:, c, ob * 512:(ob + 1) * 512],
                                                      start=(c == 0), stop=(c == 3))
                                    return r
                                kb.op("pe", mmd, reads=[B_hT[wi], B_dd], writes=[B_py[yi]])
                                if e == 0:
                                    kb.op("dve", lambda v, tt=tt, ob=ob, yi=yi: v.tensor_copy(out=acc[:, tt, ob * 512:(ob + 1) * 512], in_=py[yi][:]),
                                          reads=[B_py[yi]], writes=[B_acc[tt]], waw=False)
                                else:
                                    kb.op("dve", lambda v, tt=tt, ob=ob, yi=yi: v.tensor_tensor(out=acc[:, tt, ob * 512:(ob + 1) * 512],
                                                                                                in0=py[yi][:], in1=acc[:, tt, ob * 512:(ob + 1) * 512],
                                                                                                op=ALU.add),
                                          reads=[B_py[yi], B_acc[tt]], writes=[B_acc[tt]], waw=False)

                    emit_sl(0)
                    emit_wbc(0, 0)
                    emit_wbc(0, 1)
                    for e in range(NE + 1):
                        if e + 1 <= NE:
                            emit_sl(e + 1)
                        for c in range(4):
                            u = e * 4 + c
                            if c == 1 and e > 0:
                                emit_down(e - 1)
                                load_d(e)
                            if u + 2 < NU:
                                load_gu(u + 2)
                            emit_gu(e, c)
                            if c in (1, 2) and e + 1 <= NE:
                                emit_wbc(e + 1, c - 1)
                    emit_down(NE)
                    kb.barrier()

                with contextlib.ExitStack() as s6b:
                    gtM = sb(s6b, "gtM", [128, D], F32)
                    gF = sb(s6b, "gF", [128, D], F32)
                    B_gc = Buf()
                    h1f = [sb(s6b, f"h1f{i}", [128, D], F32) for i in range(2)]
                    B_h1f = [Buf(), Buf()]
                    ot = [sb(s6b, f"ot{i}", [128, D], F32) for i in range(2)]
                    B_ot = [Buf(), Buf()]
                    jk = sb(s6b, "jk6", [128, D], BF16)
                    fs = [sb(s6b, f"fs{i}", [128, 1], F32) for i in range(2)]
                    B_fs = [Buf(), Buf()]
                    B_jk = Buf()
                    B_out = Buf()
                    kb.dma("sp", dq[8], gtM[:], modsave[1, :].partition_broadcast(128), writes=[B_gc])
                    kb.dma("sp", dq[8], gF[:], g_final[0, :].partition_broadcast(128), writes=[B_gc])
                    kb.dma("sp", dq[0], h1f[0][:], h1d[t0:t0 + 128, :], writes=[B_h1f[0]])
                    for tt in range(8):
                        i = tt % 2
                        r0 = t0 + tt * 128
                        if tt + 1 < 8:
                            kb.dma("sp", dq[1 - i], h1f[1 - i][:], h1d[r0 + 128:r0 + 256, :], writes=[B_h1f[1 - i]])
                        kb.op("dve", lambda v, tt=tt: v.tensor_tensor(out=acc[:, tt, :], in0=acc[:, tt, :], in1=gtM[:], op=ALU.mult),
                              reads=[B_acc[tt], B_gc], writes=[B_acc[tt]])
                        kb.op("dve", lambda v, tt=tt, i=i: v.tensor_tensor(out=h1f[i][:], in0=h1f[i][:], in1=acc[:, tt, :], op=ALU.add),
                              reads=[B_acc[tt], B_h1f[i]], writes=[B_h1f[i]])
                        kb.op("act", lambda a, i=i: a.activation(out=jk[:], in_=h1f[i][:], func=AF.Square, accum_out=fs[i][:]),
                              reads=[B_h1f[i]], writes=[B_jk, B_fs[i]])
                        kb.op("act", lambda a, i=i: a.activation(out=fs[i][:], in_=fs[i][:], func=AF.Sqrt, bias=1e-6, scale=1.0 / D),
                              reads=[B_fs[i]], writes=[B_fs[i]])
                        kb.op("dve", lambda v, i=i: v.reciprocal(out=fs[i][:], in_=fs[i][:]), reads=[B_fs[i]], writes=[B_fs[i]])
                        kb.op("dve", lambda v, i=i: v.scalar_tensor_tensor(out=ot[i][:], in0=h1f[i][:], scalar=fs[i][:, 0:1], in1=gF[:],
                                                                           op0=ALU.mult, op1=ALU.mult),
                              reads=[B_h1f[i], B_fs[i], B_gc], writes=[B_ot[i]])
                        kb.dma("sp", dq[2 + i], out[r0:r0 + 128, :], ot[i][:], reads=[B_ot[i]], writes=[B_out], waw=False)
                    kb.barrier()
    return nc


_NC = None


def t5_bucket(rel):
    nb = 16
    ret = (rel > 0).astype(np.int32) * nb
    n = np.abs(rel)
    max_exact = nb // 2
    large = max_exact + (np.log(np.maximum(n, 1) / max_exact) / math.log(128 / max_exact) * (nb - max_exact)).astype(np.int32)
    large = np.minimum(large, nb - 1)
    return (ret + np.where(n < max_exact, n, large)).astype(np.int32)


def attn_bias_tiles(t5, relb):
    k = np.arange(128)[:, None]
    q = np.arange(512)[None, :]
    tilesA = []
    for o in range(-1, 4):
        kpos = (4 + o) * 128 + k
        qpos = 4 * 128 + q
        idx = t5_bucket(kpos - qpos)
        allowed = (kpos // 64) <= (qpos // 64)
        tilesA.append(np.stack([np.where(allowed, t5[idx, h], np.float32(NEG)) for h in range(8)]))
    biasAT = np.concatenate(tilesA, axis=2).astype(np.float32)
    tilesB = []
    for o in range(-4, 4):
        kpos = (4 + o) * 128 + k
        qpos = 4 * 128 + q
        rel = np.clip(kpos - qpos, -256, 256) + 256
        kc, qc = kpos // 64, qpos // 64
        allowed = (kc <= qc) & (kc >= qc - 8)
        tilesB.append(np.stack([np.where(allowed, relb[rel, h], np.float32(NEG)) for h in range(8)]))
    biasBT = np.concatenate(tilesB, axis=2).astype(np.float32)
    cvecA = np.ascontiguousarray(np.broadcast_to(t5[15, :][None, :], (128, 8))).astype(np.float32)
    return np.ascontiguousarray(biasAT), np.ascontiguousarray(biasBT), cvecA


def kernel(x, c, w_ada, b_ada, g_attn, w_in, lambda_qk, t5_bias, rel_bias_b, w_up_a, w_up_b, w_o,
           g_moe, w_router, router_bias, w_exp_gate, w_exp_up, w_exp_down,
           w_sh_gate, w_sh_up, w_sh_down, g_final):
    global _NC
    f = lambda a: np.ascontiguousarray(np.asarray(a, dtype=np.float32))
    x = f(x); c = f(c)
    gfm = np.concatenate([f(g_attn)[0].reshape(16, 128).T, f(g_moe)[0].reshape(16, 128).T], axis=1)
    biasAT, biasBT, cvecA = attn_bias_tiles(f(t5_bias), f(rel_bias_b)[0])

    def lay(w):
        return w.reshape(16, 128, 4, 128).transpose(2, 1, 0, 3).reshape(4, 128, 2048)
    wg = f(w_exp_gate)[0]; wu = f(w_exp_up)[0]
    wgu = np.empty((NE + 1, 4, 128, 4096), np.float32)
    for e in range(NE):
        wgu[e, :, :, 0:2048] = lay(wg[e])
        wgu[e, :, :, 2048:4096] = lay(wu[e])
    wgu[NE, :, :, 0:2048] = lay(f(w_sh_gate)[0])
    wgu[NE, :, :, 2048:4096] = lay(f(w_sh_up)[0])
    wdn = np.concatenate([f(w_exp_down)[0], f(w_sh_down)], axis=0)

    shared = {
        "w_ada": f(w_ada)[0], "b_ada": f(b_ada), "gfm": np.ascontiguousarray(gfm), "g_final": f(g_final).reshape(1, D),
        "w_in": f(w_in)[0], "lam": f(lambda_qk).reshape(1, 256), "biasAT": biasAT, "biasBT": biasBT, "cvecA": cvecA,
        "w_up_a": f(w_up_a)[0], "w_up_b": f(w_up_b)[0], "w_o": f(w_o)[0], "w_router": f(w_router)[0],
        "rbias": f(router_bias), "wgu": wgu, "wd": np.ascontiguousarray(wdn),
    }
    in_maps = []
    for b in range(8):
        m = dict(shared)
        m["x"] = x[b]
        m["cT"] = np.ascontiguousarray(c[b].reshape(16, 128).T)
        in_maps.append(m)
    if _NC is None:
        _NC = build()
    res = run_bass_kernel_spmd(_NC, in_maps, core_ids=list(range(8)))
    return np.stack([np.asarray(r["out"], dtype=np.float32) for r in res.results], axis=0)
```

```python
import contextlib
import os
import math
import numpy as np
import concourse.bass as bass
import concourse.mybir as mybir
from concourse.bass_utils import run_bass_kernel_spmd

F32 = mybir.dt.float32
BF16 = mybir.dt.bfloat16
AF = mybir.ActivationFunctionType
ALU = mybir.AluOpType
AX = mybir.AxisListType

T = 2048
D = 2048
KC = 16
NE = 64
NEG = -1e30


class _Stop(Exception):
    pass


class Tk:
    __slots__ = ("sem", "val", "eng")

    def __init__(self, sem, val, eng):
        self.sem = sem
        self.val = val
        self.eng = eng


class Buf:
    def __init__(self, name=""):
        self.name = name
        self.w = {}
        self.r = {}


class DSem:
    def __init__(self, h):
        self.h = h
        self.cnt = 0


class KB:
    def __init__(self, nc, es):
        self.nc = nc
        self.es = es
        self.E = {"pe": nc.tensor, "act": nc.scalar, "dve": nc.vector, "pool": nc.gpsimd, "sp": nc.sync}
        self.psem = {e: es.enter_context(nc.semaphore("prog_" + e)) for e in ("pe", "act", "dve", "pool")}
        self.pcnt = {e: 0 for e in self.psem}
        self.waited = {e: {} for e in self.E}
        self.dsems = []

    def dsem(self, name):
        d = DSem(self.es.enter_context(self.nc.semaphore(name)))
        self.dsems.append(d)
        return d

    def wait(self, e, tk):
        if tk is None:
            return
        if tk.eng == "pe" and e == "pe":
            return
        k = id(tk.sem)
        if self.waited[e].get(k, 0) >= tk.val:
            return
        self.E[e].wait_ge(tk.sem, tk.val)
        self.waited[e][k] = tk.val

    def _deps(self, e, reads, writes, waw, skipsem=None):
        for b in reads:
            for t in b.w.values():
                self.wait(e, t)
        for b in writes:
            if waw:
                for t in b.w.values():
                    if skipsem is not None and t.sem is skipsem:
                        continue
                    self.wait(e, t)
            for t in b.r.values():
                self.wait(e, t)

    def _commit(self, tk, reads, writes, waw):
        k = id(tk.sem)
        for b in reads:
            o = b.r.get(k)
            if o is None or o.val < tk.val:
                b.r[k] = tk
        for b in writes:
            o = b.w.get(k)
            if o is None or o.val < tk.val:
                b.w[k] = tk

    def op(self, e, fn, reads=(), writes=(), waw=True):
        self._deps(e, reads, writes, waw)
        ins = fn(self.E[e])
        self.pcnt[e] += 1
        ins.then_inc(self.psem[e], 1)
        tk = Tk(self.psem[e], self.pcnt[e], e)
        self._commit(tk, reads, writes, waw)
        return tk

    def dma(self, q, ds, out, in_, reads=(), writes=(), waw=True):
        self._deps(q, reads, writes, waw, skipsem=ds.h)
        ins = self.E[q].dma_start(out=out, in_=in_)
        ds.cnt += 16
        ins.then_inc(ds.h, 16)
        tk = Tk(ds.h, ds.cnt, "dma")
        self._commit(tk, reads, writes, waw)
        return tk

    def barrier(self):
        for e in self.E:
            for p in self.psem:
                if self.pcnt[p] > 0:
                    self.wait(e, Tk(self.psem[p], self.pcnt[p], "x"))
            for d in self.dsems:
                if d.cnt > 0:
                    self.wait(e, Tk(d.h, d.cnt, "dma"))


def build(upto=9, dbg=False):
    nc = bass.Bass("TRN2", target_bir_lowering=False)

    def din(name, shape, dt=F32):
        return nc.dram_tensor(name, shape, dt, kind="ExternalInput").ap()

    def dscr(name, shape, dt):
        return nc.dram_tensor(name, shape, dt, kind=("ExternalOutput" if dbg else "Internal")).ap()

    x = din("x", [T, D])
    cT = din("cT", [128, 16])
    w_ada = din("w_ada", [D, 6 * D])
    b_ada = din("b_ada", [1, 6 * D])
    gfm = din("gfm", [128, 32])
    g_final = din("g_final", [1, D])
    w_in = din("w_in", [D, 10240])
    lam_in = din("lam", [1, 256])
    biasAT = din("biasAT", [8, 128, 5 * 512])
    biasBT = din("biasBT", [8, 128, 8 * 512])
    cvecA = din("cvecA", [128, 8])
    w_up_a = din("w_up_a", [1024, D])
    w_up_b = din("w_up_b", [1024, D])
    w_o = din("w_o", [D, D])
    w_router = din("w_router", [D, NE])
    rbias = din("rbias", [1, NE])
    if upto >= 7:
        wgu = din("wgu", [NE + 1, 4, 128, 4096])
        wd = din("wd", [NE + 1, 512, D])
    out = nc.dram_tensor("out", [T, D], F32, kind="ExternalOutput").ap()

    qkT = [dscr(f"qkT{i}", [1024, T], BF16) for i in range(4)]
    vv = [dscr(f"vv{i}", [T, 1024], BF16) for i in range(2)]
    gT = dscr("gT", [4096, T], BF16)
    h1d = dscr("h1d", [T, D], F32)
    VTd = dscr("VTd", [16, 128, KC * 128], BF16)
    wcTd = dscr("wcTd", [NE, T], F32)
    modsave = dscr("modsave", [2, D], F32)

    with contextlib.suppress(_Stop), contextlib.ExitStack() as es:
        kb = KB(nc, es)

        def phase(n):
            if n > upto:
                kb.barrier()
                raise _Stop()

        uniq = [0]

        def sb(stack, name, shape, dt):
            uniq[0] += 1
            return stack.enter_context(nc.sbuf_tensor(f"{name}_{uniq[0]}", shape, dt))

        def ps(stack, name, shape, dt):
            uniq[0] += 1
            return stack.enter_context(nc.psum_tensor(f"{name}_{uniq[0]}", shape, dt))

        identf = sb(es, "identf", [128, 128], F32)
        identb = sb(es, "identb", [128, 128], BF16)
        ones_f = sb(es, "ones_f", [128, 128], F32)
        B_const = Buf("const")
        kb.op("pool", lambda g: g.memset(identf[:], 1.0), writes=[B_const])
        kb.op("pool", lambda g: g.affine_select(out=identf[:], in_=identf[:], pattern=[[-1, 128]],
                                                compare_op=ALU.is_equal, fill=0.0, base=0,
                                                channel_multiplier=1), reads=[B_const], writes=[B_const])
        kb.op("dve", lambda v: v.tensor_copy(out=identb[:], in_=identf[:]), reads=[B_const], writes=[B_const], waw=False)
        kb.op("dve", lambda v: v.memset(ones_f[:], 1.0), writes=[B_const], waw=False)

        A_a = sb(es, "A_a", [128, 16], F32)
        S_a = sb(es, "S_a", [128, 16], F32)
        A_m = sb(es, "A_m", [128, 16], F32)
        S_m = sb(es, "S_m", [128, 16], F32)
        B_mod = Buf("modfm")

        dq = [kb.dsem(f"dq{i}") for i in range(12)]
        dp = [kb.dsem(f"dp{i}") for i in range(4)]

        with contextlib.ExitStack() as s0:
            cts = sb(s0, "cts", [128, 16], F32)
            scs = sb(s0, "scs", [128, 16], F32)
            gfs = sb(s0, "gfs", [128, 32], F32)
            scb = sb(s0, "scb", [128, 16, 128], F32)
            wab = [sb(s0, f"wab{i}", [128, 16, 512], F32) for i in range(2)]
            bbc = [sb(s0, f"bbc{i}", [128, 512], F32) for i in range(2)]
            modbc = [sb(s0, f"modbc{i}", [128, D], F32) for i in range(6)]
            tmpd = sb(s0, "tmpd", [128, 128], F32)
            fm = sb(s0, "fm", [128, 4, 16], F32)
            pmod = [ps(s0, f"pmod{i}", [128, 512], F32) for i in range(2)]
            B_c = Buf()
            B_scb = Buf()
            B_wab = [Buf(), Buf()]
            B_bbc = [Buf(), Buf()]
            B_pm = [Buf(), Buf()]
            B_modbc = [Buf() for _ in range(6)]
            B_tmp = Buf()
            B_fm = Buf()
            kb.dma("sp", dq[0], cts[:], cT[:, :], writes=[B_c])
            kb.dma("sp", dq[0], gfs[:], gfm[:, :], writes=[B_c])
            kb.op("act", lambda a: a.activation(out=scs[:], in_=cts[:], func=AF.Silu), reads=[B_c], writes=[B_scb])
            for kc in range(KC):
                kb.op("dve", lambda v, kc=kc: v.tensor_scalar(out=scb[:, kc, :], in0=ones_f[:], scalar1=scs[:, kc:kc + 1],
                                                             scalar2=None, op0=ALU.mult),
                      reads=[B_scb, B_const], writes=[B_c], waw=False)
            wav = w_ada.rearrange("(kc p) n -> p kc n", p=128)
            for j in range(24):
                i = j % 2
                kb.dma("sp", dq[1 + i], wab[i][:, 0:8, :], wav[:, 0:8, j * 512:(j + 1) * 512], writes=[B_wab[i]])
                kb.dma("act", dq[1 + i], wab[i][:, 8:16, :], wav[:, 8:16, j * 512:(j + 1) * 512], writes=[B_wab[i]])
                kb.dma("sp", dq[3 + i], bbc[i][:], b_ada[0, j * 512:(j + 1) * 512].partition_broadcast(128),
                       writes=[B_bbc[i]])

                def mm(pe, i=i):
                    r = None
                    for kc in range(KC):
                        r = pe.matmul(pmod[i][:], lhsT=scb[:, kc, :], rhs=wab[i][:, kc, :], start=(kc == 0), stop=(kc == KC - 1))
                    return r
                kb.op("pe", mm, reads=[B_c, B_wab[i]], writes=[B_pm[i]])
                mi, cb = j // 4, (j % 4) * 512
                kb.op("dve", lambda v, i=i, mi=mi, cb=cb: v.tensor_tensor(out=modbc[mi][:, cb:cb + 512], in0=pmod[i][:],
                                                                           in1=bbc[i][:], op=ALU.add),
                      reads=[B_pm[i], B_bbc[i]], writes=[B_modbc[mi]], waw=False)
            for fi, mi in enumerate((0, 1, 3, 4)):
                for kc in range(KC):
                    kb.op("dve", lambda v, mi=mi, kc=kc: v.tensor_tensor(out=tmpd[:], in0=modbc[mi][:, kc * 128:(kc + 1) * 128],
                                                                         in1=identf[:], op=ALU.mult),
                          reads=[B_modbc[mi], B_const], writes=[B_tmp])
                    kb.op("dve", lambda v, fi=fi, kc=kc: v.reduce_sum(out=fm[:, fi, kc:kc + 1], in_=tmpd[:], axis=AX.X),
                          reads=[B_tmp], writes=[B_fm], waw=False)
            kb.op("dve", lambda v: v.tensor_copy(out=S_a[:], in_=fm[:, 0, :]), reads=[B_fm], writes=[B_mod], waw=False)
            kb.op("dve", lambda v: v.tensor_copy(out=S_m[:], in_=fm[:, 2, :]), reads=[B_fm], writes=[B_mod], waw=False)
            kb.op("dve", lambda v: v.scalar_tensor_tensor(out=A_a[:], in0=fm[:, 1, :], scalar=1.0, in1=gfs[:, 0:16],
                                                          op0=ALU.add, op1=ALU.mult), reads=[B_fm, B_c], writes=[B_mod], waw=False)
            kb.op("dve", lambda v: v.scalar_tensor_tensor(out=A_m[:], in0=fm[:, 3, :], scalar=1.0, in1=gfs[:, 16:32],
                                                          op0=ALU.add, op1=ALU.mult), reads=[B_fm, B_c], writes=[B_mod], waw=False)
            B_ms = Buf()
            kb.dma("sp", dq[5], modsave[0:1, :], modbc[2][0:1, :], reads=[B_modbc[2]], writes=[B_ms], waw=False)
            kb.dma("sp", dq[5], modsave[1:2, :], modbc[5][0:1, :], reads=[B_modbc[5]], writes=[B_ms], waw=False)
            kb.barrier()

        def norm_tile(stack_bufs, src_tile, B_src, tt, A_fm, S_fm, dst_bf, B_dst, dst32=None, B_dst16=None):
            junk2, ss2, rstd2, xh2, p4, B_junk2, B_ss2, B_xh2, B_p4 = stack_bufs
            junk, ss, rstd, xh = junk2[tt % 2], ss2[tt % 2], rstd2[tt % 2], xh2[tt % 2]
            B_junk, B_ss, B_xh = B_junk2[tt % 2], B_ss2[tt % 2], B_xh2[tt % 2]
            kb.op("act", lambda a: a.activation(out=junk[:], in_=src_tile[:], func=AF.Square, accum_out=ss[:]),
                  reads=[B_src], writes=[B_junk, B_ss])
            kb.op("act", lambda a: a.activation(out=rstd[:], in_=ss[:], func=AF.Sqrt, bias=1e-6, scale=1.0 / D),
                  reads=[B_ss], writes=[B_ss])
            kb.op("dve", lambda v: v.reciprocal(out=rstd[:], in_=rstd[:]), reads=[B_ss], writes=[B_ss])
            kb.op("dve", lambda v: v.tensor_scalar(out=xh[:], in0=src_tile[:], scalar1=rstd[:, 0:1], scalar2=None, op0=ALU.mult),
                  reads=[B_src, B_ss], writes=[B_xh])

            def tr(pe):
                r = None
                for kc in range(KC):
                    r = pe.transpose(out=p4[:, kc * 128:(kc + 1) * 128], in_=xh[:, kc * 128:(kc + 1) * 128], identity=identf[:])
                return r
            kb.op("pe", tr, reads=[B_xh, B_const], writes=[B_p4])
            for kc in range(KC):
                o = dst32(kc) if dst32 is not None else dst_bf(kc)
                if kc < 8:
                    kb.op("act", lambda a, kc=kc, o=o: a.activation(out=o, in_=p4[:, kc * 128:(kc + 1) * 128], func=AF.Identity,
                                                                    bias=S_fm[:, kc:kc + 1], scale=A_fm[:, kc:kc + 1]),
                          reads=[B_p4, B_mod], writes=[B_dst], waw=False)
                else:
                    kb.op("dve", lambda v, kc=kc, o=o: v.tensor_scalar(out=o, in0=p4[:, kc * 128:(kc + 1) * 128],
                                                                       scalar1=A_fm[:, kc:kc + 1], scalar2=S_fm[:, kc:kc + 1],
                                                                       op0=ALU.mult, op1=ALU.add),
                          reads=[B_p4, B_mod], writes=[B_dst], waw=False)
            if dst32 is not None:
                kb.op("pool", lambda g: g.tensor_copy(out=dst_bf(None), in_=dst32(None)), reads=[B_dst], writes=[B_dst16])

        with contextlib.ExitStack() as s1:
            uT = sb(s1, "uT", [128, KC, T], BF16)
            B_uT = Buf("uT")
            phase(1)
            with contextlib.ExitStack() as s1a:
                xt = [sb(s1a, f"xt{i}", [128, D], F32) for i in range(2)]
                junk = [sb(s1a, f"junk{i}", [128, D], BF16) for i in range(2)]
                ss = [sb(s1a, f"ss{i}", [128, 1], F32) for i in range(2)]
                rstd = [sb(s1a, f"rstd{i}", [128, 1], F32) for i in range(2)]
                xh = [sb(s1a, f"xh{i}", [128, D], F32) for i in range(2)]
                p4 = ps(s1a, "p4", [128, D], F32)
                B_xt = [Buf(), Buf()]
                nb = (junk, ss, rstd, xh, p4, [Buf(), Buf()], [Buf(), Buf()], [Buf(), Buf()], Buf())
                for tt in range(int(os.environ.get('NT1', '16'))):
                    i = tt % 2
                    kb.dma("sp", dq[i], xt[i][:], x[tt * 128:(tt + 1) * 128, :], writes=[B_xt[i]])
                    norm_tile(nb, xt[i], B_xt[i], tt, A_a, S_a,
                              lambda kc, tt=tt: uT[:, kc, tt * 128:(tt + 1) * 128], B_uT)
                kb.barrier()

            phase(2)
            with contextlib.ExitStack() as s2:
                wb = [sb(s2, f"wb{i}", [128, KC, 512], BF16) for i in range(2)]
                B_wb = [Buf(), Buf()]
                stg = [sb(s2, f"stg{i}", [128, 512], BF16) for i in range(4)]
                B_stg = [Buf() for _ in range(4)]
                pb = [ps(s2, f"pb{i}", [128, 512], F32) for i in range(4)]
                B_pb = [Buf() for _ in range(4)]
                B_scr = Buf("scratchA2")
                wiv = w_in.rearrange("(kc p) n -> p kc n", p=128)
                cnt = [0]

                def evac_store(pi, kind, scale, dst_ap):
                    n = cnt[0]
                    cnt[0] += 1
                    si = n % 4
                    if kind == "sig":
                        kb.op("act", lambda a: a.activation(out=stg[si][:], in_=pb[pi][:], func=AF.Sigmoid),
                              reads=[B_pb[pi]], writes=[B_stg[si]])
                    elif n % 2 == 0:
                        kb.op("act", lambda a: a.activation(out=stg[si][:], in_=pb[pi][:], func=AF.Identity, scale=float(scale)),
                              reads=[B_pb[pi]], writes=[B_stg[si]])
                    else:
                        kb.op("dve", lambda v: v.tensor_scalar(out=stg[si][:], in0=pb[pi][:], scalar1=float(scale), scalar2=None,
                                                               op0=ALU.mult),
                              reads=[B_pb[pi]], writes=[B_stg[si]])
                    kb.dma("sp", dq[4 + si], dst_ap, stg[si][:], reads=[B_stg[si]], writes=[B_scr], waw=False)

                groups = []
                for g in range(20):
                    if g < 2:
                        groups.append(("fm", qkT[0], g * 512, 0.125, "lin"))
                    elif g < 4:
                        groups.append(("fm", qkT[1], (g - 2) * 512, 1.0, "lin"))
                    elif g < 6:
                        groups.append(("tm", vv[0], (g - 4) * 512, 1.0, "lin"))
                    elif g < 8:
                        groups.append(("fm", qkT[2], (g - 6) * 512, 128.0 ** -0.5, "lin"))
                    elif g < 10:
                        groups.append(("fm", qkT[3], (g - 8) * 512, 1.0, "lin"))
                    elif g < 12:
                        groups.append(("tm", vv[1], (g - 10) * 512, 1.0, "lin"))
                    else:
                        groups.append(("fm", gT, (g - 12) * 512, 1.0, "sig"))
                pcount = 0
                for g in range(20):
                    i = g % 2
                    kind, dst, base, scale, ev = groups[g]
                    kb.dma("pool", dp[i], wb[i][:, 0:8, :], wiv[:, 0:8, g * 512:(g + 1) * 512], writes=[B_wb[i]])
                    kb.dma("pool", dp[i], wb[i][:, 8:16, :], wiv[:, 8:16, g * 512:(g + 1) * 512], writes=[B_wb[i]])
                    if kind == "fm":
                        for m in range(4):
                            for tb in range(4):
                                pi = pcount % 4
                                pcount += 1

                                def mm(pe, i=i, m=m, tb=tb, pi=pi):
                                    r = None
                                    for kc in range(KC):
                                        r = pe.matmul(pb[pi][:], lhsT=wb[i][:, kc, m * 128:(m + 1) * 128],
                                                      rhs=uT[:, kc, tb * 512:(tb + 1) * 512], start=(kc == 0), stop=(kc == KC - 1))
                                    return r
                                kb.op("pe", mm, reads=[B_wb[i], B_uT], writes=[B_pb[pi]])
                                evac_store(pi, ev, scale, dst[base + m * 128: base + (m + 1) * 128, tb * 512:(tb + 1) * 512])
                    else:
                        for tt in range(16):
                            pi = pcount % 4
                            pcount += 1

                            def mm(pe, i=i, tt=tt, pi=pi):
                                r = None
                                for kc in range(KC):
                                    r = pe.matmul(pb[pi][:], lhsT=uT[:, kc, tt * 128:(tt + 1) * 128], rhs=wb[i][:, kc, :],
                                                  start=(kc == 0), stop=(kc == KC - 1))
                                return r
                            kb.op("pe", mm, reads=[B_wb[i], B_uT], writes=[B_pb[pi]])
                            evac_store(pi, ev, scale, dst[tt * 128:(tt + 1) * 128, base:base + 512])
                kb.barrier()

        phase(3)
        with contextlib.ExitStack() as s3:
            yT = [sb(s3, f"yT{i}", [128, 8, T], BF16) for i in range(2)]
            B_yT = [Buf(), Buf()]
            with contextlib.ExitStack() as s3a:
                qTh = [sb(s3a, f"qTh{i}", [128, T], BF16) for i in range(2)]
                kTh = [sb(s3a, f"kTh{i}", [128, T], BF16) for i in range(2)]
                vh = [sb(s3a, f"vh{i}", [128, 16, 128], BF16) for i in range(2)]
                Rh = [sb(s3a, f"Rh{i}", [128, 8 * 512], BF16) for i in range(2)]
                B_hd = [Buf(), Buf()]
                cA = sb(s3a, "cA", [128, 8], F32)
                ones_b = sb(s3a, "ones_b", [128, 128], BF16)
                lq = sb(s3a, "lq", [128, 256], F32)
                ltmp = sb(s3a, "ltmp", [128, 64], F32)
                lst = sb(s3a, "lst", [128, 4], F32)
                nl = sb(s3a, "nl", [128, 1], F32)
                B_l = Buf()
                NPB = 3
                PTs = [sb(s3a, f"PTs{i}", [128, 512], BF16) for i in range(NPB)]
                B_PTs = [Buf() for _ in range(NPB)]
                rden = [sb(s3a, f"rden{i}", [128, 512], F32) for i in range(2)]
                B_rden = [Buf(), Buf()]
                t0s = sb(s3a, "t0s", [128, 512], F32)
                oos = sb(s3a, "oos", [128, 512], F32)
                sqs = sb(s3a, "sqs", [128, 512], BF16)
                stds = sb(s3a, "stds", [128, 512], F32)
                B_t0, B_oo, B_sq, B_std = Buf(), Buf(), Buf(), Buf()
                ST = [ps(s3a, f"ST{i}", [128, 512], F32) for i in range(NPB)]
                B_ST = [Buf() for _ in range(NPB)]
                ACC = [ps(s3a, f"ACC{i}", [128, 512], F32) for i in range(2)]
                DEN = [ps(s3a, f"DEN{i}", [128, 512], F32) for i in range(2)]
                B_ACC = [Buf(), Buf()]
                B_DEN = [Buf(), Buf()]
                SSQ = ps(s3a, "SSQ", [128, 512], F32)
                B_SSQ = Buf()

                kb.op("dve", lambda v: v.memset(ones_b[:], 1.0), writes=[B_l])
                kb.dma("sp", dq[8], cA[:], cvecA[:, :], writes=[B_l], waw=False)
                kb.dma("sp", dq[8], lq[:], lam_in[0, :].partition_broadcast(128), writes=[B_l], waw=False)
                for j in range(2):
                    kb.op("dve", lambda v, j=j: v.tensor_tensor(out=ltmp[:], in0=lq[:, j * 128:j * 128 + 64],
                                                                in1=lq[:, j * 128 + 64:j * 128 + 128], op=ALU.mult),
                          reads=[B_l], writes=[B_l])
                    kb.op("dve", lambda v, j=j: v.reduce_sum(out=lst[:, j:j + 1], in_=ltmp[:], axis=AX.X), reads=[B_l], writes=[B_l])
                kb.op("act", lambda a: a.activation(out=lst[:, 2:4], in_=lst[:, 0:2], func=AF.Exp), reads=[B_l], writes=[B_l])
                lam_init = 0.8 - 0.6 * math.exp(-0.3 * 0)
                kb.op("dve", lambda v: v.scalar_tensor_tensor(out=nl[:], in0=lst[:, 3:4], scalar=-lam_init, in1=lst[:, 2:3],
                                                              op0=ALU.add, op1=ALU.subtract), reads=[B_l], writes=[B_l])

                pcnt_ = [0]
                gcnt_ = [0]
                deferred = []
                for which in range(2):
                    nmaps = 2 if which == 0 else 1
                    dk = 64 if which == 0 else 128
                    omin = -1 if which == 0 else -4
                    ntile = 5 if which == 0 else 8
                    bsrc = biasAT if which == 0 else biasBT
                    qd, kd, vd = qkT[2 * which], qkT[2 * which + 1], vv[which]
                    vdv = vd.rearrange("(tt p) c -> p tt c", p=128)
                    for h in range(8):
                        hi = (which * 8 + h) % 2
                        kb.dma("sp", dq[hi], qTh[hi][:], qd[h * 128:(h + 1) * 128, :], writes=[B_hd[hi]], waw=False)
                        kb.dma("sp", dq[hi], kTh[hi][:], kd[h * 128:(h + 1) * 128, :], writes=[B_hd[hi]], waw=False)
                        kb.dma("act", dq[hi], vh[hi][:], vdv[:, :, h * 128:(h + 1) * 128], writes=[B_hd[hi]], waw=False)
                        for half_ in range(2):
                            hw_ = ntile * 512 // 2
                            kb.dma("pool", dp[hi], Rh[hi][:, half_ * hw_:(half_ + 1) * hw_], bsrc[h, :, half_ * hw_:(half_ + 1) * hw_],
                                   writes=[B_hd[hi]], waw=False)
                        for g in range(4):
                            for m in range(nmaps):
                                gi = gcnt_[0] % 2
                                gcnt_[0] += 1
                                kb_lo = 0 if which == 0 else max(0, 4 * g - 4)
                                kbs = list(range(kb_lo, 4 * g + 4))
                                pend = []

                                def emit_pv(item, gi=gi, hi=hi):
                                    kbi, pi, c0, first, last = item

                                    def pv(pe):
                                        pe.matmul(ACC[gi][:, c0:512], lhsT=vh[hi][:, kbi, :], rhs=PTs[pi][:, c0:512], start=first, stop=last)
                                        return pe.matmul(DEN[gi][:, c0:512], lhsT=ones_b[:], rhs=PTs[pi][:, c0:512], start=first, stop=last)
                                    kb.op("pe", pv, reads=[B_PTs[pi], B_hd[hi], B_l], writes=[B_ACC[gi], B_DEN[gi]])

                                for idx, kbi in enumerate(kbs):
                                    o = kbi - 4 * g
                                    pi = pcnt_[0] % NPB
                                    pcnt_[0] += 1
                                    c0 = max(0, o) * 128
                                    near = (o >= omin)

                                    def qk(pe, hi=hi, m=m, g=g, kbi=kbi, o=o, pi=pi, c0=c0, near=near):
                                        r = pe.matmul(ST[pi][:, c0:512], lhsT=kTh[hi][m * dk:(m + 1) * dk, kbi * 128:(kbi + 1) * 128],
                                                      rhs=qTh[hi][m * dk:(m + 1) * dk, g * 512 + c0:(g + 1) * 512], start=True, stop=(not near))
                                        if near:
                                            t_ = o - omin
                                            r = pe.matmul(ST[pi][:, c0:512], lhsT=identb[:], rhs=Rh[hi][:, t_ * 512 + c0:(t_ + 1) * 512],
                                                          start=False, stop=True)
                                        return r
                                    kb.op("pe", qk, reads=[B_hd[hi], B_const], writes=[B_ST[pi]])
                                    if near:
                                        kb.op("act", lambda a, pi=pi, c0=c0: a.activation(out=PTs[pi][:, c0:512], in_=ST[pi][:, c0:512], func=AF.Exp),
                                              reads=[B_ST[pi]], writes=[B_PTs[pi]])
                                    else:
                                        kb.op("act", lambda a, pi=pi, c0=c0, h=h: a.activation(out=PTs[pi][:, c0:512], in_=ST[pi][:, c0:512], func=AF.Exp,
                                                                                               bias=cA[:, h:h + 1], scale=1.0),
                                              reads=[B_ST[pi], B_l], writes=[B_PTs[pi]])
                                    pend.append((kbi, pi, c0, idx == 0, idx == len(kbs) - 1))
                                    if len(pend) > 2:
                                        emit_pv(pend.pop(0))
                                    if idx == 3:
                                        while deferred:
                                            deferred.pop(0)()
                                while pend:
                                    emit_pv(pend.pop(0))
                                while deferred:
                                    deferred.pop(0)()
                                qs = slice(g * 512, (g + 1) * 512)
                                kb.op("dve", lambda v, gi=gi: v.reciprocal(out=rden[gi][:], in_=DEN[gi][:]), reads=[B_DEN[gi]], writes=[B_rden[gi]])
                                if which == 1:
                                    kb.op("dve", lambda v, gi=gi, h=h, qs=qs: v.tensor_tensor(out=yT[1][:, h, qs], in0=ACC[gi][:], in1=rden[gi][:], op=ALU.mult),
                                          reads=[B_ACC[gi], B_rden[gi]], writes=[B_yT[1]], waw=False)
                                elif m == 0:
                                    kb.op("dve", lambda v, gi=gi: v.tensor_tensor(out=t0s[:], in0=ACC[gi][:], in1=rden[gi][:], op=ALU.mult),
                                          reads=[B_ACC[gi], B_rden[gi]], writes=[B_t0])
                                else:
                                    kb.op("dve", lambda v, gi=gi: v.tensor_tensor(out=oos[:], in0=ACC[gi][:], in1=rden[gi][:], op=ALU.mult),
                                          reads=[B_ACC[gi], B_rden[gi]], writes=[B_oo])
                                    kb.op("dve", lambda v: v.scalar_tensor_tensor(out=oos[:], in0=oos[:], scalar=nl[:, 0:1], in1=t0s[:],
                                                                                  op0=ALU.mult, op1=ALU.add),
                                          reads=[B_oo, B_t0, B_l], writes=[B_oo])
                                    kb.op("dve", lambda v: v.tensor_tensor(out=sqs[:], in0=oos[:], in1=oos[:], op=ALU.mult), reads=[B_oo], writes=[B_sq])
                                    def tail(h=h, qs=qs):
                                        kb.op("pe", lambda pe: pe.matmul(SSQ[:], lhsT=ones_b[:], rhs=sqs[:], start=True, stop=True),
                                              reads=[B_sq, B_l], writes=[B_SSQ])
                                        kb.op("act", lambda a: a.activation(out=stds[:], in_=SSQ[:], func=AF.Sqrt, bias=1e-5, scale=1.0 / 128),
                                              reads=[B_SSQ], writes=[B_std])
                                        kb.op("dve", lambda v: v.reciprocal(out=stds[:], in_=stds[:]), reads=[B_std], writes=[B_std])
                                        kb.op("dve", lambda v: v.scalar_tensor_tensor(out=yT[0][:, h, qs], in0=oos[:], scalar=float(1.0 - lam_init),
                                                                                      in1=stds[:], op0=ALU.mult, op1=ALU.mult),
                                              reads=[B_oo, B_std], writes=[B_yT[0]], waw=False)
                                    deferred.append(tail)
                kb.barrier()

            phase(4)
            with contextlib.ExitStack() as s4:
                mT = sb(s4, "mT", [128, KC, T], BF16)
                B_mT = Buf("mT")
                with contextlib.ExitStack() as s4a:
                    wua = [sb(s4a, f"wua{i}", [128, 8, 256], BF16) for i in range(2)]
                    wub = [sb(s4a, f"wub{i}", [128, 8, 256], BF16) for i in range(2)]
                    B_wu = [Buf(), Buf()]
                    gat = [sb(s4a, f"gat{i}", [128, 512], BF16) for i in range(2)]
                    gbt = [sb(s4a, f"gbt{i}", [128, 512], BF16) for i in range(2)]
                    B_gt = [Buf(), Buf()]
                    m1 = [sb(s4a, f"m1{i}", [128, 512], F32) for i in range(2)]
                    m2 = [sb(s4a, f"m2{i}", [128, 512], F32) for i in range(2)]
                    B_m = [Buf(), Buf()]
                    pa = [ps(s4a, f"pa{i}", [128, 512], F32) for i in range(2)]
                    pbb = [ps(s4a, f"pbb{i}", [128, 512], F32) for i in range(2)]
                    B_pab = [Buf(), Buf()]
                    wuav = w_up_a.rearrange("(kc p) n -> p kc n", p=128)
                    wubv = w_up_b.rearrange("(kc p) n -> p kc n", p=128)
                    n = 0
                    for dg in range(8):
                        wi = dg % 2
                        kb.dma("pool", dp[wi], wua[wi][:], wuav[:, :, dg * 256:(dg + 1) * 256], writes=[B_wu[wi]])
                        kb.dma("pool", dp[wi], wub[wi][:], wubv[:, :, dg * 256:(dg + 1) * 256], writes=[B_wu[wi]])
                        for sub in range(2):
                            dc = dg * 2 + sub
                            for tb in range(4):
                                i = n % 2
                                n += 1
                                kb.dma("sp", dq[2 + i], gat[i][:], gT[dc * 128:(dc + 1) * 128, tb * 512:(tb + 1) * 512], writes=[B_gt[i]])
                                kb.dma("sp", dq[2 + i], gbt[i][:], gT[2048 + dc * 128:2048 + (dc + 1) * 128, tb * 512:(tb + 1) * 512],
                                       writes=[B_gt[i]])

                                def mm(pe, wi=wi, sub=sub, tb=tb, i=i):
                                    r = None
                                    for kc in range(8):
                                        pe.matmul(pa[i][:], lhsT=wua[wi][:, kc, sub * 128:(sub + 1) * 128],
                                                  rhs=yT[0][:, kc, tb * 512:(tb + 1) * 512], start=(kc == 0), stop=(kc == 7))
                                    for kc in range(8):
                                        r = pe.matmul(pbb[i][:], lhsT=wub[wi][:, kc, sub * 128:(sub + 1) * 128],
                                                      rhs=yT[1][:, kc, tb * 512:(tb + 1) * 512], start=(kc == 0), stop=(kc == 7))
                                    return r
                                kb.op("pe", mm, reads=[B_wu[wi], B_yT[0], B_yT[1]], writes=[B_pab[i]])
                                kb.op("dve", lambda v, i=i: v.tensor_tensor(out=m1[i][:], in0=pa[i][:], in1=gat[i][:], op=ALU.mult),
                                      reads=[B_pab[i], B_gt[i]], writes=[B_m[i]])
                                kb.op("dve", lambda v, i=i: v.tensor_tensor(out=m2[i][:], in0=pbb[i][:], in1=gbt[i][:], op=ALU.mult),
                                      reads=[B_pab[i], B_gt[i]], writes=[B_m[i]], waw=False)
                                kb.op("dve", lambda v, i=i, dc=dc, tb=tb: v.tensor_tensor(out=mT[:, dc, tb * 512:(tb + 1) * 512], in0=m1[i][:],
                                                                                          in1=m2[i][:], op=ALU.add),
                                      reads=[B_m[i]], writes=[B_mT], waw=False)
                    kb.barrier()

                phase(5)
                with contextlib.ExitStack() as s4b:
                    wo = [sb(s4b, f"wo{i}", [128, KC, 512], BF16) for i in range(2)]
                    B_wo = [Buf(), Buf()]
                    gtA = sb(s4b, "gtA", [128, D], F32)
                    B_gtA = Buf()
                    xp = [sb(s4b, f"xp{i}", [128, 512], F32) for i in range(3)]
                    B_xp = [Buf() for _ in range(3)]
                    hp = [sb(s4b, f"hp{i}", [128, 512], F32) for i in range(3)]
                    B_hp = [Buf() for _ in range(3)]
                    po = [ps(s4b, f"po{i}", [128, 512], F32) for i in range(3)]
                    B_po = [Buf() for _ in range(3)]
                    B_h1 = Buf("h1d")
                    kb.dma("sp", dq[8], gtA[:], modsave[0, :].partition_broadcast(128), writes=[B_gtA])
                    wov = w_o.rearrange("(kc p) n -> p kc n", p=128)
                    n = 0
                    for ob in range(4):
                        wi = ob % 2
                        kb.dma("pool", dp[wi], wo[wi][:, 0:8, :], wov[:, 0:8, ob * 512:(ob + 1) * 512], writes=[B_wo[wi]])
                        kb.dma("pool", dp[wi], wo[wi][:, 8:16, :], wov[:, 8:16, ob * 512:(ob + 1) * 512], writes=[B_wo[wi]])
                        for tt in range(16):
                            i = n % 3
                            n += 1
                            kb.dma("sp", dq[2 + i], xp[i][:], x[tt * 128:(tt + 1) * 128, ob * 512:(ob + 1) * 512], writes=[B_xp[i]])

                            def mm(pe, wi=wi, tt=tt, i=i):
                                r = None
                                for kc in range(KC):
                                    r = pe.matmul(po[i][:], lhsT=mT[:, kc, tt * 128:(tt + 1) * 128], rhs=wo[wi][:, kc, :],
                                                  start=(kc == 0), stop=(kc == KC - 1))
                                return r
                            kb.op("pe", mm, reads=[B_wo[wi], B_mT], writes=[B_po[i]])
                            kb.op("dve", lambda v, i=i, ob=ob: v.tensor_tensor(out=hp[i][:], in0=po[i][:], in1=gtA[:, ob * 512:(ob + 1) * 512],
                                                                               op=ALU.mult),
                                  reads=[B_po[i], B_gtA], writes=[B_hp[i]])
                            kb.op("dve", lambda v, i=i: v.tensor_tensor(out=hp[i][:], in0=hp[i][:], in1=xp[i][:], op=ALU.add),
                                  reads=[B_hp[i], B_xp[i]], writes=[B_hp[i]])
                            kb.dma("act", dq[5 + i], h1d[tt * 128:(tt + 1) * 128, ob * 512:(ob + 1) * 512], hp[i][:],
                                   reads=[B_hp[i]], writes=[B_h1], waw=False)
                    kb.barrier()

        phase(6)
        with contextlib.ExitStack() as s5:
            h1t = [sb(s5, f"h1t{i}", [128, D], F32) for i in range(2)]
            B_h1t = [Buf(), Buf()]
            junk = [sb(s5, f"junk5{i}", [128, D], BF16) for i in range(2)]
            ss = [sb(s5, f"ss5{i}", [128, 1], F32) for i in range(2)]
            rstd = [sb(s5, f"rstd5{i}", [128, 1], F32) for i in range(2)]
            xh = [sb(s5, f"xh5{i}", [128, D], F32) for i in range(2)]
            p4 = ps(s5, "p45", [128, D], F32)
            nb = (junk, ss, rstd, xh, p4, [Buf(), Buf()], [Buf(), Buf()], [Buf(), Buf()], Buf())
            v16 = [sb(s5, f"v16{i}", [128, KC, 128], BF16) for i in range(2)]
            v32 = [sb(s5, f"v32{i}", [128, KC, 128], F32) for i in range(2)]
            B_v = [Buf(), Buf()]
            B_v16 = [Buf(), Buf()]
            wr = sb(s5, "wr", [128, KC, NE], F32)
            rb = sb(s5, "rb", [128, NE], F32)
            B_wr = Buf()
            plg_ = ps(s5, "plg", [128, 512], F32)
            plg = plg_[:, 0:NE]
            pwt_ = ps(s5, "pwt", [128, 512], F32)
            pwt = pwt_[0:NE, 0:128]
            B_plg, B_pwt = Buf(), Buf()
            sc = sb(s5, "sc", [128, NE], F32)
            bi = sb(s5, "bi", [128, NE], F32)
            t8 = sb(s5, "t8", [128, 8, 8], F32)
            gs = sb(s5, "gs", [128, 8], F32)
            g8 = sb(s5, "g8", [128, 8], F32)
            gm = sb(s5, "gm", [128, 8], F32)
            mb = sb(s5, "mb", [128, 8], F32)
            msk = sb(s5, "msk", [128, NE], F32)
            m8 = sb(s5, "m8", [128, 8], F32)
            sel = sb(s5, "sel", [128, NE], F32)
            den = sb(s5, "den", [128, 1], F32)
            wc = sb(s5, "wc", [128, NE], F32)
            wcs = [sb(s5, f"wcs{i}", [NE, 128], F32) for i in range(2)]
            B_wcs = [Buf(), Buf()]
            B_rt = Buf()
            B_VTd, B_wcTd = Buf(), Buf()
            kb.dma("sp", dq[8], wr[:], w_router.rearrange("(kc p) n -> p kc n", p=128), writes=[B_wr])
            kb.dma("sp", dq[8], rb[:], rbias[0, :].partition_broadcast(128), writes=[B_wr])
            kb.dma("sp", dq[0], h1t[0][:], h1d[0:128, :], writes=[B_h1t[0]])

            def stage_a(tt):
                i = tt % 2
                if tt + 1 < 16:
                    kb.dma("sp", dq[1 - i], h1t[1 - i][:], h1d[(tt + 1) * 128:(tt + 2) * 128, :], writes=[B_h1t[1 - i]])
                norm_tile(nb, h1t[i], B_h1t[i], tt, A_m, S_m,
                          lambda kc, i=i: (v16[i][:] if kc is None else v16[i][:, kc, :]), B_v[i],
                          dst32=lambda kc, i=i: (v32[i][:] if kc is None else v32[i][:, kc, :]), B_dst16=B_v16[i])
                kb.dma("sp", dq[2 + i], VTd[tt], v16[i][:].rearrange("p k t -> p (k t)"),
                       reads=[B_v16[i]], writes=[B_VTd], waw=False)

            def stage_b(tt):
                i = tt % 2
                def mm(pe, i=i):
                    r = None
                    for kc in range(KC):
                        r = pe.matmul(plg[:], lhsT=v32[i][:, kc, :], rhs=wr[:, kc, :], start=(kc == 0), stop=(kc == KC - 1))
                    return r
                kb.op("pe", mm, reads=[B_v[i], B_wr], writes=[B_plg])
                R = [B_rt]
                kb.op("act", lambda a: a.activation(out=sc[:], in_=plg[:], func=AF.Sigmoid), reads=[B_plg], writes=R)
                kb.op("dve", lambda v: v.tensor_tensor(out=bi[:], in0=sc[:], in1=rb[:], op=ALU.add), reads=R + [B_wr], writes=R)
                for g in range(8):
                    kb.op("dve", lambda v, g=g: v.max(out=t8[:, g, :], in_=bi[:, g * 8:(g + 1) * 8]), reads=R, writes=R)
                kb.op("dve", lambda v: v.tensor_tensor(out=gs[:], in0=t8[:, :, 0], in1=t8[:, :, 1], op=ALU.add), reads=R, writes=R)
                kb.op("dve", lambda v: v.max(out=g8[:], in_=gs[:]), reads=R, writes=R)
                kb.op("dve", lambda v: v.tensor_scalar(out=gm[:], in0=gs[:], scalar1=g8[:, 3:4], scalar2=None, op0=ALU.is_ge),
                      reads=R, writes=R)
                kb.op("dve", lambda v: v.tensor_scalar(out=mb[:], in0=gm[:], scalar1=-1.0, scalar2=1e9, op0=ALU.add, op1=ALU.mult),
                      reads=R, writes=R)
                for g in range(8):
                    kb.op("dve", lambda v, g=g: v.tensor_scalar(out=msk[:, g * 8:(g + 1) * 8], in0=bi[:, g * 8:(g + 1) * 8],
                                                               scalar1=gm[:, g:g + 1], scalar2=mb[:, g:g + 1], op0=ALU.mult, op1=ALU.add),
                          reads=R, writes=R)
                kb.op("dve", lambda v: v.max(out=m8[:], in_=msk[:]), reads=R, writes=R)
                kb.op("dve", lambda v: v.tensor_scalar(out=sel[:], in0=msk[:], scalar1=m8[:, 7:8], scalar2=None, op0=ALU.is_ge),
                      reads=R, writes=R)
                kb.op("dve", lambda v: v.tensor_tensor(out=sel[:], in0=sel[:], in1=sc[:], op=ALU.mult), reads=R, writes=R)
                kb.op("dve", lambda v: v.reduce_sum(out=den[:], in_=sel[:], axis=AX.X), reads=R, writes=R)
                kb.op("dve", lambda v: v.reciprocal(out=den[:], in_=den[:]), reads=R, writes=R)
                kb.op("dve", lambda v: v.tensor_scalar(out=wc[:], in0=sel[:], scalar1=den[:, 0:1], scalar2=2.5, op0=ALU.mult, op1=ALU.mult),
                      reads=R, writes=R)
                kb.op("pe", lambda pe: pe.transpose(out=pwt[:], in_=wc[:], identity=identf[:]), reads=R + [B_const], writes=[B_pwt])
                kb.op("act", lambda a, i=i: a.copy(out=wcs[i][:], in_=pwt[:]), reads=[B_pwt], writes=[B_wcs[i]])
                kb.dma("pool", dp[2 + i], wcTd[:, tt * 128:(tt + 1) * 128], wcs[i][:], reads=[B_wcs[i]], writes=[B_wcTd], waw=False)

            stage_a(0)
            for tt in range(16):
                if tt + 1 < 16:
                    stage_a(tt + 1)
                stage_b(tt)
            kb.barrier()

        phase(7)
        for pz in range(2):
            t0 = pz * 1024
            with contextlib.ExitStack() as s6:
                acc = sb(s6, "acc", [128, 8, D], F32)
                B_acc = [Buf() for _ in range(8)]
                with contextlib.ExitStack() as s6a:
                    vt = sb(s6a, "vt", [128, 8, KC * 128], BF16)
                    B_vt = Buf()
                    wct = sb(s6a, "wct", [NE, 1024], F32)
                    B_wct = Buf()
                    gu = [sb(s6a, f"gu{i}", [128, 2, KC, 128], BF16) for i in range(3)]
                    B_gu = [Buf() for _ in range(3)]
                    dd = sb(s6a, "dd", [128, 4, D], BF16)
                    B_dd = Buf()
                    hT = [sb(s6a, f"hT{i}", [128, 4, 1024], BF16) for i in range(2)]
                    B_hT = [Buf(), Buf()]
                    sg = [sb(s6a, f"sg{i}", [128, 512], F32) for i in range(2)]
                    sgw = [sb(s6a, f"sgw{i}", [128, 512], F32) for i in range(2)]
                    B_sg = [Buf(), Buf()]
                    B_sgw = [Buf(), Buf()]
                    wbc = [sb(s6a, f"wbc{i}", [128, 1024], F32) for i in range(2)]
                    B_wbc = [Buf(), Buf()]
                    sl = [sb(s6a, f"sl{i}", [NE, 128], F32) for i in range(2)]
                    B_sl = [Buf(), Buf()]
                    pg = [ps(s6a, f"pg{i}", [128, 512], F32) for i in range(2)]
                    pu = [ps(s6a, f"pu{i}", [128, 512], F32) for i in range(2)]
                    B_pgu = [Buf(), Buf()]
                    py = [ps(s6a, f"py{i}", [128, 512], F32) for i in range(3)]
                    B_py = [Buf() for _ in range(3)]
                    pw = ps(s6a, "pw", [128, 512], F32)
                    B_pw = Buf()

                    for q4 in range(4):
                        kb.dma("sp", dq[8], vt[:, q4 * 2:(q4 + 1) * 2, :],
                               VTd[pz * 8 + q4 * 2:pz * 8 + (q4 + 1) * 2].rearrange("t p f -> p t f"), writes=[B_vt])
                    kb.dma("sp", dq[9], wct[:], wcTd[:, t0:t0 + 1024], writes=[B_wct])

                    NU = (NE + 1) * 4

                    def load_gu(u):
                        e, c = divmod(u, 4)
                        bi_ = u % 3
                        kb.dma("pool", dp[bi_], gu[bi_][:].rearrange("p g k m -> p g (k m)"),
                               wgu[e, c].rearrange("p (g j) -> p g j", g=2), writes=[B_gu[bi_]])

                    def load_d(e):
                        for c in range(4):
                            kb.dma("pool", dp[3], dd[:, c, :], wd[e, c * 128:(c + 1) * 128, :], writes=[B_dd])

                    load_gu(0)
                    load_gu(1)
                    load_d(0)
                    ny = [0]

                    def emit_sl(e):
                        si = e % 2
                        if e < NE:
                            kb.op("dve", lambda v: v.tensor_scalar(out=sl[si][:], in0=ones_f[0:NE, :], scalar1=identf[0:NE, e:e + 1],
                                                                   scalar2=None, op0=ALU.mult),
                                  reads=[B_const], writes=[B_sl[si]])
                        else:
                            kb.op("dve", lambda v: v.memset(wbc[si][:], 1.0), writes=[B_wbc[si]])

                    def emit_wbc(e, j):
                        si = e % 2
                        if e >= NE:
                            return
                        kb.op("pe", lambda pe: pe.matmul(pw[:], lhsT=sl[si][:], rhs=wct[:, j * 512:(j + 1) * 512], start=True, stop=True),
                              reads=[B_sl[si], B_wct], writes=[B_pw])
                        kb.op("act", lambda a: a.copy(out=wbc[si][:, j * 512:(j + 1) * 512], in_=pw[:]),
                              reads=[B_pw], writes=[B_wbc[si]], waw=(j == 0))

                    def emit_gu(e, c):
                        u = e * 4 + c
                        bi_ = u % 3
                        wi = e % 2
                        for tb in range(2):
                            i = (u * 2 + tb) % 2

                            def mm(pe, bi_=bi_, tb=tb, i=i):
                                r = None
                                for kc in range(KC):
                                    pe.matmul(pg[i][:].rearrange("p (a b) -> p a b", a=4), lhsT=gu[bi_][:, 0, kc, :],
                                              rhs=vt[:, tb * 4:(tb + 1) * 4, kc * 128:(kc + 1) * 128],
                                              start=(kc == 0), stop=(kc == KC - 1))
                                for kc in range(KC):
                                    r = pe.matmul(pu[i][:].rearrange("p (a b) -> p a b", a=4), lhsT=gu[bi_][:, 1, kc, :],
                                                  rhs=vt[:, tb * 4:(tb + 1) * 4, kc * 128:(kc + 1) * 128],
                                                  start=(kc == 0), stop=(kc == KC - 1))
                                return r
                            kb.op("pe", mm, reads=[B_gu[bi_], B_vt], writes=[B_pgu[i]])
                            kb.op("act", lambda a, i=i: a.activation(out=sg[i][:], in_=pg[i][:], func=AF.Silu),
                                  reads=[B_pgu[i]], writes=[B_sg[i]])
                            kb.op("dve", lambda v, i=i, tb=tb: v.tensor_tensor(out=sgw[i][:], in0=sg[i][:], in1=wbc[wi][:, tb * 512:(tb + 1) * 512],
                                                                               op=ALU.mult),
                                  reads=[B_sg[i], B_wbc[wi]], writes=[B_sgw[i]])
                            kb.op("dve", lambda v, i=i, tb=tb: v.tensor_tensor(out=hT[wi][:, c, tb * 512:(tb + 1) * 512], in0=pu[i][:],
                                                                               in1=sgw[i][:], op=ALU.mult),
                                  reads=[B_pgu[i], B_sgw[i]], writes=[B_hT[wi]], waw=False)

                    def emit_down(e):
                        wi = e % 2
                        for tt in range(8):
                            for ob in range(4):
                                yi = ny[0] % 3
                                ny[0] += 1

                                def mmd(pe, tt=tt, ob=ob, yi=yi):
                                    r = None
                                    for c in range(4):
                                        r = pe.matmul(py[yi][:], lhsT=hT[wi][:, c, tt * 128:(tt + 1) * 128], rhs=dd[:, c, ob * 512:(ob + 1) * 512],
                                                      start=(c == 0), stop=(c == 3))
                                    return r
                                kb.op("pe", mmd, reads=[B_hT[wi], B_dd], writes=[B_py[yi]])
                                if e == 0:
                                    kb.op("dve", lambda v, tt=tt, ob=ob, yi=yi: v.tensor_copy(out=acc[:, tt, ob * 512:(ob + 1) * 512], in_=py[yi][:]),
                                          reads=[B_py[yi]], writes=[B_acc[tt]], waw=False)
                                else:
                                    kb.op("dve", lambda v, tt=tt, ob=ob, yi=yi: v.tensor_tensor(out=acc[:, tt, ob * 512:(ob + 1) * 512],
                                                                                                in0=py[yi][:], in1=acc[:, tt, ob * 512:(ob + 1) * 512],
                                                                                                op=ALU.add),
                                          reads=[B_py[yi], B_acc[tt]], writes=[B_acc[tt]], waw=False)

                    emit_sl(0)
                    emit_wbc(0, 0)
                    emit_wbc(0, 1)
                    for e in range(NE + 1):
                        if e + 1 <= NE:
                            emit_sl(e + 1)
                        for c in range(4):
                            u = e * 4 + c
                            if c == 1 and e > 0:
                                emit_down(e - 1)
                                load_d(e)
                            if u + 2 < NU:
                                load_gu(u + 2)
                            emit_gu(e, c)
                            if c in (1, 2) and e + 1 <= NE:
                                emit_wbc(e + 1, c - 1)
                    emit_down(NE)
                    kb.barrier()

                with contextlib.ExitStack() as s6b:
                    gtM = sb(s6b, "gtM", [128, D], F32)
                    gF = sb(s6b, "gF", [128, D], F32)
                    B_gc = Buf()
                    h1f = [sb(s6b, f"h1f{i}", [128, D], F32) for i in range(2)]
                    B_h1f = [Buf(), Buf()]
                    ot = [sb(s6b, f"ot{i}", [128, D], F32) for i in range(2)]
                    B_ot = [Buf(), Buf()]
                    jk = sb(s6b, "jk6", [128, D], BF16)
                    fs = [sb(s6b, f"fs{i}", [128, 1], F32) for i in range(2)]
                    B_fs = [Buf(), Buf()]
                    B_jk = Buf()
                    B_out = Buf()
                    kb.dma("sp", dq[8], gtM[:], modsave[1, :].partition_broadcast(128), writes=[B_gc])
                    kb.dma("sp", dq[8], gF[:], g_final[0, :].partition_broadcast(128), writes=[B_gc])
                    kb.dma("sp", dq[0], h1f[0][:], h1d[t0:t0 + 128, :], writes=[B_h1f[0]])
                    for tt in range(8):
                        i = tt % 2
                        r0 = t0 + tt * 128
                        if tt + 1 < 8:
                            kb.dma("sp", dq[1 - i], h1f[1 - i][:], h1d[r0 + 128:r0 + 256, :], writes=[B_h1f[1 - i]])
                        kb.op("dve", lambda v, tt=tt: v.tensor_tensor(out=acc[:, tt, :], in0=acc[:, tt, :], in1=gtM[:], op=ALU.mult),
                              reads=[B_acc[tt], B_gc], writes=[B_acc[tt]])
                        kb.op("dve", lambda v, tt=tt, i=i: v.tensor_tensor(out=h1f[i][:], in0=h1f[i][:], in1=acc[:, tt, :], op=ALU.add),
                              reads=[B_acc[tt], B_h1f[i]], writes=[B_h1f[i]])
                        kb.op("act", lambda a, i=i: a.activation(out=jk[:], in_=h1f[i][:], func=AF.Square, accum_out=fs[i][:]),
                              reads=[B_h1f[i]], writes=[B_jk, B_fs[i]])
                        kb.op("act", lambda a, i=i: a.activation(out=fs[i][:], in_=fs[i][:], func=AF.Sqrt, bias=1e-6, scale=1.0 / D),
                              reads=[B_fs[i]], writes=[B_fs[i]])
                        kb.op("dve", lambda v, i=i: v.reciprocal(out=fs[i][:], in_=fs[i][:]), reads=[B_fs[i]], writes=[B_fs[i]])
                        kb.op("dve", lambda v, i=i: v.scalar_tensor_tensor(out=ot[i][:], in0=h1f[i][:], scalar=fs[i][:, 0:1], in1=gF[:],
                                                                           op0=ALU.mult, op1=ALU.mult),
                              reads=[B_h1f[i], B_fs[i], B_gc], writes=[B_ot[i]])
                        kb.dma("sp", dq[2 + i], out[r0:r0 + 128, :], ot[i][:], reads=[B_ot[i]], writes=[B_out], waw=False)
                    kb.barrier()
    return nc


_NC = None


def t5_bucket(rel):
    nb = 16
    ret = (rel > 0).astype(np.int32) * nb
    n = np.abs(rel)
    max_exact = nb // 2
    large = max_exact + (np.log(np.maximum(n, 1) / max_exact) / math.log(128 / max_exact) * (nb - max_exact)).astype(np.int32)
    large = np.minimum(large, nb - 1)
    return (ret + np.where(n < max_exact, n, large)).astype(np.int32)


def attn_bias_tiles(t5, relb):
    k = np.arange(128)[:, None]
    q = np.arange(512)[None, :]
    tilesA = []
    for o in range(-1, 4):
        kpos = (4 + o) * 128 + k
        qpos = 4 * 128 + q
        idx = t5_bucket(kpos - qpos)
        allowed = (kpos // 64) <= (qpos // 64)
        tilesA.append(np.stack([np.where(allowed, t5[idx, h], np.float32(NEG)) for h in range(8)]))
    biasAT = np.concatenate(tilesA, axis=2).astype(np.float32)
    tilesB = []
    for o in range(-4, 4):
        kpos = (4 + o) * 128 + k
        qpos = 4 * 128 + q
        rel = np.clip(kpos - qpos, -256, 256) + 256
        kc, qc = kpos // 64, qpos // 64
        allowed = (kc <= qc) & (kc >= qc - 8)
        tilesB.append(np.stack([np.where(allowed, relb[rel, h], np.float32(NEG)) for h in range(8)]))
    biasBT = np.concatenate(tilesB, axis=2).astype(np.float32)
    cvecA = np.ascontiguousarray(np.broadcast_to(t5[15, :][None, :], (128, 8))).astype(np.float32)
    return np.ascontiguousarray(biasAT), np.ascontiguousarray(biasBT), cvecA


def kernel(x, c, w_ada, b_ada, g_attn, w_in, lambda_qk, t5_bias, rel_bias_b, w_up_a, w_up_b, w_o,
           g_moe, w_router, router_bias, w_exp_gate, w_exp_up, w_exp_down,
           w_sh_gate, w_sh_up, w_sh_down, g_final):
    global _NC
    f = lambda a: np.ascontiguousarray(np.asarray(a, dtype=np.float32))
    x = f(x); c = f(c)
    gfm = np.concatenate([f(g_attn)[0].reshape(16, 128).T, f(g_moe)[0].reshape(16, 128).T], axis=1)
    biasAT, biasBT, cvecA = attn_bias_tiles(f(t5_bias), f(rel_bias_b)[0])

    def lay(w):
        return w.reshape(16, 128, 4, 128).transpose(2, 1, 0, 3).reshape(4, 128, 2048)
    wg = f(w_exp_gate)[0]; wu = f(w_exp_up)[0]
    wgu = np.empty((NE + 1, 4, 128, 4096), np.float32)
    for e in range(NE):
        wgu[e, :, :, 0:2048] = lay(wg[e])
        wgu[e, :, :, 2048:4096] = lay(wu[e])
    wgu[NE, :, :, 0:2048] = lay(f(w_sh_gate)[0])
    wgu[NE, :, :, 2048:4096] = lay(f(w_sh_up)[0])
    wdn = np.concatenate([f(w_exp_down)[0], f(w_sh_down)], axis=0)

    shared = {
        "w_ada": f(w_ada)[0], "b_ada": f(b_ada), "gfm": np.ascontiguousarray(gfm), "g_final": f(g_final).reshape(1, D),
        "w_in": f(w_in)[0], "lam": f(lambda_qk).reshape(1, 256), "biasAT": biasAT, "biasBT": biasBT, "cvecA": cvecA,
        "w_up_a": f(w_up_a)[0], "w_up_b": f(w_up_b)[0], "w_o": f(w_o)[0], "w_router": f(w_router)[0],
        "rbias": f(router_bias), "wgu": wgu, "wd": np.ascontiguousarray(wdn),
    }
    in_maps = []
    for b in range(8):
        m = dict(shared)
        m["x"] = x[b]
        m["cT"] = np.ascontiguousarray(c[b].reshape(16, 128).T)
        in_maps.append(m)
    if _NC is None:
        _NC = build()
    res = run_bass_kernel_spmd(_NC, in_maps, core_ids=list(range(8)))
    return np.stack([np.asarray(r["out"], dtype=np.float32) for r in res.results], axis=0)
```
